# Optimizing a Trainium2 kernel written in Bass

```python
import jax, jax.numpy as jnp
from jax import lax
import numpy as np

D_MODEL = 2048
BATCH = 2
SEQ = 8192
DEPTH = 4

N_A_LAYERS = DEPTH // 2
N_B_LAYERS = DEPTH - N_A_LAYERS
D_PLE = 256
D_FF = 5504
MLSTM_HEADS = 4
MLSTM_DQK = D_MODEL // (2 * MLSTM_HEADS)
MLSTM_DV = D_MODEL // MLSTM_HEADS
MLSTM_CHUNK = 64
MLSTM_PROJ = 2 * MLSTM_HEADS * MLSTM_DQK + 2 * MLSTM_HEADS * MLSTM_DV + 2 * MLSTM_HEADS
FORGET_BIAS_OFFSET = 3.0
SB_HEADS = 16
SB_HEAD_DIM = D_MODEL // SB_HEADS
SB_BLOCK = 128
RMS_EPS = 1e-6

kernel_name = "yoco_mlstm_stickbreaking_macaron"


def rmsnorm(x, g):
    xf = x.astype(jnp.float32)
    y = xf * lax.rsqrt(jnp.mean(xf * xf, axis=-1, keepdims=True) + RMS_EPS)
    return (y * g.astype(jnp.float32)).astype(x.dtype)


def swiglu(x, w_gate, w_up, w_down):
    return (jax.nn.silu(x @ w_gate) * (x @ w_up)) @ w_down


def mlstm_mixer(h, w_in, b_if, g_head, w_out):
    B, S, _ = h.shape
    H, dk, dv, L = MLSTM_HEADS, MLSTM_DQK, MLSTM_DV, MLSTM_CHUNK
    NC = S // L
    proj = (h @ w_in).astype(jnp.float32)
    o1 = H * dk; o2 = 2 * H * dk; o3 = o2 + H * dv; o4 = o3 + H * dv; o5 = o4 + H
    q, k, v, og, gi, gf = jnp.split(proj, [o1, o2, o3, o4, o5], axis=-1)
    b_if = b_if.astype(jnp.float32)
    log_i = gi + b_if[:H]
    log_f = jax.nn.log_sigmoid(gf + b_if[H:])
    q = q * (dk ** -0.5)

    def to_chunks(t, d):
        return t.reshape(B, NC, L, H, d).transpose(1, 0, 3, 2, 4)

    def gate_chunks(t):
        return t.reshape(B, NC, L, H).transpose(1, 0, 3, 2)

    xs = (to_chunks(q, dk), to_chunks(k, dk), to_chunks(v, dv), gate_chunks(log_i), gate_chunks(log_f))
    causal = jnp.tril(jnp.ones((L, L), dtype=bool))

    def chunk_step(carry, inp):
        C, n, m = carry
        qc, kc, vc, ic, fc = inp
        b = jnp.cumsum(fc, axis=-1)
        a = b + m[..., None]
        Dm = jnp.where(causal, b[..., :, None] - b[..., None, :] + ic[..., None, :], -jnp.inf)
        m_t = jnp.maximum(a, jnp.max(Dm, axis=-1))
        w_inter = jnp.exp(a - m_t)
        s_qk = jnp.einsum('bhtd,bhsd->bhts', qc, kc) * jnp.exp(Dm - m_t[..., None])
        num = w_inter[..., None] * jnp.einsum('bhtd,bhde->bhte', qc, C) + jnp.einsum('bhts,bhse->bhte', s_qk, vc)
        den = w_inter * jnp.einsum('bhtd,bhd->bht', qc, n) + jnp.sum(s_qk, axis=-1)
        h_out = num / jnp.maximum(jnp.abs(den), jnp.exp(-m_t))[..., None]
        bL = b[..., -1]
        g_end = bL[..., None] - b + ic
        m_new = jnp.maximum(bL + m, jnp.max(g_end, axis=-1))
        decay = jnp.exp(bL + m - m_new)
        w_end = jnp.exp(g_end - m_new[..., None])
        C_new = decay[..., None, None] * C + jnp.einsum('bhs,bhsd,bhse->bhde', w_end, kc, vc)
        n_new = decay[..., None] * n + jnp.einsum('bhs,bhsd->bhd', w_end, kc)
        return (C_new, n_new, m_new), h_out

    init = (jnp.zeros((B, H, dk, dv), jnp.float32), jnp.zeros((B, H, dk), jnp.float32), jnp.zeros((B, H), jnp.float32))
    _, hs = lax.scan(chunk_step, init, xs)
    hs = hs.transpose(1, 0, 3, 2, 4).reshape(B, S, H, dv)
    hs = rmsnorm(hs, g_head.reshape(H, dv)).reshape(B, S, H * dv)
    hs = hs * jax.nn.sigmoid(og)
    return hs.astype(h.dtype) @ w_out


def shared_kv(h, g_kv, w_kv, g_k):
    B, S, _ = h.shape
    kv = (rmsnorm(h, g_kv) @ w_kv).astype(jnp.float32)
    k, v = jnp.split(kv, 2, axis=-1)
    k = rmsnorm(k.reshape(B, S, SB_HEADS, SB_HEAD_DIM), g_k)
    v = v.reshape(B, S, SB_HEADS, SB_HEAD_DIM)
    return k.transpose(0, 2, 1, 3), v.transpose(0, 2, 1, 3)


def stick_breaking_mixer(h, w_q, g_q, w_o, k, v):
    B, S, _ = h.shape
    q = (h @ w_q).astype(jnp.float32).reshape(B, S, SB_HEADS, SB_HEAD_DIM)
    q = (rmsnorm(q, g_q) * (SB_HEAD_DIM ** -0.5)).transpose(0, 2, 1, 3)
    outs = []
    for blk in range(S // SB_BLOCK):
        t0 = blk * SB_BLOCK
        kn = t0 + SB_BLOCK
        z = jnp.einsum('bhtd,bhsd->bhts', q[:, :, t0:kn], k[:, :, :kn])
        mask = jnp.arange(kn)[None, :] < (t0 + jnp.arange(SB_BLOCK))[:, None]
        log_beta = jax.nn.log_sigmoid(z)
        log_keep = jnp.where(mask, jax.nn.log_sigmoid(-z), 0.0)
        after = lax.cumsum(log_keep, axis=log_keep.ndim - 1, reverse=True) - log_keep
        A = jnp.where(mask, jnp.exp(log_beta + after), 0.0)
        outs.append(jnp.einsum('bhts,bhsd->bhtd', A, v[:, :, :kn]))
    o = jnp.concatenate(outs, axis=2).transpose(0, 2, 1, 3).reshape(B, S, SB_HEADS * SB_HEAD_DIM)
    return o.astype(h.dtype) @ w_o


def per_layer_embedding(h, p_i, g, w_proj, w_gate):
    gate = jax.nn.sigmoid(rmsnorm(h, g) @ w_gate)
    return h + (p_i.astype(h.dtype) @ w_proj) * gate


def setup_inputs(seed: int = 0) -> dict:
    key = jax.random.key(seed)
    ks = jax.random.split(key, 24)
    f32 = jnp.float32

    def nrm(k, shape, scale):
        return jax.random.normal(k, shape, f32) * scale

    def gain(k, shape):
        return 1.0 + 0.02 * jax.random.normal(k, shape, f32)

    H = MLSTM_HEADS
    b_i = 0.1 * jax.random.normal(ks[8], (N_A_LAYERS, H), f32)
    b_f = FORGET_BIAS_OFFSET + 0.1 * jax.random.normal(ks[9], (N_A_LAYERS, H), f32)
    return {
        "x": nrm(ks[0], (BATCH, SEQ, D_MODEL), 1.0),
        "p": nrm(ks[1], (DEPTH, BATCH, SEQ, D_PLE), 1.0),
        "ffn_norm": gain(ks[2], (DEPTH, 2, D_MODEL)),
        "ffn_w_gate": nrm(ks[3], (DEPTH, 2, D_MODEL, D_FF), D_MODEL ** -0.5),
        "ffn_w_up": nrm(ks[4], (DEPTH, 2, D_MODEL, D_FF), D_MODEL ** -0.5),
        "ffn_w_down": nrm(ks[5], (DEPTH, 2, D_FF, D_MODEL), D_FF ** -0.5),
        "mix_norm": gain(ks[6], (DEPTH, D_MODEL)),
        "mlstm_w_in": nrm(ks[7], (N_A_LAYERS, D_MODEL, MLSTM_PROJ), D_MODEL ** -0.5),
        "mlstm_b_if": jnp.concatenate([b_i, b_f], axis=-1),
        "mlstm_head_norm": gain(ks[10], (N_A_LAYERS, H * MLSTM_DV)),
        "mlstm_w_out": nrm(ks[11], (N_A_LAYERS, H * MLSTM_DV, D_MODEL), (H * MLSTM_DV) ** -0.5),
        "kv_norm": gain(ks[12], (D_MODEL,)),
        "sb_w_kv": nrm(ks[13], (D_MODEL, 2 * SB_HEADS * SB_HEAD_DIM), D_MODEL ** -0.5),
        "sb_k_norm": gain(ks[14], (SB_HEAD_DIM,)),
        "sb_w_q": nrm(ks[15], (N_B_LAYERS, D_MODEL, SB_HEADS * SB_HEAD_DIM), D_MODEL ** -0.5),
        "sb_q_norm": gain(ks[16], (N_B_LAYERS, SB_HEAD_DIM)),
        "sb_w_o": nrm(ks[17], (N_B_LAYERS, SB_HEADS * SB_HEAD_DIM, D_MODEL), (SB_HEADS * SB_HEAD_DIM) ** -0.5),
        "ple_norm": gain(ks[18], (DEPTH, D_MODEL)),
        "ple_w_proj": nrm(ks[19], (DEPTH, D_PLE, D_MODEL), D_PLE ** -0.5),
        "ple_w_gate": nrm(ks[20], (DEPTH, D_MODEL, D_MODEL), D_MODEL ** -0.5),
    }


def reference(x, p, ffn_norm, ffn_w_gate, ffn_w_up, ffn_w_down, mix_norm,
              mlstm_w_in, mlstm_b_if, mlstm_head_norm, mlstm_w_out,
              kv_norm, sb_w_kv, sb_k_norm, sb_w_q, sb_q_norm, sb_w_o,
              ple_norm, ple_w_proj, ple_w_gate):
    h = x
    k_sh = None
    v_sh = None
    for i in range(DEPTH):
        if i == N_A_LAYERS:
            k_sh, v_sh = shared_kv(h, kv_norm, sb_w_kv, sb_k_norm)
        h = h + 0.5 * swiglu(rmsnorm(h, ffn_norm[i, 0]), ffn_w_gate[i, 0], ffn_w_up[i, 0], ffn_w_down[i, 0])
        hn = rmsnorm(h, mix_norm[i])
        if i < N_A_LAYERS:
            h = h + mlstm_mixer(hn, mlstm_w_in[i], mlstm_b_if[i], mlstm_head_norm[i], mlstm_w_out[i])
        else:
            j = i - N_A_LAYERS
            h = h + stick_breaking_mixer(hn, sb_w_q[j], sb_q_norm[j], sb_w_o[j], k_sh, v_sh)
        h = h + 0.5 * swiglu(rmsnorm(h, ffn_norm[i, 1]), ffn_w_gate[i, 1], ffn_w_up[i, 1], ffn_w_down[i, 1])
        h = per_layer_embedding(h, p[i], ple_norm[i], ple_w_proj[i], ple_w_gate[i])
    return h
```

```python
import contextlib
import numpy as np
import concourse.bass as bass
import concourse.mybir as mybir
from concourse.bass_utils import run_bass_kernel_spmd

F32 = mybir.dt.float32
BF16 = mybir.dt.bfloat16
AF = mybir.ActivationFunctionType
ALU = mybir.AluOpType

D = 2048
KC = D // 128
DFF = 5504
FC = DFF // 128
EPS = 1e-6
NCORES = 8
PENG = 'dve'
SELF_ORDERED = {'pe'}
NSTF = 4


class Op:
    __slots__ = ("eng", "fn", "deps", "slot", "needed", "tok", "waits", "know", "inc")

    def __init__(self, eng, fn, slot, inc=16):
        self.eng = eng
        self.fn = fn
        self.slot = slot
        self.inc = inc
        self.deps = set()
        self.needed = False
        self.tok = None
        self.waits = []
        self.know = None


class Sched:
    ENGS = ("pe", "act", "dve", "pool", "sp")

    def __init__(self, nc, prefix=""):
        self.nc = nc
        self.prefix = prefix
        self.ops = []
        self.last_w = {}
        self.readers = {}
        self.stack = contextlib.ExitStack()
        self.n_t = 0

    def uid(self):
        self.n_t += 1
        return self.n_t

    def sb(self, name, shape, dt):
        return self.stack.enter_context(self.nc.sbuf_tensor(self.prefix + name, shape, dt))

    def ps(self, name, shape, dt=F32):
        return self.stack.enter_context(self.nc.psum_tensor(self.prefix + name, shape, dt))

    def add(self, eng, fn, r=(), w=(), slot=None, inc=16, group=False):
        op = Op(eng, fn, slot, inc)
        deps = op.deps
        for k in r:
            d = self.last_w.get(k)
            if d is not None:
                deps.add(d)
        for k in w:
            d = self.last_w.get(k)
            if d is not None and not (group and d.slot == slot):
                deps.add(d)
            elif d is not None:
                deps.update(x for x in d.deps if x.slot != slot)
            rs = self.readers.get(k)
            if rs:
                deps.update(rs)
        for k in w:
            self.last_w[k] = op
            self.readers[k] = set()
        for k in r:
            self.readers.setdefault(k, set()).add(op)
        deps.discard(op)
        self.ops.append(op)
        return op

    def emit(self):
        nc = self.nc
        ops = self.ops
        for op in ops:
            for d in op.deps:
                if d.eng == op.eng and d.eng in SELF_ORDERED and d.slot is None and op.slot is None:
                    continue
                d.needed = True
        cnt = {e: 0 for e in self.ENGS}
        slot_cnt = {}
        for op in ops:
            if op.slot is not None:
                slot_cnt[op.slot] = slot_cnt.get(op.slot, 0) + op.inc
                op.tok = (("slot", op.slot), slot_cnt[op.slot])
            elif op.needed:
                cnt[op.eng] += 1
                op.tok = (("eng", op.eng), cnt[op.eng])
        sems = {}
        for e in self.ENGS:
            sems[("eng", e)] = nc.alloc_semaphore(self.prefix + "s_" + e)
        for i, sl in enumerate(slot_cnt):
            sems[("slot", sl)] = nc.alloc_semaphore(self.prefix + "d%d" % i)
        known = {e: {} for e in self.ENGS}
        for op in ops:
            kn = known[op.eng]
            for d in op.deps:
                if d.eng == op.eng and d.eng in SELF_ORDERED and d.slot is None and op.slot is None:
                    continue
                sk, val = d.tok
                if kn.get(sk, 0) >= val:
                    continue
                op.waits.append((sk, val))
                for k2, v2 in d.know.items():
                    if kn.get(k2, 0) < v2:
                        kn[k2] = v2
            if op.tok is not None:
                kn2 = dict(kn)
                sk, val = op.tok
                if kn2.get(sk, 0) < val:
                    kn2[sk] = val
                op.know = kn2
        per_eng = {e: [] for e in self.ENGS}
        for op in ops:
            w = {}
            for sk, val in op.waits:
                if w.get(sk, 0) < val:
                    w[sk] = val
            op.waits = list(w.items())
            per_eng[op.eng].append(op)
        final_waits = [(("slot", sl), v) for sl, v in slot_cnt.items()]
        self.stats = {e: len(per_eng[e]) for e in self.ENGS}

        def run(engobj, lst, final=False):
            for op in lst:
                for sk, val in op.waits:
                    engobj.wait_ge(sems[sk], val)
                ins = op.fn(engobj)
                if op.tok is not None:
                    if op.slot is not None and op.inc == 1:
                        ins.then_inc(sems[op.tok[0]])
                    else:
                        ins.then_inc(sems[op.tok[0]], op.inc if op.slot is not None else 1)
            if final:
                for sk, val in final_waits:
                    engobj.wait_ge(sems[sk], val)

        with nc.Block(self.prefix + "blk") as block:
            @block.tensor
            def _(e):
                run(e, per_eng["pe"])

            @block.scalar
            def _(e):
                run(e, per_eng["act"])

            @block.vector
            def _(e):
                run(e, per_eng["dve"])

            @block.gpsimd
            def _(e):
                run(e, per_eng["pool"])

            @block.sync
            def _(e):
                run(e, per_eng["sp"], final=True)


class Ctx:
    def __init__(self, S, TT):
        self.S = S
        self.TT = TT
        nc = S.nc
        self.ones = S.sb("ones_bf", [128, 128], BF16)
        self.banks = [S.ps("bank%d" % i, [128, 512], F32) for i in range(8)]
        S.add("dve", lambda e: e.memset(self.ones[:], 1.0), w=[("ones",)])
        self.xt = S.sb("xt", [128, KC, TT], F32)
        self.xn = S.sb("xn", [128, KC, TT], BF16)
        self.sq = S.sb("sq", [128, KC, TT], BF16)
        self.rs = S.sb("rstd", [128, TT], F32)
        self.rs2 = S.sb("rstd2", [128, TT], F32)
        self.gv = {}

    def load_vec(self, name, dram_ap, nchunk):
        S = self.S
        t = S.sb("v_" + name, [128, nchunk], F32)
        S.add("sp", lambda e: e.dma_start(out=t[:], in_=dram_ap), w=[("v", name)], slot="v_" + name)
        self.gv[name] = t
        return t


def emit_norm(C, xin, t0, gname, load_x=True):
    S, TT = C.S, C.TT
    xt, xn, sq, rs, rs2 = C.xt, C.xn, C.sq, C.rs, C.rs2
    g = C.gv[gname]
    ssb = C.banks[0]
    if load_x:
        src = xin.rearrange("(c p) t -> p c t", p=128)[:, :, t0:t0 + TT]
        S.add("sp", lambda e: e.dma_start(out=xt[:], in_=src), w=[("xt",)], slot="xt")
    S.add("act", lambda e: e.activation(out=sq[:], in_=xt[:], func=AF.Square), r=[("xt",)], w=[("sq",)])
    for c in range(KC):
        S.add("pe", lambda e, c=c: e.matmul(ssb[:, :TT], C.ones[:], sq[:, c, :], start=(c == 0), stop=(c == KC - 1)),
              r=[("sq",), ("ones",)], w=[("ps", 0)])
    S.add("dve", lambda e: e.tensor_scalar(rs[:], ssb[:, :TT], 1.0 / D, EPS, ALU.mult, ALU.add),
          r=[("ps", 0)], w=[("rs",)])
    S.add("act", lambda e: e.activation(out=rs2[:], in_=rs[:], func=AF.Sqrt), r=[("rs",)], w=[("rs2",)])
    S.add("dve", lambda e: e.reciprocal(rs[:], rs2[:]), r=[("rs2",)], w=[("rs",)])
    for c in range(KC):
        S.add("dve", lambda e, c=c: e.scalar_tensor_tensor(xn[:, c, :], xt[:, c, :], g[:, c:c + 1], rs[:],
                                                           ALU.mult, ALU.mult),
              r=[("xt",), ("rs",), ("v", gname)], w=[("xn", c)])


def emit_ffn(C, xin, xout, T, gname, wg, wu, wd):
    S, TT = C.S, C.TT
    if not hasattr(C, "wg"):
        C.wg = [S.sb("wg%d" % i, [128, KC, 512], BF16) for i in range(2)]
        C.wu = [S.sb("wu%d" % i, [128, KC, 512], BF16) for i in range(2)]
        C.wd = [S.sb("wd%d" % i, [128, 11, 512], BF16) for i in range(2)]
        C.hid = S.sb("hid", [128, FC, TT], BF16)
        C.sg = [S.sb("sg%d" % i, [128, TT], F32) for i in range(2)]
        C.ctr = {"gu": 0, "wd": 0, "f": 0}
    wg_r = wg.rearrange("(c p) n -> p c n", p=128)
    wu_r = wu.rearrange("(c p) n -> p c n", p=128)
    wd_r = wd.rearrange("(f p) n -> p f n", p=128)
    xo_r = xout.rearrange("(c p) t -> p c t", p=128)
    hid = C.hid
    for t0 in range(0, T, TT):
        emit_norm(C, xin, t0, gname)
        for fg in range(0, FC, 4):
            nf = min(4, FC - fg)
            sl = C.ctr["gu"] % 2
            C.ctr["gu"] += 1
            wgt, wut = C.wg[sl], C.wu[sl]
            S.add("pool", lambda e, wgt=wgt, fg=fg, nf=nf: e.dma_start(
                out=wgt[:, :, :nf * 128], in_=wg_r[:, :, fg * 128:(fg + nf) * 128]),
                w=[("wg", sl)], slot="wg%d" % sl)
            S.add("pool", lambda e, wut=wut, fg=fg, nf=nf: e.dma_start(
                out=wut[:, :, :nf * 128], in_=wu_r[:, :, fg * 128:(fg + nf) * 128]),
                w=[("wu", sl)], slot="wu%d" % sl)
            for fi in range(nf):
                f = fg + fi
                pb = C.ctr["f"] % 2
                C.ctr["f"] += 1
                gb, ub = C.banks[4 + pb], C.banks[6 + pb]
                for c in range(KC):
                    S.add("pe", lambda e, c=c, fi=fi, gb=gb, wgt=wgt: e.matmul(
                        gb[:, :TT], wgt[:, c, fi * 128:(fi + 1) * 128], C.xn[:, c, :], start=(c == 0), stop=(c == KC - 1)),
                        r=[("wg", sl), ("xn", c)], w=[("ps", 4 + pb)])
                for c in range(KC):
                    S.add("pe", lambda e, c=c, fi=fi, ub=ub, wut=wut: e.matmul(
                        ub[:, :TT], wut[:, c, fi * 128:(fi + 1) * 128], C.xn[:, c, :], start=(c == 0), stop=(c == KC - 1)),
                        r=[("wu", sl), ("xn", c)], w=[("ps", 6 + pb)])
                sg = C.sg[pb]
                S.add("act", lambda e, sg=sg, gb=gb: e.activation(out=sg[:], in_=gb[:, :TT], func=AF.Silu),
                      r=[("ps", 4 + pb)], w=[("sg", pb)])
                S.add("dve", lambda e, sg=sg, ub=ub, f=f: e.tensor_tensor(hid[:, f, :], ub[:, :TT], sg[:], ALU.mult),
                      r=[("ps", 6 + pb), ("sg", pb)], w=[("hid", f)])
        for ng in range(4):
            for f0 in range(0, FC, 11):
                nf = min(11, FC - f0)
                sl = C.ctr["wd"] % 2
                C.ctr["wd"] += 1
                wdt = C.wd[sl]
                S.add("pool", lambda e, wdt=wdt, f0=f0, nf=nf, ng=ng: e.dma_start(
                    out=wdt[:, :nf, :], in_=wd_r[:, f0:f0 + nf, ng * 512:(ng + 1) * 512]),
                    w=[("wd", sl)], slot="wd%d" % sl)
                for fi in range(nf):
                    f = f0 + fi
                    for j in range(4):
                        S.add("pe", lambda e, wdt=wdt, fi=fi, f=f, j=j: e.matmul(
                            C.banks[j][:, :TT], wdt[:, fi, j * 128:(j + 1) * 128], hid[:, f, :],
                            start=(f == 0), stop=(f == FC - 1)),
                            r=[("wd", sl), ("hid", f)], w=[("ps", j)])
            for j in range(4):
                c = ng * 4 + j
                S.add("dve", lambda e, j=j, c=c: e.scalar_tensor_tensor(
                    C.xt[:, c, :], C.banks[j][:, :TT], 0.5, C.xt[:, c, :], ALU.mult, ALU.add),
                    r=[("ps", j), ("xt",)], w=[("xt",)])
        S.add("sp", lambda e, t0=t0: e.dma_start(out=xo_r[:, :, t0:t0 + TT], in_=C.xt[:]),
              r=[("xt",)], w=[("dram", "xout")], slot="xt_st")


def ctx_extra(C):
    S, TT = C.S, C.TT
    if hasattr(C, "wsl"):
        return
    if not hasattr(C, "wg"):
        C.wg = [S.sb("wg%d" % i, [128, KC, 512], BF16) for i in range(2)]
        C.wu = [S.sb("wu%d" % i, [128, KC, 512], BF16) for i in range(2)]
        C.sg = [S.sb("sg%d" % i, [128, TT], F32) for i in range(2)]
    C.wsl = [(C.wg[0], ("wg", 0), "wg0"), (C.wu[0], ("wu", 0), "wu0"),
             (C.wg[1], ("wg", 1), "wg1"), (C.wu[1], ("wu", 1), "wu1")]
    C.wctr = 0
    C.bctr = 0
    C.stb = [S.sb("stb%d" % i, [128, 4, 512], BF16) for i in range(2)]
    C.stf = [S.sb("stf%d" % i, [128, 4, 512], F32) for i in range(NSTF)]
    C.stctr = {"b": 0, "f": 0}
    C.kf = S.sb("kf", [128, TT], F32)
    C.sqh = S.sb("sqh", [128, TT], BF16)
    C.hr = S.sb("hr", [128, TT], F32)
    C.hr2 = S.sb("hr2", [128, TT], F32)


def next_w(C):
    t = C.wsl[C.wctr % 4]
    C.wctr += 1
    return t


def next_bank(C):
    b = 4 + (C.bctr % 4)
    C.bctr += 1
    return b


def next_stage(C, kind):
    i = C.stctr[kind] % 2
    C.stctr[kind] += 1
    return (C.stb if kind == "b" else C.stf)[i], ("st" + kind, i), "st%s%d" % (kind, i)


def fm_linear(C, W_r, c0, ncols, kch, rhs, rkey, epilogue):
    S, TT = C.S, C.TT
    for gi, g0 in enumerate(range(0, ncols, 512)):
        gw = min(512, ncols - g0)
        wt, wkey, wslot = next_w(C)
        S.add("pool", lambda e, wt=wt, g0=g0, gw=gw: e.dma_start(
            out=wt[:, :kch, :gw], in_=W_r[:, :, c0 + g0:c0 + g0 + gw]), w=[wkey], slot=wslot)
        nj = (gw + 127) // 128
        for j in range(nj):
            m = min(128, gw - j * 128)
            b = next_bank(C)
            for c in range(kch):
                S.add("pe", lambda e, wt=wt, c=c, j=j, m=m, b=b: e.matmul(
                    C.banks[b][:m, :TT], wt[:, c, j * 128:j * 128 + m], rhs[:, c, :], start=(c == 0), stop=(c == kch - 1)),
                    r=[wkey, rkey(c)], w=[("ps", b)])
            epilogue(gi, j, nj, m, b)


def tm_linear(C, W_r, c0, ncols, epilogue):
    S, TT = C.S, C.TT
    for gi, g0 in enumerate(range(0, ncols, 512)):
        gw = min(512, ncols - g0)
        wt, wkey, wslot = next_w(C)
        S.add("pool", lambda e, wt=wt, g0=g0, gw=gw: e.dma_start(
            out=wt[:, :, :gw], in_=W_r[:, :, c0 + g0:c0 + g0 + gw]), w=[wkey], slot=wslot)
        for tb in range(TT // 128):
            b = next_bank(C)
            for c in range(KC):
                S.add("pe", lambda e, wt=wt, c=c, tb=tb, gw=gw, b=b: e.matmul(
                    C.banks[b][:, :gw], C.xn[:, c, tb * 128:(tb + 1) * 128], wt[:, c, :gw], start=(c == 0), stop=(c == KC - 1)),
                    r=[wkey, ("xn", c)], w=[("ps", b)])
            epilogue(gi, tb, gw, b)


def headnorm(C, b, gname, scale, out_ap, out_key):
    S, TT = C.S, C.TT
    bank = C.banks[b]
    g = C.gv[gname]
    S.add("act", lambda e: e.activation(out=C.kf[:], in_=bank[:, :TT], func=AF.Copy, scale=float(scale)),
          r=[("ps", b)], w=[("kf",)])
    S.add("act", lambda e: e.activation(out=C.sqh[:], in_=bank[:, :TT], func=AF.Square), r=[("ps", b)], w=[("sqh",)])
    S.add("pe", lambda e: e.matmul(C.banks[1][:, :TT], C.ones[:], C.sqh[:], start=True, stop=True),
          r=[("sqh",), ("ones",)], w=[("ps", 1)])
    S.add("dve", lambda e: e.tensor_scalar(C.hr[:], C.banks[1][:, :TT], 1.0 / 128, EPS, ALU.mult, ALU.add),
          r=[("ps", 1)], w=[("hr",)])
    S.add("act", lambda e: e.activation(out=C.hr2[:], in_=C.hr[:], func=AF.Sqrt), r=[("hr",)], w=[("hr2",)])
    S.add("dve", lambda e: e.reciprocal(C.hr[:], C.hr2[:]), r=[("hr2",)], w=[("hr",)])
    S.add("dve", lambda e: e.scalar_tensor_tensor(out_ap, C.kf[:], g[:, 0:1], C.hr[:], ALU.mult, ALU.mult),
          r=[("kf",), ("hr",), ("v", gname)], w=[out_key])


def xn_key(c):
    return ("xn", c)


def emit_ple(C, xin, xout, pT, T, gname, wpe, wpg):
    S, TT = C.S, C.TT
    ctx_extra(C)
    if not hasattr(C, "pt"):
        C.pt = S.sb("pt", [128, 2, TT], BF16)
        C.wpe = [S.sb("wpe%d" % i, [128, 2, 512], BF16) for i in range(2)]
        C.pectr = 0
    wpg_r = wpg.rearrange("(c p) n -> p c n", p=128)
    wpe_r = wpe.rearrange("(c p) n -> p c n", p=128)
    pT_r = pT.rearrange("(c p) t -> p c t", p=128)
    xo_r = xout.rearrange("(c p) t -> p c t", p=128)
    for t0 in range(0, T, TT):
        emit_norm(C, xin, t0, gname)
        S.add("pool", lambda e, t0=t0: e.dma_start(out=C.pt[:], in_=pT_r[:, :, t0:t0 + TT]), w=[("pt",)], slot="pt")
        for ng in range(4):
            sl = C.pectr % 2
            C.pectr += 1
            wpet = C.wpe[sl]
            S.add("pool", lambda e, wpet=wpet, ng=ng: e.dma_start(out=wpet[:], in_=wpe_r[:, :, ng * 512:(ng + 1) * 512]),
                  w=[("wpe", sl)], slot="wpe%d" % sl)

            def epi(gi, j, nj, m, b, ng=ng, wpet=wpet, sl=sl):
                c = ng * 4 + j
                pb = 2 + (c % 2)
                for cc in range(2):
                    S.add("pe", lambda e, cc=cc: e.matmul(C.banks[pb][:, :TT], wpet[:, cc, j * 128:(j + 1) * 128], C.pt[:, cc, :],
                                                          start=(cc == 0), stop=(cc == 1)),
                          r=[("wpe", sl), ("pt",)], w=[("ps", pb)])
                sg = C.sg[c % 2]
                S.add("act", lambda e: e.activation(out=sg[:], in_=C.banks[b][:, :TT], func=AF.Sigmoid),
                      r=[("ps", b)], w=[("sg", c % 2)])
                S.add("dve", lambda e: e.tensor_tensor(sg[:], C.banks[pb][:, :TT], sg[:], ALU.mult),
                      r=[("ps", pb), ("sg", c % 2)], w=[("sg", c % 2)])
                S.add("dve", lambda e: e.tensor_tensor(C.xt[:, c, :], C.xt[:, c, :], sg[:], ALU.add),
                      r=[("sg", c % 2), ("xt",)], w=[("xt",)])
            fm_linear(C, wpg_r, ng * 512, 512, KC, C.xn, xn_key, epi)
        S.add("sp", lambda e, t0=t0: e.dma_start(out=xo_r[:, :, t0:t0 + TT], in_=C.xt[:]),
              r=[("xt",)], w=[("dram", "xout")], slot="xt_st")


def emit_proj_res(C, xin, xout, aT, bT, T, W, gth=None):
    S, TT = C.S, C.TT
    ctx_extra(C)
    W_r = W.rearrange("(c p) n -> p c n", p=128)
    x_r = xin.rearrange("(c p) t -> p c t", p=128)
    a_r = aT.rearrange("(c p) t -> p c t", p=128) if gth is None else None
    b_r = bT.rearrange("(c p) t -> p c t", p=128) if bT is not None else None
    xo_r = xout.rearrange("(c p) t -> p c t", p=128)
    for t0 in range(0, T, TT):
        S.add("sp", lambda e, t0=t0: e.dma_start(out=C.xt[:], in_=x_r[:, :, t0:t0 + TT]), w=[("xt",)], slot="xt")
        for g4 in range(4):
            ai = C.stctr["f"] % NSTF
            C.stctr["f"] += 1
            if gth is None:
                S.add("sp", lambda e, t0=t0, g4=g4, ai=ai: e.dma_start(out=C.stf[ai][:, :, :TT], in_=a_r[:, g4 * 4:(g4 + 1) * 4, t0:t0 + TT]),
                      w=[("stf", ai)], slot="stf%d" % ai)
            else:
                view, ixt, ixkey, cb, agk = gth
                for cc in range(4):
                    col = cb + (g4 * 4 + cc) * 4 + t0 // TT
                    S.add("pool", lambda e, cc=cc, ai=ai, col=col: e.indirect_dma_start(
                        out=C.stf[ai][:, cc, :TT], out_offset=None, in_=view,
                        in_offset=bass.IndirectOffsetOnAxis(ap=ixt[:, col:col + 1], axis=0)),
                        r=[ixkey] + list(agk), w=[("stf", ai)], slot="stf%d" % ai, group=True)
            if b_r is not None:
                bi_ = C.stctr["f"] % NSTF
                C.stctr["f"] += 1
                S.add("sp", lambda e, t0=t0, g4=g4, bi_=bi_: e.dma_start(out=C.stf[bi_][:, :, :TT], in_=b_r[:, g4 * 4:(g4 + 1) * 4, t0:t0 + TT]),
                      w=[("stf", bi_)], slot="stf%d" % bi_)
                for cc in range(4):
                    c = g4 * 4 + cc
                    S.add("dve", lambda e, c=c, cc=cc, ai=ai, bi_=bi_: e.tensor_tensor(C.xn[:, c, :], C.stf[ai][:, cc, :TT], C.stf[bi_][:, cc, :TT], ALU.mult),
                          r=[("stf", ai), ("stf", bi_)], w=[("xn", c)])
            else:
                for cc in range(4):
                    c = g4 * 4 + cc
                    if cc % 2 == 0:
                        S.add("dve", lambda e, c=c, cc=cc, ai=ai: e.tensor_copy(C.xn[:, c, :], C.stf[ai][:, cc, :TT]), r=[("stf", ai)], w=[("xn", c)])
                    else:
                        S.add("act", lambda e, c=c, cc=cc, ai=ai: e.activation(out=C.xn[:, c, :], in_=C.stf[ai][:, cc, :TT], func=AF.Copy), r=[("stf", ai)], w=[("xn", c)])

        def epi(gi, j, nj, m, b):
            c = gi * 4 + j
            S.add("dve", lambda e: e.tensor_tensor(C.xt[:, c, :], C.banks[b][:, :TT], C.xt[:, c, :], ALU.add),
                  r=[("ps", b), ("xt",)], w=[("xt",)])
        fm_linear(C, W_r, 0, D, KC, C.xn, xn_key, epi)
        S.add("sp", lambda e, t0=t0: e.dma_start(out=xo_r[:, :, t0:t0 + TT], in_=C.xt[:]),
              r=[("xt",)], w=[("dram", "xout")], slot="xt_st")


def emit_mlstm_in(C, xin, T, gname, w_in, qT, kT, k_tm, v_tm, sog, gates, agf=None):
    S, TT = C.S, C.TT
    pending = []
    tkeys = {}
    ctx_extra(C)
    W_r = w_in.rearrange("(c p) n -> p c n", p=128)
    qT_r = qT.rearrange("(c p) t -> p c t", p=128)
    kT_r = kT.rearrange("(c p) t -> p c t", p=128)
    sog_r = sog.rearrange("(c p) t -> p c t", p=128)
    ktm_r = k_tm.rearrange("(b p) n -> p b n", p=128)
    vtm_r = v_tm.rearrange("(b p) n -> p b n", p=128)
    if not hasattr(C, "gst"):
        C.gst = S.sb("gst", [8, TT], F32)
    for t0 in range(0, T, TT):
        emit_norm(C, xin, t0, gname)
        cur = {}

        def fm_epi(kind, dst_r, scale, func):
            def epi(gi, j, nj, m, b):
                if j == 0:
                    cur["st"] = next_stage(C, kind)
                st, skey, sslot = cur["st"]
                S.add("act", lambda e: e.activation(out=st[:, j, :TT], in_=C.banks[b][:, :TT], func=func, scale=float(scale)),
                      r=[("ps", b)], w=[skey])
                if j == nj - 1:
                    S.add("sp", lambda e, t0=t0: e.dma_start(out=dst_r[:, gi * 4:gi * 4 + nj, t0:t0 + TT], in_=st[:, :nj, :TT]),
                          r=[skey], w=[("dram", "o", S.uid())], slot=sslot)
            return epi
        fm_linear(C, W_r, 0, 1024, KC, C.xn, xn_key, fm_epi("b", qT_r, 256 ** -0.5, AF.Copy))
        prev_pending = list(pending)
        del pending[:]
        fm_linear(C, W_r, 1024, 1024, KC, C.xn, xn_key, fm_epi("b", kT_r, 1.0, AF.Copy))
        fm_linear(C, W_r, 4096, 2048, KC, C.xn, xn_key, fm_epi("f", sog_r, 1.0, AF.Sigmoid))

        def g_epi(gi, j, nj, m, b):
            S.add("act", lambda e: e.activation(out=C.gst[:, :], in_=C.banks[b][:8, :TT], func=AF.Copy),
                  r=[("ps", b)], w=[("gst",)])
            S.add("sp", lambda e, t0=t0: e.dma_start(out=gates[:, t0:t0 + TT], in_=C.gst[:, :]), r=[("gst",)], w=[("dram", "o", S.uid())], slot="gst")
        fm_linear(C, W_r, 6144, 8, KC, C.xn, xn_key, g_epi)

        def tm_epi(dst_r, name):
            def epi(gi, tb, gw, b):
                if tb == 0:
                    cur["st"] = next_stage(C, "b")
                st, skey, sslot = cur["st"]
                eng = "dve" if tb % 2 == 0 else "act"
                if eng == "dve":
                    S.add("dve", lambda e: e.tensor_copy(st[:, tb, :gw], C.banks[b][:, :gw]), r=[("ps", b)], w=[skey])
                else:
                    S.add("act", lambda e: e.activation(out=st[:, tb, :gw], in_=C.banks[b][:, :gw], func=AF.Copy), r=[("ps", b)], w=[skey])
                if tb == TT // 128 - 1:
                    tb0 = t0 // 128
                    dkey = ("dram", "o", S.uid())
                    tkeys.setdefault((name, t0), []).append(dkey)
                    S.add("sp", lambda e: e.dma_start(out=dst_r[:, tb0:tb0 + TT // 128, gi * 512:gi * 512 + gw], in_=st[:, :TT // 128, :gw]),
                          r=[skey], w=[dkey], slot=sslot)
            return epi
        tm_linear(C, W_r, 1024, 1024, tm_epi(ktm_r, "ktm"))
        tm_linear(C, W_r, 2048, 2048, tm_epi(vtm_r, "vtm"))
        for fn in prev_pending:
            fn()
        if agf is not None:
            ti = t0 // TT
            pending.append(lambda ti=ti, t0=t0: agf(S, "ktm", 512, [ti], tkeys[("ktm", t0)]))
            pending.append(lambda ti=ti, t0=t0: agf(S, "vtm", 256, [2 * ti, 2 * ti + 1], tkeys[("vtm", t0)]))
    for fn in pending:
        fn()


def emit_headproj(C, xin, T, gname, W, c0, hgname, scale, outT):
    S, TT = C.S, C.TT
    ctx_extra(C)
    W_r = W.rearrange("(c p) n -> p c n", p=128)
    o_r = outT.rearrange("(c p) t -> p c t", p=128)
    cur = {}
    for t0 in range(0, T, TT):
        emit_norm(C, xin, t0, gname)

        def epi(gi, j, nj, m, b):
            if j == 0:
                cur["st"] = next_stage(C, "b")
            st, skey, sslot = cur["st"]
            headnorm(C, b, hgname, scale, st[:, j, :TT], skey)
            if j == nj - 1:
                S.add("sp", lambda e, t0=t0: e.dma_start(out=o_r[:, gi * 4:gi * 4 + nj, t0:t0 + TT], in_=st[:, :nj, :TT]),
                      r=[skey], w=[("dram", "o", S.uid())], slot=sslot)
        fm_linear(C, W_r, c0, 2048, KC, C.xn, xn_key, epi)
        yield t0


def emit_kv(C, xin, T, gname, w_kv, kT, v_tm, agf=None):
    S, TT = C.S, C.TT
    ctx_extra(C)
    W_r = w_kv.rearrange("(c p) n -> p c n", p=128)
    vtm_r = v_tm.rearrange("(b p) n -> p b n", p=128)
    cur = {}
    tkeys = {}
    pend = []
    for t0 in emit_headproj(C, xin, T, gname, w_kv, 0, "g_k", 1.0, kT):
        prev_pend = list(pend)
        del pend[:]
        def epi(gi, tb, gw, b):
            if tb == 0:
                cur["st"] = next_stage(C, "b")
            st, skey, sslot = cur["st"]
            if tb % 2 == 0:
                S.add("dve", lambda e: e.tensor_copy(st[:, tb, :gw], C.banks[b][:, :gw]), r=[("ps", b)], w=[skey])
            else:
                S.add("act", lambda e: e.activation(out=st[:, tb, :gw], in_=C.banks[b][:, :gw], func=AF.Copy), r=[("ps", b)], w=[skey])
            if tb == TT // 128 - 1:
                tb0 = t0 // 128
                dkey = ("dram", "o", S.uid())
                tkeys.setdefault(t0, []).append(dkey)
                S.add("sp", lambda e: e.dma_start(out=vtm_r[:, tb0:tb0 + TT // 128, gi * 512:gi * 512 + gw], in_=st[:, :TT // 128, :gw]),
                      r=[skey], w=[dkey], slot=sslot)
        tm_linear(C, W_r, 2048, 2048, epi)
        for fn in prev_pend:
            fn()
        if agf is not None:
            ti = t0 // TT
            pend.append(lambda ti=ti, t0=t0: agf(S, "v", 256, [2 * ti, 2 * ti + 1], tkeys[t0]))
    for fn in pend:
        fn()


def emit_mlstm_core(S, SEQ, qT, kT, k_tm, v_tm, gi, gf, bif, ghead, hout, SEG=1024, dbg=None, gth=None, out_ag=None):
    nc = S.nc
    CH = 128
    NCH = SEG // CH
    DK, DV = 256, 512
    sb, ps = S.sb, S.ps
    banks = [ps("bank%d" % i, [128, 512], F32) for i in range(8)]
    ones_bf = sb("ones_bf", [128, 128], BF16)
    ones_f = sb("ones_f", [128, 128], F32)
    tri = sb("tri", [128, 128], F32)
    S.add("dve", lambda e: e.memset(ones_bf[:], 1.0), w=["ones_bf"])
    S.add("dve", lambda e: e.memset(ones_f[:], 1.0), w=["ones_f"])
    S.add("pool", lambda e: e.memset(tri[:], 1.0), w=["tri"])
    S.add("pool", lambda e: e.affine_select(out=tri[:], in_=tri[:], pattern=[[1, 128]], compare_op=ALU.is_ge, fill=0.0,
                                            base=0, channel_multiplier=-1), r=["tri"], w=["tri"])
    bt = sb("bif_sb", [1, 2], F32)
    S.add("sp", lambda e: e.dma_start(out=bt[:], in_=bif), w=["bif"], slot="bif")
    nbf = sb("nbf", [1, 1], F32)
    S.add("dve", lambda e: e.tensor_scalar(nbf[:], bt[0:1, 1:2], -1.0, None, ALU.mult), r=["bif"], w=["nbf"])
    gh = sb("gh_sb", [128, 4], F32)
    S.add("sp", lambda e: e.dma_start(out=gh[:], in_=ghead), w=["gh"], slot="gh")
    rows = {n: sb("r_" + n, [1, SEG], F32) for n in ("gi", "gf", "e", "lf", "Bn", "U", "G", "nG", "wi", "cl", "we", "one")}
    S.add("dve", lambda e: e.memset(rows["one"][:], 1.0), w=["r_one"])
    carry = sb("carry", [1, 4], F32)
    S.add("dve", lambda e: e.memset(carry[:], 0.0), w=["carry"])
    bc = {n: sb("bc_" + n, [128, SEG], F32) for n in ("nG", "wi", "cl")}
    cols = sb("cols", [128, 3 * NCH], F32)
    qt = [sb("qt%d" % i, [128, 2, SEG], BF16) for i in range(2)]
    kt = [sb("kt%d" % i, [128, 2, SEG], BF16) for i in range(2)]
    ktm = [sb("ktm%d" % i, [128, NCH, DK], BF16) for i in range(2)]
    vtm = [sb("vtm%d" % i, [128, NCH, DV], BF16) for i in range(2)]
    hst = [sb("hst%d" % i, [128, 4, SEG], F32) for i in range(2)]
    PT = sb("PT", [128, 128], F32)
    PTm = sb("PTm", [128, 128], F32)
    AT = sb("AT", [128, 128], BF16)
    qw = sb("qw", [128, 2, 128], BF16)
    dd = sb("dd", [128, 128], F32)
    rd = sb("rd", [128, 128], F32)
    hT = sb("hT", [128, 4, 128], F32)
    hsq = sb("hsq", [128, 4, 128], BF16)
    hr = sb("hr", [128, 128], F32)
    hr2 = sb("hr2", [128, 128], F32)
    kw = sb("kw", [128, DK], BF16)
    Cf = sb("Cf", [128, 2, DV], F32)
    Cb = sb("Cb", [128, 2, DV], BF16)
    nf = sb("nf", [128, 2], F32)
    nrep = sb("nrep", [128, 2, 128], BF16)
    S.add("dve", lambda e: e.memset(Cf[:], 0.0), w=["Cf"])
    S.add("dve", lambda e: e.memset(nf[:], 0.0), w=["nf"])
    S.add("pool", lambda e: e.memset(Cb[:], 0.0), w=["Cb"])
    S.add("pool", lambda e: e.memset(nrep[:], 0.0), w=["nrep"])
    ho_r = hout.rearrange("(c p) t -> p c t", p=128) if out_ag is None else None
    R = rows
    ag_pending = []
    if gth is None:
        qT_r = qT.rearrange("(c p) t -> p c t", p=128)
        kT_r = kT.rearrange("(c p) t -> p c t", p=128)
        ktm_r = k_tm.rearrange("(b p) n -> p b n", p=128)
        vtm_r = v_tm.rearrange("(b p) n -> p b n", p=128)
    else:
        g8 = sb("g8", [1, 8, SEG], F32)
        oh = sb("oh", [1, 4], F32)
        S.add("sp", lambda e: e.dma_start(out=oh[:], in_=gth["onehot"]), w=["oh"], slot="oh")
    for sg in range(SEQ // SEG):
        t0 = sg * SEG
        sl = sg % 2
        if gth is None:
            for dk in range(2):
                S.add("sp", lambda e, t0=t0, sl=sl, dk=dk: e.dma_start(out=qt[sl][:, dk, :], in_=qT_r[:, dk, t0:t0 + SEG]), w=[("qt", sl, dk)], slot="qt%d_%d" % (sl, dk))
                S.add("sp", lambda e, t0=t0, sl=sl, dk=dk: e.dma_start(out=kt[sl][:, dk, :], in_=kT_r[:, dk, t0:t0 + SEG]), w=[("kt", sl, dk)], slot="kt%d_%d" % (sl, dk))
            for c in range(NCH):
                S.add("sp", lambda e, t0=t0, sl=sl, c=c: e.dma_start(out=ktm[sl][:, c, :], in_=ktm_r[:, t0 // 128 + c, :]), w=[("ktm", sl, c)], slot="ktm%d_%d" % (sl, c))
                S.add("sp", lambda e, t0=t0, sl=sl, c=c: e.dma_start(out=vtm[sl][:, c, :], in_=vtm_r[:, t0 // 128 + c, :]), w=[("vtm", sl, c)], slot="vtm%d_%d" % (sl, c))
            S.add("sp", lambda e, t0=t0: e.dma_start(out=R["gi"][:], in_=gi[:, t0:t0 + SEG]), w=["r_gi"], slot="r_gi")
            S.add("sp", lambda e, t0=t0: e.dma_start(out=R["gf"][:], in_=gf[:, t0:t0 + SEG]), w=["r_gf"], slot="r_gf")
        else:
            ixt, ixkey = gth["ixt"], gth["ixkey"]
            for dk in range(2):
                col = gth["MQ"] + sg * 2 + dk
                S.add("pool", lambda e, sl=sl, dk=dk, col=col: e.indirect_dma_start(
                    out=qt[sl][:, dk, :], out_offset=None, in_=gth["q_view"],
                    in_offset=bass.IndirectOffsetOnAxis(ap=ixt[:, col:col + 1], axis=0)),
                    r=[ixkey] + list(gth["agk"]["q"]), w=[("qt", sl, dk)], slot="qt%d_%d" % (sl, dk))
                S.add("pool", lambda e, sl=sl, dk=dk, col=col: e.indirect_dma_start(
                    out=kt[sl][:, dk, :], out_offset=None, in_=gth["k_view"],
                    in_offset=bass.IndirectOffsetOnAxis(ap=ixt[:, col:col + 1], axis=0)),
                    r=[ixkey] + list(gth["agk"]["k"]), w=[("kt", sl, dk)], slot="kt%d_%d" % (sl, dk))
            for c in range(NCH):
                col = gth["MK"] + sg * NCH + c
                colv = gth["MV"] + sg * NCH + c
                S.add("pool", lambda e, sl=sl, c=c, col=col: e.indirect_dma_start(
                    out=ktm[sl][:, c, :], out_offset=None, in_=gth["ktm_view"],
                    in_offset=bass.IndirectOffsetOnAxis(ap=ixt[:, col:col + 1], axis=0)),
                    r=[ixkey] + list(gth["agk"]["ktm"]), w=[("ktm", sl, c)], slot="ktm%d_%d" % (sl, c))
                S.add("pool", lambda e, sl=sl, c=c, colv=colv: e.indirect_dma_start(
                    out=vtm[sl][:, c, :], out_offset=None, in_=gth["vtm_view"],
                    in_offset=bass.IndirectOffsetOnAxis(ap=ixt[:, colv:colv + 1], axis=0)),
                    r=[ixkey] + list(gth["agk"]["vtm"]), w=[("vtm", sl, c)], slot="vtm%d_%d" % (sl, c))
            srank, half = sg // 2, sg % 2
            gsrc = gth["gates_all"][srank * 8:(srank + 1) * 8, half * SEG:(half + 1) * SEG].rearrange("(o r) t -> o r t", o=1)
            S.add("sp", lambda e, gsrc=gsrc: e.dma_start(out=g8[:], in_=gsrc), r=list(gth["agk"]["g"]), w=["g8"], slot="g8")
            for gi_, (dst, off) in enumerate((("gi", 0), ("gf", 4))):
                S.add("dve", lambda e, dst=dst, off=off: e.tensor_scalar(R[dst][:], g8[0:1, off, :], oh[0:1, 0:1], None, ALU.mult),
                      r=["g8", "oh"], w=["r_" + dst])
                for j in range(1, 4):
                    S.add("dve", lambda e, dst=dst, off=off, j=j: e.scalar_tensor_tensor(R[dst][:], g8[0:1, off + j, :], oh[0:1, j:j + 1], R[dst][:], ALU.mult, ALU.add),
                          r=["g8", "oh", "r_" + dst], w=["r_" + dst])
        for fn in ag_pending:
            fn()
        del ag_pending[:]
        S.add("act", lambda e: e.activation(out=R["e"][:], in_=R["gf"][:], func=AF.Exp, scale=-1.0, bias=nbf[0:1, 0:1]),
              r=["r_gf", "nbf"], w=["r_e"])
        S.add("act", lambda e: e.activation(out=R["lf"][:], in_=R["e"][:], func=AF.Ln, bias=ones_f[0:1, 0:1]),
              r=["r_e", "ones_f"], w=["r_lf"])
        S.add("dve", lambda e: e.tensor_tensor_scan(R["Bn"][:], R["one"][:], R["lf"][:], carry[0:1, 0:1], ALU.mult, ALU.add),
              r=["r_lf", "carry", "r_one"], w=["r_Bn"])
        S.add("dve", lambda e: e.scalar_tensor_tensor(R["U"][:], R["gi"][:], bt[0:1, 0:1], R["Bn"][:], ALU.add, ALU.add),
              r=["r_gi", "bif", "r_Bn"], w=["r_U"])
        S.add("dve", lambda e: e.tensor_tensor_scan(R["G"][:], R["U"][:], R["U"][:], carry[0:1, 1:2], ALU.max, ALU.max),
              r=["r_U", "carry"], w=["r_G"])
        S.add("dve", lambda e: e.tensor_scalar(R["nG"][:], R["G"][:], -1.0, None, ALU.mult), r=["r_G"], w=["r_nG"])
        S.add("dve", lambda e: e.tensor_tensor(R["cl"][:], R["Bn"][:], R["G"][:], ALU.subtract), r=["r_Bn", "r_G"], w=["r_cl"])
        S.add("act", lambda e: e.activation(out=R["cl"][:], in_=R["cl"][:], func=AF.Exp), r=["r_cl"], w=["r_cl"])
        for c in range(NCH):
            a, b_ = c * CH, (c + 1) * CH
            gprev = carry[0:1, 1:2] if c == 0 else R["G"][0:1, a - 1:a]
            S.add("act", lambda e, a=a, b_=b_, gprev=gprev: e.activation(out=R["wi"][0:1, a:b_], in_=R["nG"][0:1, a:b_], func=AF.Exp, bias=gprev),
                  r=["r_nG", "r_G", "carry"], w=["r_wi"])
            S.add("act", lambda e, a=a, b_=b_: e.activation(out=R["we"][0:1, a:b_], in_=R["U"][0:1, a:b_], func=AF.Exp, bias=R["nG"][0:1, b_ - 1:b_]),
                  r=["r_nG", "r_U"], w=["r_we"])
        for n in ("nG", "wi", "cl"):
            for h in range(SEG // 512):
                S.add("pe", lambda e, n=n, h=h: e.matmul(banks[7][:, :], ones_f[0:1, :], R[n][0:1, h * 512:(h + 1) * 512], start=True, stop=True),
                      r=["r_" + n, "ones_f"], w=[("ps", 7)])
                S.add("act", lambda e, n=n, h=h: e.activation(out=bc[n][:, h * 512:(h + 1) * 512], in_=banks[7][:, :], func=AF.Copy),
                      r=[("ps", 7)], w=["bc_" + n])
        for c in range(NCH):
            a, b_ = c * CH, (c + 1) * CH
            S.add("pe", lambda e, c=c, a=a, b_=b_: e.matmul(banks[7][:, c:c + 1], R["U"][0:1, a:b_], ones_f[0:1, 0:1], start=True, stop=True),
                  r=["r_U", "ones_f"], w=[("ps", 7)])
            S.add("pe", lambda e, c=c, a=a, b_=b_: e.matmul(banks[7][:, NCH + c:NCH + c + 1], R["we"][0:1, a:b_], ones_f[0:1, 0:1], start=True, stop=True),
                  r=["r_we", "ones_f"], w=[("ps", 7)])
            S.add("pe", lambda e, c=c, b_=b_: e.matmul(banks[7][:, 2 * NCH + c:2 * NCH + c + 1], ones_f[0:1, :], R["wi"][0:1, b_ - 1:b_], start=True, stop=True),
                  r=["r_wi", "ones_f"], w=[("ps", 7)])
        S.add("dve", lambda e: e.tensor_copy(cols[:], banks[7][:, :3 * NCH]), r=[("ps", 7)], w=["cols"])
        S.add("dve", lambda e: e.tensor_copy(carry[0:1, 0:1], R["Bn"][0:1, SEG - 1:SEG]), r=["r_Bn", "r_wi", "r_we"], w=["carry"])
        S.add("dve", lambda e: e.tensor_copy(carry[0:1, 1:2], R["G"][0:1, SEG - 1:SEG]), r=["r_G", "r_wi", "r_we"], w=["carry"])
        if dbg is not None and sg == 0:
            S.add("sp", lambda e: e.dma_start(out=dbg["nG"], in_=bc["nG"][:]), r=["bc_nG"], slot="dbg0")
            S.add("sp", lambda e: e.dma_start(out=dbg["wi"], in_=bc["wi"][:]), r=["bc_wi"], slot="dbg1")
            S.add("sp", lambda e: e.dma_start(out=dbg["cl"], in_=bc["cl"][:]), r=["bc_cl"], slot="dbg2")
            S.add("sp", lambda e: e.dma_start(out=dbg["cols"], in_=cols[:]), r=["cols"], slot="dbg3")
            S.add("sp", lambda e: e.dma_start(out=dbg["tri"], in_=tri[:]), r=["tri"], slot="dbg4")
        for c in range(NCH):
            a, b_ = c * CH, (c + 1) * CH
            qs, ks, kms, vs = qt[sl], kt[sl], ktm[sl], vtm[sl]
            for dk in range(2):
                S.add("pe", lambda e, dk=dk, a=a, b_=b_, ks=ks, qs=qs: e.matmul(banks[0][:, :128], ks[:, dk, a:b_], qs[:, dk, a:b_], start=(dk == 0), stop=(dk == 1)),
                      r=[("kt", sl, dk), ("qt", sl, dk)], w=[("ps", 0)])
            S.add("act", lambda e, a=a, b_=b_, c=c: e.activation(out=PT[:], in_=bc["nG"][:, a:b_], func=AF.Exp, bias=cols[:, c:c + 1]),
                  r=["bc_nG", "cols"], w=["PT"])
            S.add(PENG, lambda e: e.tensor_tensor(PTm[:], PT[:], tri[:], ALU.mult), r=["PT", "tri"], w=["PTm"])
            S.add("dve", lambda e: e.tensor_tensor(AT[:], banks[0][:, :128], PTm[:], ALU.mult), r=[("ps", 0), "PTm"], w=["AT"])
            for dk in range(2):
                S.add("dve", lambda e, dk=dk, a=a, b_=b_, qs=qs: e.tensor_tensor(qw[:, dk, :], qs[:, dk, a:b_], bc["wi"][:, a:b_], ALU.mult),
                      r=[("qt", sl, dk), "bc_wi"], w=["qw"])
            for j in range(4):
                S.add("pe", lambda e, j=j, c=c, vs=vs: e.matmul(banks[1][:, j * 128:(j + 1) * 128], vs[:, c, j * 128:(j + 1) * 128], AT[:], start=True, stop=False),
                      r=[("vtm", sl, c), "AT"], w=[("ps", 1)])
                for dk in range(2):
                    S.add("pe", lambda e, j=j, dk=dk: e.matmul(banks[1][:, j * 128:(j + 1) * 128], Cb[:, dk, j * 128:(j + 1) * 128], qw[:, dk, :], start=False, stop=(dk == 1)),
                          r=["Cb", "qw"], w=[("ps", 1)])
            S.add("pe", lambda e: e.matmul(banks[2][:, :128], ones_bf[:], AT[:], start=True, stop=False), r=["ones_bf", "AT"], w=[("ps", 2)])
            for dk in range(2):
                S.add("pe", lambda e, dk=dk: e.matmul(banks[2][:, :128], nrep[:, dk, :], qw[:, dk, :], start=False, stop=(dk == 1)),
                      r=["nrep", "qw"], w=[("ps", 2)])
            S.add("act", lambda e: e.activation(out=dd[:], in_=banks[2][:, :128], func=AF.Abs), r=[("ps", 2)], w=["dd"])
            S.add("dve", lambda e, a=a, b_=b_: e.tensor_tensor(dd[:], dd[:], bc["cl"][:, a:b_], ALU.max), r=["dd", "bc_cl"], w=["dd"])
            S.add("dve", lambda e: e.reciprocal(rd[:], dd[:]), r=["dd"], w=["rd"])
            for j in range(4):
                S.add("dve", lambda e, j=j: e.tensor_tensor(hT[:, j, :], banks[1][:, j * 128:(j + 1) * 128], rd[:], ALU.mult),
                      r=[("ps", 1), "rd"], w=["hT"])
            S.add("act", lambda e: e.activation(out=hsq[:], in_=hT[:], func=AF.Square), r=["hT"], w=["hsq"])
            for j in range(4):
                S.add("pe", lambda e, j=j: e.matmul(banks[3][:, :128], ones_bf[:], hsq[:, j, :], start=(j == 0), stop=(j == 3)),
                      r=["ones_bf", "hsq"], w=[("ps", 3)])
            S.add("dve", lambda e: e.tensor_scalar(hr[:], banks[3][:, :128], 1.0 / DV, EPS, ALU.mult, ALU.add), r=[("ps", 3)], w=["hr"])
            S.add("act", lambda e: e.activation(out=hr2[:], in_=hr[:], func=AF.Sqrt), r=["hr"], w=["hr2"])
            S.add("dve", lambda e: e.reciprocal(hr[:], hr2[:]), r=["hr2"], w=["hr"])
            for j in range(4):
                S.add("dve", lambda e, j=j, a=a, b_=b_, sl=sl: e.scalar_tensor_tensor(hst[sl][:, j, a:b_], hT[:, j, :], gh[:, j:j + 1], hr[:], ALU.mult, ALU.mult),
                      r=["hT", "hr", "gh"], w=[("hst", sl)])
            S.add("dve", lambda e, c=c, kms=kms: e.tensor_scalar(kw[:], kms[:, c, :], cols[:, NCH + c:NCH + c + 1], None, ALU.mult),
                  r=[("ktm", sl, c), "cols"], w=["kw"])
            for dk in range(2):
                S.add("pe", lambda e, dk=dk, c=c, vs=vs: e.matmul(banks[4 + dk][:, :], kw[:, dk * 128:(dk + 1) * 128], vs[:, c, :], start=True, stop=True),
                      r=["kw", ("vtm", sl, c)], w=[("ps", 4 + dk)])
                S.add("pe", lambda e, dk=dk: e.matmul(banks[6][:, dk:dk + 1], kw[:, dk * 128:(dk + 1) * 128], ones_bf[:, 0:1], start=True, stop=True),
                      r=["kw", "ones_bf"], w=[("ps", 6)])
            for dk in range(2):
                S.add("dve", lambda e, dk=dk, c=c: e.scalar_tensor_tensor(Cf[:, dk, :], Cf[:, dk, :], cols[:, 2 * NCH + c:2 * NCH + c + 1], banks[4 + dk][:, :], ALU.mult, ALU.add),
                      r=["Cf", "cols", ("ps", 4 + dk), "Cb"], w=["Cf"])
            S.add("dve", lambda e, c=c: e.scalar_tensor_tensor(nf[:], nf[:], cols[:, 2 * NCH + c:2 * NCH + c + 1], banks[6][:, 0:2], ALU.mult, ALU.add),
                  r=["nf", "cols", ("ps", 6)], w=["nf"])
            S.add("act", lambda e: e.activation(out=Cb[:], in_=Cf[:], func=AF.Copy), r=["Cf"], w=["Cb"])
            for dk in range(2):
                S.add(PENG, lambda e, dk=dk: e.tensor_scalar(nrep[:, dk, :], ones_f[:], nf[:, dk:dk + 1], None, ALU.mult),
                      r=["nf", "ones_f"], w=["nrep"])
        if out_ag is None:
            S.add("sp", lambda e, t0=t0, sl=sl: e.dma_start(out=ho_r[:, :, t0:t0 + SEG], in_=hst[sl][:]), r=[("hst", sl)], w=[("dram", "ho", sg)], slot="hst%d" % sl)
        else:
            dst = out_ag["h_b"][sg * 512:(sg + 1) * 512, :].rearrange("(c p) t -> p c t", p=128)
            S.add("sp", lambda e, dst=dst, sl=sl: e.dma_start(out=dst, in_=hst[sl][:]), r=[("hst", sl)], w=[("dram", "ho", sg)], slot="hst%d" % sl)
            ag_pending.append(lambda sg=sg: out_ag["agf"](S, "a", 256, [2 * sg, 2 * sg + 1], [("dram", "ho", sg)]))
    for fn in ag_pending:
        fn()


def build_mlstm_core(SEQ, debug=False):
    nc = bass.Bass("TRN2", target_bir_lowering=False)
    qT = nc.dram_tensor("qT", [256, SEQ], BF16, kind="ExternalInput").ap()
    kT = nc.dram_tensor("kT", [256, SEQ], BF16, kind="ExternalInput").ap()
    k_tm = nc.dram_tensor("k_tm", [SEQ, 256], BF16, kind="ExternalInput").ap()
    v_tm = nc.dram_tensor("v_tm", [SEQ, 512], BF16, kind="ExternalInput").ap()
    gi = nc.dram_tensor("gi", [1, SEQ], F32, kind="ExternalInput").ap()
    gf = nc.dram_tensor("gf", [1, SEQ], F32, kind="ExternalInput").ap()
    bif = nc.dram_tensor("bif", [1, 2], F32, kind="ExternalInput").ap()
    ghead = nc.dram_tensor("ghead", [128, 4], F32, kind="ExternalInput").ap()
    hout = nc.dram_tensor("hout", [512, SEQ], F32, kind="ExternalOutput").ap()
    S = Sched(nc)
    dbg = None
    if debug:
        dbg = {n: nc.dram_tensor("dbg_" + n, [128, 1024], F32, kind="ExternalOutput").ap() for n in ("nG", "wi", "cl")}
        dbg["cols"] = nc.dram_tensor("dbg_cols", [128, 24], F32, kind="ExternalOutput").ap()
        dbg["tri"] = nc.dram_tensor("dbg_tri", [128, 128], F32, kind="ExternalOutput").ap()
    emit_mlstm_core(S, SEQ, qT, kT, k_tm, v_tm, gi, gf, bif, ghead, hout, dbg=dbg)
    S.emit()
    return nc, S


def emit_sb_core(S, NH, SEQ, qT, kT, v_tm, oT, gth=None, out_ag=None):
    sb, ps = S.sb, S.ps
    NB = SEQ // 128
    NQB = SEQ // 512
    zb = [ps("zb%d" % i, [128, 512], F32) for i in range(2)]
    ab = [ps("ab%d" % i, [128, 512], F32) for i in range(2)]
    ob = [ps("ob%d" % i, [128, 512], F32) for i in range(2)]
    ones_f = sb("ones_f", [128, 128], F32)
    ntri = sb("ntri", [128, 128], BF16)
    nones = sb("nones", [128, 128], BF16)
    smask = sb("smask", [128, 128], BF16)
    tmpf = sb("tmpf", [128, 128], F32)
    S.add("dve", lambda e: e.memset(ones_f[:], 1.0), w=["ones_f"])
    S.add("dve", lambda e: e.memset(nones[:], -1.0), w=["nones"])
    S.add("pool", lambda e: e.memset(tmpf[:], -1.0), w=["tmpf"])
    S.add("pool", lambda e: e.affine_select(out=tmpf[:], in_=tmpf[:], pattern=[[-1, 128]], compare_op=ALU.is_ge, fill=0.0,
                                            base=0, channel_multiplier=1), r=["tmpf"], w=["tmpf"])
    S.add("dve", lambda e: e.tensor_copy(ntri[:], tmpf[:]), r=["tmpf"], w=["ntri"])
    tmpg = sb("tmpg", [128, 128], F32)
    S.add("pool", lambda e: e.memset(tmpg[:], 1.0), w=["tmpg"])
    S.add("pool", lambda e: e.affine_select(out=tmpg[:], in_=tmpg[:], pattern=[[1, 128]], compare_op=ALU.is_ge, fill=0.0,
                                            base=-1, channel_multiplier=-1), r=["tmpg"], w=["tmpg"])
    S.add("dve", lambda e: e.tensor_copy(smask[:], tmpg[:]), r=["tmpg"], w=["smask"])
    qhs = [sb("qh%d" % i, [128, SEQ], BF16) for i in range(2)]
    khs = [sb("kh%d" % i, [128, SEQ], BF16) for i in range(2)]
    vhs = [sb("vh%d" % i, [128, NB, 128], BF16) for i in range(2)]
    NBUF = 3
    e_sb = [sb("e_sb%d" % i, [128, 512], F32) for i in range(2)]
    sp_bf = [sb("sp_bf%d" % i, [128, 512], BF16) for i in range(NBUF)]
    A_bf = [sb("A_bf%d" % i, [128, 512], BF16) for i in range(NBUF)]
    RS_f = sb("RS_f", [128, 512], F32)
    RS_b = [sb("RS_b%d" % i, [128, 512], BF16) for i in range(NBUF)]
    ost = [sb("ost%d" % i, [128, 512], F32) for i in range(2)]
    TC = SEQ // 4

    its = []
    for h in range(NH):
        for QB in range(NQB):
            kb_hi = 4 * QB + 3
            for kb in range(kb_hi, -1, -1):
                its.append((h, QB, kb))

    def geom(n):
        h, QB, kb = its[n]
        r_ = kb - 4 * QB
        col0 = r_ * 128 if r_ >= 0 else 0
        return h, QB, kb, r_, col0, 512 - col0, kb == 4 * QB + 3, QB * 512

    def load_head(h):
        hp = h % 2
        qh, kh, vh = qhs[hp], khs[hp], vhs[hp]
        if gth is None:
            S.add("sp", lambda e, h=h: e.dma_start(out=qh[:], in_=qT[h]), w=[("qh", hp, i) for i in range(4)], slot="qh%d" % hp)
            S.add("sp", lambda e, h=h: e.dma_start(out=kh[:], in_=kT[h]), w=[("kh", hp, i) for i in range(4)], slot="kh%d" % hp)
            S.add("sp", lambda e, h=h: e.dma_start(out=vh[:], in_=v_tm[h].rearrange("(b p) d -> p b d", p=128)),
                  w=[("vh", hp, i) for i in range((NB + 7) // 8)], slot="vh%d" % hp)
        else:
            ixt, ixkey = gth["ixt"], gth["ixkey"]
            for s_ in range(4):
                col = gth["SQ"] + h * 4 + s_
                S.add("pool", lambda e, s_=s_, col=col: e.indirect_dma_start(
                    out=qh[:, s_ * TC:(s_ + 1) * TC], out_offset=None, in_=gth["q_view"],
                    in_offset=bass.IndirectOffsetOnAxis(ap=ixt[:, col:col + 1], axis=0)),
                    r=[ixkey] + list(gth["agk"]["q"]), w=[("qh", hp, s_)], slot="qh%d_%d" % (hp, s_))
                S.add("pool", lambda e, s_=s_, col=col: e.indirect_dma_start(
                    out=kh[:, s_ * TC:(s_ + 1) * TC], out_offset=None, in_=gth["k_view"],
                    in_offset=bass.IndirectOffsetOnAxis(ap=ixt[:, col:col + 1], axis=0)),
                    r=[ixkey] + list(gth["agk"]["k"]), w=[("kh", hp, s_)], slot="kh%d_%d" % (hp, s_))
            for blk in range(NB):
                col = gth["SV"] + h * NB + blk
                S.add("pool", lambda e, blk=blk, col=col: e.indirect_dma_start(
                    out=vh[:, blk, :], out_offset=None, in_=gth["v_view"],
                    in_offset=bass.IndirectOffsetOnAxis(ap=ixt[:, col:col + 1], axis=0)),
                    r=[ixkey] + list(gth["agk"]["v"]), w=[("vh", hp, blk // 8)], slot="vh%d_%d" % (hp, blk // 8), group=True)

    def part_a(n):
        h, QB, kb, r_, col0, ncol, first, q0 = geom(n)
        if n == 0:
            load_head(0)
        if (n == 0 or its[n - 1][0] != h) and h + 1 < NH:
            load_head(h + 1)
        i2, i3 = n % 2, n % NBUF
        hp = h % 2
        kblk = khs[hp][:, kb * 128:(kb + 1) * 128]
        qcols = qhs[hp][:, q0 + col0:q0 + 512]
        khk, qhk = ("kh", hp, (kb * 128) // TC), ("qh", hp, q0 // TC)
        if first:
            S.add("dve", lambda e: e.memset(RS_f[:], 0.0), w=["RS_f"])
        S.add("pe", lambda e: e.matmul(zb[i2][:, :ncol], kblk, qcols, start=True, stop=True), r=[khk, qhk], w=[("zb", i2)])
        S.add("act", lambda e: e.activation(out=e_sb[i2][:, :ncol], in_=zb[i2][:, :ncol], func=AF.Exp), r=[("zb", i2)], w=[("e_sb", i2)])
        S.add("act", lambda e: e.activation(out=sp_bf[i3][:, :ncol], in_=e_sb[i2][:, :ncol], func=AF.Ln, bias=ones_f[:, 0:1]),
              r=[("e_sb", i2), "ones_f"], w=[("sp", i3)])
        if r_ >= 0:
            S.add("pool", lambda e: e.tensor_tensor(sp_bf[i3][:, :128], sp_bf[i3][:, :128], smask[:], ALU.mult),
                  r=[("sp", i3), "smask"], w=[("sp", i3)])
        if kb > 0:
            S.add("dve", lambda e: e.tensor_tensor(RS_f[:, col0:512], RS_f[:, col0:512], sp_bf[i3][:, :ncol], ALU.add),
                  r=[("sp", i3), "RS_f"], w=["RS_f"])
            nx = (n + 1) % NBUF
            S.add("dve", lambda e: e.tensor_copy(RS_b[nx][:], RS_f[:]), r=["RS_f"], w=[("RS_b", nx)])

    def part_b(n):
        h, QB, kb, r_, col0, ncol, first, q0 = geom(n)
        i2, i3 = n % 2, n % NBUF
        hp = h % 2
        kblk = khs[hp][:, kb * 128:(kb + 1) * 128]
        qcols = qhs[hp][:, q0 + col0:q0 + 512]
        khk, qhk = ("kh", hp, (kb * 128) // TC), ("qh", hp, q0 // TC)
        S.add("pe", lambda e: e.matmul(ab[i2][:, :ncol], kblk, qcols, start=True, stop=False), r=[khk, qhk], w=[("ab", i2)])
        S.add("pe", lambda e: e.matmul(ab[i2][:, :ncol], ntri[:], sp_bf[i3][:, :ncol], start=False, stop=first),
              r=["ntri", ("sp", i3)], w=[("ab", i2)])
        if not first:
            S.add("pe", lambda e: e.matmul(ab[i2][:, :ncol], nones[:], RS_b[i3][:, col0:512], start=False, stop=True),
                  r=["nones", ("RS_b", i3)], w=[("ab", i2)])
        S.add("act", lambda e: e.activation(out=A_bf[i3][:, :ncol], in_=ab[i2][:, :ncol], func=AF.Exp), r=[("ab", i2)], w=[("A", i3)])
        if r_ >= 0:
            S.add("pool", lambda e: e.tensor_tensor(A_bf[i3][:, :128], A_bf[i3][:, :128], smask[:], ALU.mult),
                  r=[("A", i3), "smask"], w=[("A", i3)])

    def part_c(n):
        h, QB, kb, r_, col0, ncol, first, q0 = geom(n)
        i3 = n % NBUF
        o_i = (h * NQB + QB) % 2
        hp = h % 2
        S.add("pe", lambda e: e.matmul(ob[o_i][:, col0:512], vhs[hp][:, kb, :], A_bf[i3][:, :ncol], start=first, stop=(kb == 0)),
              r=[("vh", hp, kb // 8), ("A", i3)], w=[("ob", o_i)])
        if kb == 0:
            S.add("dve", lambda e: e.tensor_copy(ost[o_i][:], ob[o_i][:]), r=[("ob", o_i)], w=[("ost", o_i)])
            if out_ag is None:
                S.add("sp", lambda e: e.dma_start(out=oT[h][:, q0:q0 + 512], in_=ost[o_i][:]),
                      r=[("ost", o_i)], w=[("dram", "o", h, QB)], slot="ost%d" % o_i)
            else:
                ck = h * 4 + QB // 4
                dst = out_ag["o_b"][ck * 128:(ck + 1) * 128, (QB % 4) * 512:(QB % 4 + 1) * 512]
                S.add("sp", lambda e: e.dma_start(out=dst, in_=ost[o_i][:]),
                      r=[("ost", o_i)], w=[("dram", "o", h, QB)], slot="ost%d" % o_i)
                if QB % 4 == 3:
                    ag_pending.append([n + 12, lambda: out_ag["agf"](S, "a", 128, [ck], [("dram", "o", h, QB - i_) for i_ in range(4)])])

    N = len(its)
    ag_pending = []
    for n in range(N + 2):
        for item in [it_ for it_ in ag_pending if it_[0] <= n]:
            item[1]()
            ag_pending.remove(item)
        if n < N:
            part_a(n)
        if 0 <= n - 1 < N:
            part_b(n - 1)
        if 0 <= n - 2 < N:
            part_c(n - 2)
    for item in ag_pending:
        item[1]()


def build_sb_core(NH, SEQ):
    nc = bass.Bass("TRN2", target_bir_lowering=False)
    qT = nc.dram_tensor("qT", [NH, 128, SEQ], BF16, kind="ExternalInput").ap()
    kT = nc.dram_tensor("kT", [NH, 128, SEQ], BF16, kind="ExternalInput").ap()
    v_tm = nc.dram_tensor("v_tm", [NH, SEQ, 128], BF16, kind="ExternalInput").ap()
    oT = nc.dram_tensor("oT", [NH, 128, SEQ], F32, kind="ExternalOutput").ap()
    S = Sched(nc)
    emit_sb_core(S, NH, SEQ, qT, kT, v_tm, oT)
    S.emit()
    return nc, S


def _vec(nc, name, n):
    return nc.dram_tensor(name, [128, n], F32, kind="ExternalInput").ap()


def build_ple(T, TT=512):
    nc = bass.Bass("TRN2", target_bir_lowering=False)
    xin = nc.dram_tensor("xin", [D, T], F32, kind="ExternalInput").ap()
    pT = nc.dram_tensor("pT", [256, T], F32, kind="ExternalInput").ap()
    g = _vec(nc, "g", KC)
    wpe = nc.dram_tensor("wpe", [256, D], F32, kind="ExternalInput").ap()
    wpg = nc.dram_tensor("wpg", [D, D], F32, kind="ExternalInput").ap()
    xout = nc.dram_tensor("xout", [D, T], F32, kind="ExternalOutput").ap()
    S = Sched(nc)
    C = Ctx(S, TT)
    C.load_vec("g", g, KC)
    emit_ple(C, xin, xout, pT, T, "g", wpe, wpg)
    S.emit()
    return nc, S


def build_proj_res(T, gated, TT=512):
    nc = bass.Bass("TRN2", target_bir_lowering=False)
    xin = nc.dram_tensor("xin", [D, T], F32, kind="ExternalInput").ap()
    aT = nc.dram_tensor("aT", [D, T], F32, kind="ExternalInput").ap()
    bT = nc.dram_tensor("bT", [D, T], F32, kind="ExternalInput").ap() if gated else None
    W = nc.dram_tensor("W", [D, D], F32, kind="ExternalInput").ap()
    xout = nc.dram_tensor("xout", [D, T], F32, kind="ExternalOutput").ap()
    S = Sched(nc)
    C = Ctx(S, TT)
    emit_proj_res(C, xin, xout, aT, bT, T, W)
    S.emit()
    return nc, S


def build_mlstm_in(T, TT=512):
    nc = bass.Bass("TRN2", target_bir_lowering=False)
    xin = nc.dram_tensor("xin", [D, T], F32, kind="ExternalInput").ap()
    g = _vec(nc, "g", KC)
    w_in = nc.dram_tensor("w_in", [D, 6152], F32, kind="ExternalInput").ap()
    qT = nc.dram_tensor("qT", [1024, T], BF16, kind="ExternalOutput").ap()
    kT = nc.dram_tensor("kT", [1024, T], BF16, kind="ExternalOutput").ap()
    k_tm = nc.dram_tensor("k_tm", [T, 1024], BF16, kind="ExternalOutput").ap()
    v_tm = nc.dram_tensor("v_tm", [T, 2048], BF16, kind="ExternalOutput").ap()
    sog = nc.dram_tensor("sog", [2048, T], F32, kind="ExternalOutput").ap()
    gates = nc.dram_tensor("gates", [8, T], F32, kind="ExternalOutput").ap()
    S = Sched(nc)
    C = Ctx(S, TT)
    C.load_vec("g", g, KC)
    emit_mlstm_in(C, xin, T, "g", w_in, qT, kT, k_tm, v_tm, sog, gates)
    S.emit()
    return nc, S


def build_kv(T, TT=512):
    nc = bass.Bass("TRN2", target_bir_lowering=False)
    xin = nc.dram_tensor("xin", [D, T], F32, kind="ExternalInput").ap()
    g = _vec(nc, "g", KC)
    gk = _vec(nc, "g_k", 1)
    w_kv = nc.dram_tensor("w_kv", [D, 4096], F32, kind="ExternalInput").ap()
    kT = nc.dram_tensor("kT", [2048, T], BF16, kind="ExternalOutput").ap()
    v_tm = nc.dram_tensor("v_tm", [T, 2048], BF16, kind="ExternalOutput").ap()
    S = Sched(nc)
    C = Ctx(S, TT)
    C.load_vec("g", g, KC)
    C.load_vec("g_k", gk, 1)
    emit_kv(C, xin, T, "g", w_kv, kT, v_tm)
    S.emit()
    return nc, S


def build_q(T, TT=512):
    nc = bass.Bass("TRN2", target_bir_lowering=False)
    xin = nc.dram_tensor("xin", [D, T], F32, kind="ExternalInput").ap()
    g = _vec(nc, "g", KC)
    gq = _vec(nc, "g_q", 1)
    w_q = nc.dram_tensor("w_q", [D, D], F32, kind="ExternalInput").ap()
    qT = nc.dram_tensor("qT", [2048, T], BF16, kind="ExternalOutput").ap()
    S = Sched(nc)
    C = Ctx(S, TT)
    C.load_vec("g", g, KC)
    C.load_vec("g_q", gq, 1)
    for _ in emit_headproj(C, xin, T, "g", w_q, 0, "g_q", 128 ** -0.5, qT):
        pass
    S.emit()
    return nc, S


def build_ffn(T, TT=512):
    nc = bass.Bass("TRN2", target_bir_lowering=False)
    xin = nc.dram_tensor("xin", [D, T], F32, kind="ExternalInput").ap()
    g = nc.dram_tensor("g", [128, KC], F32, kind="ExternalInput").ap()
    wg = nc.dram_tensor("wg", [D, DFF], F32, kind="ExternalInput").ap()
    wu = nc.dram_tensor("wu", [D, DFF], F32, kind="ExternalInput").ap()
    wd = nc.dram_tensor("wd", [DFF, D], F32, kind="ExternalInput").ap()
    xout = nc.dram_tensor("xout", [D, T], F32, kind="ExternalOutput").ap()
    S = Sched(nc)
    C = Ctx(S, TT)
    C.load_vec("g", g, KC)
    emit_ffn(C, xin, xout, T, "g", wg, wu, wd)
    S.emit()
    return nc, S


RG = [[0, 1, 2, 3], [4, 5, 6, 7]]
IX = dict(MQ=0, MK=16, MV=80, PAM=144, PAS=208, SQ=272, SV=288)
NIDX = 544
TCORE = 2048
SEQLEN = 8192
I32 = mybir.dt.int32


def build_fused(plan="full"):
    nc = bass.Bass("TRN2", target_bir_lowering=False)
    T = TCORE
    ext_names = []
    _cache = {}
    shapes = {
        "xT": ([D, T], F32), "pT": ([4, 256, T], F32), "ffn_norm": ([4, 2, 128, KC], F32),
        "ffn_w_gate": ([4, 2, D, DFF], F32), "ffn_w_up": ([4, 2, D, DFF], F32), "ffn_w_down": ([4, 2, DFF, D], F32),
        "mix_norm": ([4, 128, KC], F32), "mlstm_w_in": ([2, D, 6152], F32), "bif": ([2, 1, 2], F32),
        "ghead": ([2, 128, 4], F32), "mlstm_w_out": ([2, D, D], F32), "kv_norm": ([128, KC], F32),
        "sb_w_kv": ([D, 4096], F32), "g_k": ([128, 1], F32), "sb_w_q": ([2, D, D], F32), "g_q": ([2, 128, 1], F32),
        "sb_w_o": ([2, D, D], F32), "ple_norm": ([4, 128, KC], F32), "ple_w_proj": ([4, 256, D], F32),
        "ple_w_gate": ([4, D, D], F32), "idx": ([128, NIDX], I32), "onehot": ([1, 4], F32),
    }

    class _Ext:
        def __getattr__(self, name):
            if name not in _cache:
                shp, dt = shapes[name]
                _cache[name] = nc.dram_tensor(name, shp, dt, kind="ExternalInput").ap()
                ext_names.append(name)
            return _cache[name]
    E = _Ext()
    nc._ext_names = ext_names
    outT = nc.dram_tensor("outT", [D, T], F32, kind="ExternalOutput").ap()

    def dt_(name, shape, dt=F32):
        return nc.dram_tensor(name, shape, dt)

    XA, XB = dt_("XA", [D, T]), dt_("XB", [D, T])
    q_b, k_b = dt_("q_b", [1024, T], BF16), dt_("k_b", [1024, T], BF16)
    ktm_b, vtm_b = dt_("ktm_b", [T, 1024], BF16), dt_("vtm_b", [T, 2048], BF16)
    gates_b = dt_("gates_b", [8, T])
    sog = dt_("sog", [2048, T])
    q_all, k_all = dt_("q_all", [4096, T], BF16), dt_("k_all", [4096, T], BF16)
    ktm_all, vtm_all = dt_("ktm_all", [4 * T, 1024], BF16), dt_("vtm_all", [4 * T, 2048], BF16)
    gates_all = dt_("gates_all", [32, T])
    h_bm, h_allm = dt_("h_bm", [8 * 512, 1024]), dt_("h_allm", [16 * 1024, 1024])
    h_bs, h_alls = dt_("h_bs", [16 * 128, 2048]), dt_("h_alls", [16 * 512, 2048])
    sq_b, sk_b, sv_b = dt_("sq_b", [2048, T], BF16), dt_("sk_b", [2048, T], BF16), dt_("sv_b", [T, 2048], BF16)
    sq_all, sk_all, sv_all = dt_("sq_all", [8192, T], BF16), dt_("sk_all", [8192, T], BF16), dt_("sv_all", [4 * T, 2048], BF16)

    stage_no = [0]

    def stage(fn):
        with nc.cleanup_on_exit():
            S = Sched(nc, "s%d_" % stage_no[0])
            stage_no[0] += 1
            fn(S)
            S.emit()

    def agc(S, name, src, dst, rk, ks, rkeys=()):
        for k in ks:
            S.add("pool", lambda e, k=k: e.collective_compute(
                "AllGather", ALU.bypass, replica_groups=RG,
                ins=[src[k * rk:(k + 1) * rk, :].opt()], outs=[dst[k * 4 * rk:(k + 1) * 4 * rk, :].opt()]),
                r=list(rkeys), w=[("ag", name)], slot="cc_%s" % name, inc=1, group=True)
        return [("ag", name)]

    def ag(S, name, src, dst, rk):
        return agc(S, name, src, dst, rk, range(src.shape[0] // rk))

    def load_idx(S):
        ixt = S.sb("ixt", [128, NIDX], I32)
        S.add("sp", lambda e: e.dma_start(out=ixt[:], in_=E.idx), w=["ixt"], slot="ixt")
        return ixt

    state = {"cur": E.xT, "flip": 0}

    def nxt_buf(final=False):
        if final:
            return outT
        t = (XA, XB)[state["flip"]]
        state["flip"] ^= 1
        return t.ap()

    def st_ffn(i, k):
        cur, nxt = state["cur"], nxt_buf()

        def f(S):
            C = Ctx(S, 512)
            C.load_vec("g", E.ffn_norm[i, k], KC)
            emit_ffn(C, cur, nxt, T, "g", E.ffn_w_gate[i, k], E.ffn_w_up[i, k], E.ffn_w_down[i, k])
        stage(f)
        state["cur"] = nxt

    def st_ple(i, final):
        cur, nxt = state["cur"], nxt_buf(final)

        def f(S):
            C = Ctx(S, 512)
            C.load_vec("g", E.ple_norm[i], KC)
            emit_ple(C, cur, nxt, E.pT[i], T, "g", E.ple_w_proj[i], E.ple_w_gate[i])
        stage(f)
        state["cur"] = nxt

    def st_proj(W, gated, final=False):
        cur, nxt = state["cur"], nxt_buf(final)

        def f(S):
            C = Ctx(S, 512)
            ixt = load_idx(S)
            if gated:
                view, cb = h_allm.ap().rearrange("r (k f) -> (r k) f", k=2), IX["PAM"]
            else:
                view, cb = h_alls.ap().rearrange("r (k f) -> (r k) f", k=4), IX["PAS"]
            emit_proj_res(C, cur, nxt, None, sog.ap() if gated else None, T, W, gth=(view, ixt, "ixt", cb, []))
        stage(f)
        state["cur"] = nxt

    def mlstm_part(i):
        def f(S, i=i):
            C = Ctx(S, 512)
            C.load_vec("g", E.mix_norm[i], KC)
            def agf(S, name, rk, ks, rkeys):
                src, dst = {"ktm": (ktm_b, ktm_all), "vtm": (vtm_b, vtm_all)}[name]
                agc(S, name, src, dst, rk, ks, rkeys)
            emit_mlstm_in(C, state["cur"], T, "g", E.mlstm_w_in[i], q_b.ap(), k_b.ap(), ktm_b.ap(), vtm_b.ap(), sog.ap(), gates_b.ap(), agf=agf)
        stage(f)

        def f(S, i=i):
            ixt = load_idx(S)
            agk = {"q": ag(S, "q", q_b, q_all, 256), "k": ag(S, "k", k_b, k_all, 256),
                   "ktm": [], "vtm": [], "g": ag(S, "g", gates_b, gates_all, 8)}
            gth = dict(IX)
            gth["agk"] = agk
            gth.update(ixt=ixt, ixkey="ixt", onehot=E.onehot,
                       q_view=q_all.ap().rearrange("r (two f) -> (r two) f", two=2),
                       k_view=k_all.ap().rearrange("r (two f) -> (r two) f", two=2),
                       ktm_view=ktm_all.ap().rearrange("t (h f) -> (t h) f", h=4),
                       vtm_view=vtm_all.ap().rearrange("t (h f) -> (t h) f", h=4),
                       gates_all=gates_all.ap())
            out_ag = {"h_b": h_bm.ap(), "agf": lambda S, name, rk, ks, rkeys: agc(S, name, h_bm, h_allm, rk, ks, rkeys)}
            emit_mlstm_core(S, SEQLEN, None, None, None, None, None, None, E.bif[i], E.ghead[i], None, gth=gth, out_ag=out_ag)
        stage(f)
        st_proj(E.mlstm_w_out[i], True, final=(plan == "mlstm0"))

    def kv_part():
        def f(S):
            C = Ctx(S, 512)
            C.load_vec("g", E.kv_norm, KC)
            C.load_vec("g_k", E.g_k, 1)
            emit_kv(C, state["cur"], T, "g", E.sb_w_kv, sk_b.ap(), sv_b.ap(),
                    agf=lambda S, name, rk, ks, rkeys: agc(S, name, sv_b, sv_all, rk, ks, rkeys))
        stage(f)

    def sb_part(j):
        def f(S, j=j):
            C = Ctx(S, 512)
            C.load_vec("g", E.mix_norm[2 + j], KC)
            C.load_vec("g_q", E.g_q[j], 1)
            for _ in emit_headproj(C, state["cur"], T, "g", E.sb_w_q[j], 0, "g_q", 128 ** -0.5, sq_b.ap()):
                pass
        stage(f)

        def f(S, j=j):
            ixt = load_idx(S)
            agk = {"q": ag(S, "q", sq_b, sq_all, 256), "k": [], "v": []}
            if j == 0:
                agk["k"] = ag(S, "k", sk_b, sk_all, 256)
            gth = dict(IX)
            gth["agk"] = agk
            gth.update(ixt=ixt, ixkey="ixt", q_view=sq_all.ap(), k_view=sk_all.ap(),
                       v_view=sv_all.ap().rearrange("t (h f) -> (t h) f", h=16))
            out_ag = {"o_b": h_bs.ap(), "agf": lambda S, name, rk, ks, rkeys: agc(S, name, h_bs, h_alls, rk, ks, rkeys)}
            emit_sb_core(S, 4, SEQLEN, None, None, None, None, gth=gth, out_ag=out_ag)
        stage(f)
        st_proj(E.sb_w_o[j], False, final=(plan == "sb0"))

    if plan == "mlstm0":
        mlstm_part(0)
        return nc
    if plan == "sb0":
        kv_part()
        sb_part(0)
        return nc
    for i in range(4):
        if i == 2:
            kv_part()
        st_ffn(i, 0)
        if i < 2:
            mlstm_part(i)
        else:
            sb_part(i - 2)
        st_ffn(i, 1)
        st_ple(i, final=(i == 3))
    return nc


_PROG = {}


def _c(a):
    return np.ascontiguousarray(a)


def _vl(g, n):
    g = np.asarray(g, dtype=np.float32)
    lead = g.shape[:-1]
    return _c(np.swapaxes(g.reshape(lead + (n, 128)), -1, -2))


def _idx_table(c):
    h = c % 4
    p = np.arange(128, dtype=np.int64)
    t = np.zeros((128, NIDX), dtype=np.int64)

    def g256(r, s_):
        return (r // 256) * 1024 + s_ * 256 + r % 256

    for sg in range(8):
        s_, half = sg // 2, sg % 2
        for dk in range(2):
            t[:, IX["MQ"] + sg * 2 + dk] = g256(h * 256 + dk * 128 + p, s_) * 2 + half
    for g in range(64):
        tg = g * 128 + p
        s_, tt = tg // 2048, tg % 2048
        t[:, IX["MK"] + g] = ((tt // 512) * 2048 + s_ * 512 + tt % 512) * 4 + h
        t[:, IX["MV"] + g] = g256(tt, s_) * 4 + h
    for cch in range(16):
        hh, r = cch // 4, (cch % 4) * 128 + p
        for ti in range(4):
            sg = h * 2 + ti // 2
            ck = sg * 2 + r // 256
            t[:, IX["PAM"] + cch * 4 + ti] = (ck * 1024 + hh * 256 + r % 256) * 2 + ti % 2
            ck = (r // 128) * 4 + h
            t[:, IX["PAS"] + cch * 4 + ti] = (ck * 512 + hh * 128 + r % 128) * 4 + ti
    for hl in range(4):
        for s_ in range(4):
            t[:, IX["SQ"] + hl * 4 + s_] = g256((4 * h + hl) * 128 + p, s_)
        for blk in range(64):
            tg = blk * 128 + p
            s_, tt = tg // 2048, tg % 2048
            t[:, IX["SV"] + hl * 64 + blk] = g256(tt, s_) * 16 + 4 * h + hl
    return t.astype(np.int32)


def kernel(x, p, ffn_norm, ffn_w_gate, ffn_w_up, ffn_w_down, mix_norm,
           mlstm_w_in, mlstm_b_if, mlstm_head_norm, mlstm_w_out,
           kv_norm, sb_w_kv, sb_k_norm, sb_w_q, sb_q_norm, sb_w_o,
           ple_norm, ple_w_proj, ple_w_gate):
    f32 = np.float32
    T = TCORE
    x = np.asarray(x, f32)
    p = np.asarray(p, f32)
    if "nc" not in _PROG:
        _PROG["nc"] = build_fused()
    nc = _PROG["nc"]
    shared = {
        "ffn_norm": _vl(ffn_norm, KC),
        "ffn_w_gate": _c(np.asarray(ffn_w_gate, f32)), "ffn_w_up": _c(np.asarray(ffn_w_up, f32)),
        "ffn_w_down": _c(np.asarray(ffn_w_down, f32)),
        "mix_norm": _vl(mix_norm, KC),
        "mlstm_w_in": _c(np.asarray(mlstm_w_in, f32)), "mlstm_w_out": _c(np.asarray(mlstm_w_out, f32)),
        "kv_norm": _vl(kv_norm, KC), "sb_w_kv": _c(np.asarray(sb_w_kv, f32)), "g_k": _vl(sb_k_norm, 1),
        "sb_w_q": _c(np.asarray(sb_w_q, f32)), "g_q": _vl(sb_q_norm, 1), "sb_w_o": _c(np.asarray(sb_w_o, f32)),
        "ple_norm": _vl(ple_norm, KC), "ple_w_proj": _c(np.asarray(ple_w_proj, f32)),
        "ple_w_gate": _c(np.asarray(ple_w_gate, f32)),
    }
    b_if = np.asarray(mlstm_b_if, f32)
    hn = np.asarray(mlstm_head_norm, f32)
    in_maps = []
    for c in range(NCORES):
        b, s = c // 4, c % 4
        h = s
        m = dict(shared)
        m["xT"] = _c(x[b, s * T:(s + 1) * T].T)
        m["pT"] = _c(p[:, b, s * T:(s + 1) * T, :].transpose(0, 2, 1))
        m["bif"] = _c(np.stack([b_if[:, h], b_if[:, 4 + h]], axis=-1)[:, None, :])
        m["ghead"] = _vl(hn[:, h * 512:(h + 1) * 512], 4)
        m["idx"] = _idx_table(c)
        oh = np.zeros((1, 4), f32)
        oh[0, h] = 1.0
        m["onehot"] = oh
        in_maps.append({k: v for k, v in m.items() if k in nc._ext_names})
    res = run_bass_kernel_spmd(nc, in_maps, core_ids=list(range(NCORES)))
    out = np.empty((2, SEQLEN, D), dtype=f32)
    for c in range(NCORES):
        b, s = c // 4, c % 4
        out[b, s * T:(s + 1) * T] = res.results[c]["outT"].T
    return out
```

```python
import contextlib
import numpy as np
import concourse.bass as bass
import concourse.mybir as mybir
from concourse.bass_utils import run_bass_kernel_spmd

F32 = mybir.dt.float32
BF16 = mybir.dt.bfloat16
AF = mybir.ActivationFunctionType
ALU = mybir.AluOpType

D = 2048
KC = D // 128
DFF = 5504
FC = DFF // 128
EPS = 1e-6
NCORES = 8
PENG = 'dve'
SELF_ORDERED = {'pe'}
NSTF = 4
USE_SOFTPLUS = True


class Op:
    __slots__ = ("eng", "fn", "deps", "slot", "needed", "tok", "waits", "know", "inc")

    def __init__(self, eng, fn, slot, inc=16):
        self.eng = eng
        self.fn = fn
        self.slot = slot
        self.inc = inc
        self.deps = set()
        self.needed = False
        self.tok = None
        self.waits = []
        self.know = None


class Sched:
    ENGS = ("pe", "act", "dve", "pool", "sp")

    def __init__(self, nc, prefix=""):
        self.nc = nc
        self.prefix = prefix
        self.ops = []
        self.last_w = {}
        self.readers = {}
        self.stack = contextlib.ExitStack()
        self.n_t = 0

    def uid(self):
        self.n_t += 1
        return self.n_t

    def sb(self, name, shape, dt):
        return self.stack.enter_context(self.nc.sbuf_tensor(self.prefix + name, shape, dt))

    def ps(self, name, shape, dt=F32):
        return self.stack.enter_context(self.nc.psum_tensor(self.prefix + name, shape, dt))

    def add(self, eng, fn, r=(), w=(), slot=None, inc=16, group=False):
        op = Op(eng, fn, slot, inc)
        deps = op.deps
        for k in r:
            d = self.last_w.get(k)
            if d is not None:
                deps.add(d)
        for k in w:
            d = self.last_w.get(k)
            if d is not None and not (group and d.slot == slot):
                deps.add(d)
            elif d is not None:
                deps.update(x for x in d.deps if x.slot != slot)
            rs = self.readers.get(k)
            if rs:
                deps.update(rs)
        for k in w:
            self.last_w[k] = op
            self.readers[k] = set()
        for k in r:
            self.readers.setdefault(k, set()).add(op)
        deps.discard(op)
        self.ops.append(op)
        return op

    def emit(self):
        nc = self.nc
        ops = self.ops
        for op in ops:
            for d in op.deps:
                if d.eng == op.eng and d.eng in SELF_ORDERED and d.slot is None and op.slot is None:
                    continue
                d.needed = True
        cnt = {e: 0 for e in self.ENGS}
        slot_cnt = {}
        for op in ops:
            if op.slot is not None:
                slot_cnt[op.slot] = slot_cnt.get(op.slot, 0) + op.inc
                op.tok = (("slot", op.slot), slot_cnt[op.slot])
            elif op.needed:
                cnt[op.eng] += 1
                op.tok = (("eng", op.eng), cnt[op.eng])
        sems = {}
        for e in self.ENGS:
            sems[("eng", e)] = nc.alloc_semaphore(self.prefix + "s_" + e)
        for i, sl in enumerate(slot_cnt):
            sems[("slot", sl)] = nc.alloc_semaphore(self.prefix + "d%d" % i)
        known = {e: {} for e in self.ENGS}
        for op in ops:
            kn = known[op.eng]
            for d in op.deps:
                if d.eng == op.eng and d.eng in SELF_ORDERED and d.slot is None and op.slot is None:
                    continue
                sk, val = d.tok
                if kn.get(sk, 0) >= val:
                    continue
                op.waits.append((sk, val))
                for k2, v2 in d.know.items():
                    if kn.get(k2, 0) < v2:
                        kn[k2] = v2
            if op.tok is not None:
                kn2 = dict(kn)
                sk, val = op.tok
                if kn2.get(sk, 0) < val:
                    kn2[sk] = val
                op.know = kn2
        per_eng = {e: [] for e in self.ENGS}
        for op in ops:
            w = {}
            for sk, val in op.waits:
                if w.get(sk, 0) < val:
                    w[sk] = val
            op.waits = list(w.items())
            per_eng[op.eng].append(op)
        final_waits = [(("slot", sl), v) for sl, v in slot_cnt.items()]
        self.stats = {e: len(per_eng[e]) for e in self.ENGS}

        def run(engobj, lst, final=False):
            for op in lst:
                for sk, val in op.waits:
                    engobj.wait_ge(sems[sk], val)
                ins = op.fn(engobj)
                if op.tok is not None:
                    if op.slot is not None and op.inc == 1:
                        ins.then_inc(sems[op.tok[0]])
                    else:
                        ins.then_inc(sems[op.tok[0]], op.inc if op.slot is not None else 1)
            if final:
                for sk, val in final_waits:
                    engobj.wait_ge(sems[sk], val)

        with nc.Block(self.prefix + "blk") as block:
            @block.tensor
            def _(e):
                run(e, per_eng["pe"])

            @block.scalar
            def _(e):
                run(e, per_eng["act"])

            @block.vector
            def _(e):
                run(e, per_eng["dve"])

            @block.gpsimd
            def _(e):
                run(e, per_eng["pool"])

            @block.sync
            def _(e):
                run(e, per_eng["sp"], final=True)


class Ctx:
    def __init__(self, S, TT):
        self.S = S
        self.TT = TT
        nc = S.nc
        self.ones = S.sb("ones_bf", [128, 128], BF16)
        self.banks = [S.ps("bank%d" % i, [128, 512], F32) for i in range(8)]
        S.add("dve", lambda e: e.memset(self.ones[:], 1.0), w=[("ones",)])
        self.xt = S.sb("xt", [128, KC, TT], F32)
        self.xn = S.sb("xn", [128, KC, TT], BF16)
        self.sq = S.sb("sq", [128, KC, TT], BF16)
        self.rs = S.sb("rstd", [128, TT], F32)
        self.rs2 = S.sb("rstd2", [128, TT], F32)
        self.gv = {}

    def load_vec(self, name, dram_ap, nchunk):
        S = self.S
        t = S.sb("v_" + name, [128, nchunk], F32)
        S.add("sp", lambda e: e.dma_start(out=t[:], in_=dram_ap), w=[("v", name)], slot="v_" + name)
        self.gv[name] = t
        return t


def emit_norm(C, xin, t0, gname, load_x=True):
    S, TT = C.S, C.TT
    xt, xn, sq, rs, rs2 = C.xt, C.xn, C.sq, C.rs, C.rs2
    g = C.gv[gname]
    ssb = C.banks[0]
    if load_x:
        src = xin.rearrange("(c p) t -> p c t", p=128)[:, :, t0:t0 + TT]
        S.add("sp", lambda e: e.dma_start(out=xt[:], in_=src), w=[("xt",)], slot="xt")
    S.add("act", lambda e: e.activation(out=sq[:], in_=xt[:], func=AF.Square), r=[("xt",)], w=[("sq",)])
    for c in range(KC):
        S.add("pe", lambda e, c=c: e.matmul(ssb[:, :TT], C.ones[:], sq[:, c, :], start=(c == 0), stop=(c == KC - 1)),
              r=[("sq",), ("ones",)], w=[("ps", 0)])
    S.add("dve", lambda e: e.tensor_scalar(rs[:], ssb[:, :TT], 1.0 / D, EPS, ALU.mult, ALU.add),
          r=[("ps", 0)], w=[("rs",)])
    S.add("act", lambda e: e.activation(out=rs2[:], in_=rs[:], func=AF.Sqrt), r=[("rs",)], w=[("rs2",)])
    S.add("dve", lambda e: e.reciprocal(rs[:], rs2[:]), r=[("rs2",)], w=[("rs",)])
    for c in range(KC):
        S.add("dve", lambda e, c=c: e.scalar_tensor_tensor(xn[:, c, :], xt[:, c, :], g[:, c:c + 1], rs[:],
                                                           ALU.mult, ALU.mult),
              r=[("xt",), ("rs",), ("v", gname)], w=[("xn", c)])


def emit_ffn(C, xin, xout, T, gname, wg, wu, wd):
    S, TT = C.S, C.TT
    if not hasattr(C, "wg"):
        C.wg = [S.sb("wg%d" % i, [128, KC, 512], BF16) for i in range(2)]
        C.wu = [S.sb("wu%d" % i, [128, KC, 512], BF16) for i in range(2)]
        C.wd = [S.sb("wd%d" % i, [128, 11, 512], BF16) for i in range(2)]
        C.hid = S.sb("hid", [128, FC, TT], BF16)
        C.sg = [S.sb("sg%d" % i, [128, TT], F32) for i in range(2)]
        C.ctr = {"gu": 0, "wd": 0, "f": 0}
    wg_r = wg.rearrange("(c p) n -> p c n", p=128)
    wu_r = wu.rearrange("(c p) n -> p c n", p=128)
    wd_r = wd.rearrange("(f p) n -> p f n", p=128)
    xo_r = xout.rearrange("(c p) t -> p c t", p=128)
    hid = C.hid
    for t0 in range(0, T, TT):
        emit_norm(C, xin, t0, gname)
        for fg in range(0, FC, 4):
            nf = min(4, FC - fg)
            sl = C.ctr["gu"] % 2
            C.ctr["gu"] += 1
            wgt, wut = C.wg[sl], C.wu[sl]
            S.add("pool", lambda e, wgt=wgt, fg=fg, nf=nf: e.dma_start(
                out=wgt[:, :, :nf * 128], in_=wg_r[:, :, fg * 128:(fg + nf) * 128]),
                w=[("wg", sl)], slot="wg%d" % sl)
            S.add("pool", lambda e, wut=wut, fg=fg, nf=nf: e.dma_start(
                out=wut[:, :, :nf * 128], in_=wu_r[:, :, fg * 128:(fg + nf) * 128]),
                w=[("wu", sl)], slot="wu%d" % sl)
            for fi in range(nf):
                f = fg + fi
                pb = C.ctr["f"] % 2
                C.ctr["f"] += 1
                gb, ub = C.banks[4 + pb], C.banks[6 + pb]
                for c in range(KC):
                    S.add("pe", lambda e, c=c, fi=fi, gb=gb, wgt=wgt: e.matmul(
                        gb[:, :TT], wgt[:, c, fi * 128:(fi + 1) * 128], C.xn[:, c, :], start=(c == 0), stop=(c == KC - 1)),
                        r=[("wg", sl), ("xn", c)], w=[("ps", 4 + pb)])
                for c in range(KC):
                    S.add("pe", lambda e, c=c, fi=fi, ub=ub, wut=wut: e.matmul(
                        ub[:, :TT], wut[:, c, fi * 128:(fi + 1) * 128], C.xn[:, c, :], start=(c == 0), stop=(c == KC - 1)),
                        r=[("wu", sl), ("xn", c)], w=[("ps", 6 + pb)])
                sg = C.sg[pb]
                S.add("act", lambda e, sg=sg, gb=gb: e.activation(out=sg[:], in_=gb[:, :TT], func=AF.Silu),
                      r=[("ps", 4 + pb)], w=[("sg", pb)])
                S.add("dve", lambda e, sg=sg, ub=ub, f=f: e.tensor_tensor(hid[:, f, :], ub[:, :TT], sg[:], ALU.mult),
                      r=[("ps", 6 + pb), ("sg", pb)], w=[("hid", f)])
        for ng in range(4):
            for f0 in range(0, FC, 11):
                nf = min(11, FC - f0)
                sl = C.ctr["wd"] % 2
                C.ctr["wd"] += 1
                wdt = C.wd[sl]
                S.add("pool", lambda e, wdt=wdt, f0=f0, nf=nf, ng=ng: e.dma_start(
                    out=wdt[:, :nf, :], in_=wd_r[:, f0:f0 + nf, ng * 512:(ng + 1) * 512]),
                    w=[("wd", sl)], slot="wd%d" % sl)
                for fi in range(nf):
                    f = f0 + fi
                    for j in range(4):
                        S.add("pe", lambda e, wdt=wdt, fi=fi, f=f, j=j: e.matmul(
                            C.banks[j][:, :TT], wdt[:, fi, j * 128:(j + 1) * 128], hid[:, f, :],
                            start=(f == 0), stop=(f == FC - 1)),
                            r=[("wd", sl), ("hid", f)], w=[("ps", j)])
            for j in range(4):
                c = ng * 4 + j
                S.add("dve", lambda e, j=j, c=c: e.scalar_tensor_tensor(
                    C.xt[:, c, :], C.banks[j][:, :TT], 0.5, C.xt[:, c, :], ALU.mult, ALU.add),
                    r=[("ps", j), ("xt",)], w=[("xt",)])
        S.add("sp", lambda e, t0=t0: e.dma_start(out=xo_r[:, :, t0:t0 + TT], in_=C.xt[:]),
              r=[("xt",)], w=[("dram", "xout")], slot="xt_st")


def ctx_extra(C):
    S, TT = C.S, C.TT
    if hasattr(C, "wsl"):
        return
    if not hasattr(C, "wg"):
        C.wg = [S.sb("wg%d" % i, [128, KC, 512], BF16) for i in range(2)]
        C.wu = [S.sb("wu%d" % i, [128, KC, 512], BF16) for i in range(2)]
        C.sg = [S.sb("sg%d" % i, [128, TT], F32) for i in range(2)]
    C.wsl = [(C.wg[0], ("wg", 0), "wg0"), (C.wu[0], ("wu", 0), "wu0"),
             (C.wg[1], ("wg", 1), "wg1"), (C.wu[1], ("wu", 1), "wu1")]
    C.wctr = 0
    C.bctr = 0
    C.stb = [S.sb("stb%d" % i, [128, 4, 512], BF16) for i in range(2)]
    C.stf = [S.sb("stf%d" % i, [128, 4, 512], F32) for i in range(NSTF)]
    C.stctr = {"b": 0, "f": 0}
    C.kf = S.sb("kf", [128, TT], F32)
    C.sqh = S.sb("sqh", [128, TT], BF16)
    C.hr = S.sb("hr", [128, TT], F32)
    C.hr2 = S.sb("hr2", [128, TT], F32)


def next_w(C):
    t = C.wsl[C.wctr % 4]
    C.wctr += 1
    return t


def next_bank(C):
    b = 4 + (C.bctr % 4)
    C.bctr += 1
    return b


def next_stage(C, kind):
    i = C.stctr[kind] % 2
    C.stctr[kind] += 1
    return (C.stb if kind == "b" else C.stf)[i], ("st" + kind, i), "st%s%d" % (kind, i)


def fm_linear(C, W_r, c0, ncols, kch, rhs, rkey, epilogue):
    S, TT = C.S, C.TT
    for gi, g0 in enumerate(range(0, ncols, 512)):
        gw = min(512, ncols - g0)
        wt, wkey, wslot = next_w(C)
        S.add("pool", lambda e, wt=wt, g0=g0, gw=gw: e.dma_start(
            out=wt[:, :kch, :gw], in_=W_r[:, :, c0 + g0:c0 + g0 + gw]), w=[wkey], slot=wslot)
        nj = (gw + 127) // 128
        for j in range(nj):
            m = min(128, gw - j * 128)
            b = next_bank(C)
            for c in range(kch):
                S.add("pe", lambda e, wt=wt, c=c, j=j, m=m, b=b: e.matmul(
                    C.banks[b][:m, :TT], wt[:, c, j * 128:j * 128 + m], rhs[:, c, :], start=(c == 0), stop=(c == kch - 1)),
                    r=[wkey, rkey(c)], w=[("ps", b)])
            epilogue(gi, j, nj, m, b)


def tm_linear(C, W_r, c0, ncols, epilogue):
    S, TT = C.S, C.TT
    for gi, g0 in enumerate(range(0, ncols, 512)):
        gw = min(512, ncols - g0)
        wt, wkey, wslot = next_w(C)
        S.add("pool", lambda e, wt=wt, g0=g0, gw=gw: e.dma_start(
            out=wt[:, :, :gw], in_=W_r[:, :, c0 + g0:c0 + g0 + gw]), w=[wkey], slot=wslot)
        for tb in range(TT // 128):
            b = next_bank(C)
            for c in range(KC):
                S.add("pe", lambda e, wt=wt, c=c, tb=tb, gw=gw, b=b: e.matmul(
                    C.banks[b][:, :gw], C.xn[:, c, tb * 128:(tb + 1) * 128], wt[:, c, :gw], start=(c == 0), stop=(c == KC - 1)),
                    r=[wkey, ("xn", c)], w=[("ps", b)])
            epilogue(gi, tb, gw, b)


def headnorm(C, b, gname, scale, out_ap, out_key):
    S, TT = C.S, C.TT
    bank = C.banks[b]
    g = C.gv[gname]
    S.add("act", lambda e: e.activation(out=C.kf[:], in_=bank[:, :TT], func=AF.Copy, scale=float(scale)),
          r=[("ps", b)], w=[("kf",)])
    S.add("act", lambda e: e.activation(out=C.sqh[:], in_=bank[:, :TT], func=AF.Square), r=[("ps", b)], w=[("sqh",)])
    S.add("pe", lambda e: e.matmul(C.banks[1][:, :TT], C.ones[:], C.sqh[:], start=True, stop=True),
          r=[("sqh",), ("ones",)], w=[("ps", 1)])
    S.add("dve", lambda e: e.tensor_scalar(C.hr[:], C.banks[1][:, :TT], 1.0 / 128, EPS, ALU.mult, ALU.add),
          r=[("ps", 1)], w=[("hr",)])
    S.add("act", lambda e: e.activation(out=C.hr2[:], in_=C.hr[:], func=AF.Sqrt), r=[("hr",)], w=[("hr2",)])
    S.add("dve", lambda e: e.reciprocal(C.hr[:], C.hr2[:]), r=[("hr2",)], w=[("hr",)])
    S.add("dve", lambda e: e.scalar_tensor_tensor(out_ap, C.kf[:], g[:, 0:1], C.hr[:], ALU.mult, ALU.mult),
          r=[("kf",), ("hr",), ("v", gname)], w=[out_key])


def xn_key(c):
    return ("xn", c)


def emit_ple(C, xin, xout, pT, T, gname, wpe, wpg):
    S, TT = C.S, C.TT
    ctx_extra(C)
    if not hasattr(C, "pt"):
        C.pt = S.sb("pt", [128, 2, TT], BF16)
        C.wpe = [S.sb("wpe%d" % i, [128, 2, 512], BF16) for i in range(2)]
        C.pectr = 0
    wpg_r = wpg.rearrange("(c p) n -> p c n", p=128)
    wpe_r = wpe.rearrange("(c p) n -> p c n", p=128)
    pT_r = pT.rearrange("(c p) t -> p c t", p=128)
    xo_r = xout.rearrange("(c p) t -> p c t", p=128)
    for t0 in range(0, T, TT):
        emit_norm(C, xin, t0, gname)
        S.add("pool", lambda e, t0=t0: e.dma_start(out=C.pt[:], in_=pT_r[:, :, t0:t0 + TT]), w=[("pt",)], slot="pt")
        for ng in range(4):
            sl = C.pectr % 2
            C.pectr += 1
            wpet = C.wpe[sl]
            S.add("pool", lambda e, wpet=wpet, ng=ng: e.dma_start(out=wpet[:], in_=wpe_r[:, :, ng * 512:(ng + 1) * 512]),
                  w=[("wpe", sl)], slot="wpe%d" % sl)

            def epi(gi, j, nj, m, b, ng=ng, wpet=wpet, sl=sl):
                c = ng * 4 + j
                pb = 2 + (c % 2)
                for cc in range(2):
                    S.add("pe", lambda e, cc=cc: e.matmul(C.banks[pb][:, :TT], wpet[:, cc, j * 128:(j + 1) * 128], C.pt[:, cc, :],
                                                          start=(cc == 0), stop=(cc == 1)),
                          r=[("wpe", sl), ("pt",)], w=[("ps", pb)])
                sg = C.sg[c % 2]
                S.add("act", lambda e: e.activation(out=sg[:], in_=C.banks[b][:, :TT], func=AF.Sigmoid),
                      r=[("ps", b)], w=[("sg", c % 2)])
                S.add("dve", lambda e: e.tensor_tensor(sg[:], C.banks[pb][:, :TT], sg[:], ALU.mult),
                      r=[("ps", pb), ("sg", c % 2)], w=[("sg", c % 2)])
                S.add("dve", lambda e: e.tensor_tensor(C.xt[:, c, :], C.xt[:, c, :], sg[:], ALU.add),
                      r=[("sg", c % 2), ("xt",)], w=[("xt",)])
            fm_linear(C, wpg_r, ng * 512, 512, KC, C.xn, xn_key, epi)
        S.add("sp", lambda e, t0=t0: e.dma_start(out=xo_r[:, :, t0:t0 + TT], in_=C.xt[:]),
              r=[("xt",)], w=[("dram", "xout")], slot="xt_st")


def emit_proj_res(C, xin, xout, aT, bT, T, W, gth=None):
    S, TT = C.S, C.TT
    ctx_extra(C)
    W_r = W.rearrange("(c p) n -> p c n", p=128)
    x_r = xin.rearrange("(c p) t -> p c t", p=128)
    a_r = aT.rearrange("(c p) t -> p c t", p=128) if gth is None else None
    b_r = bT.rearrange("(c p) t -> p c t", p=128) if bT is not None else None
    xo_r = xout.rearrange("(c p) t -> p c t", p=128)
    for t0 in range(0, T, TT):
        S.add("sp", lambda e, t0=t0: e.dma_start(out=C.xt[:], in_=x_r[:, :, t0:t0 + TT]), w=[("xt",)], slot="xt")
        for g4 in range(4):
            ai = C.stctr["f"] % NSTF
            C.stctr["f"] += 1
            if gth is None:
                S.add("sp", lambda e, t0=t0, g4=g4, ai=ai: e.dma_start(out=C.stf[ai][:, :, :TT], in_=a_r[:, g4 * 4:(g4 + 1) * 4, t0:t0 + TT]),
                      w=[("stf", ai)], slot="stf%d" % ai)
            else:
                view, ixt, ixkey, cb, agk = gth
                for cc in range(4):
                    col = cb + (g4 * 4 + cc) * 4 + t0 // TT
                    S.add("pool", lambda e, cc=cc, ai=ai, col=col: e.indirect_dma_start(
                        out=C.stf[ai][:, cc, :TT], out_offset=None, in_=view,
                        in_offset=bass.IndirectOffsetOnAxis(ap=ixt[:, col:col + 1], axis=0)),
                        r=[ixkey] + list(agk), w=[("stf", ai)], slot="stf%d" % ai, group=True)
            if b_r is not None:
                bi_ = C.stctr["f"] % NSTF
                C.stctr["f"] += 1
                S.add("sp", lambda e, t0=t0, g4=g4, bi_=bi_: e.dma_start(out=C.stf[bi_][:, :, :TT], in_=b_r[:, g4 * 4:(g4 + 1) * 4, t0:t0 + TT]),
                      w=[("stf", bi_)], slot="stf%d" % bi_)
                for cc in range(4):
                    c = g4 * 4 + cc
                    S.add("dve", lambda e, c=c, cc=cc, ai=ai, bi_=bi_: e.tensor_tensor(C.xn[:, c, :], C.stf[ai][:, cc, :TT], C.stf[bi_][:, cc, :TT], ALU.mult),
                          r=[("stf", ai), ("stf", bi_)], w=[("xn", c)])
            else:
                for cc in range(4):
                    c = g4 * 4 + cc
                    if cc % 2 == 0:
                        S.add("dve", lambda e, c=c, cc=cc, ai=ai: e.tensor_copy(C.xn[:, c, :], C.stf[ai][:, cc, :TT]), r=[("stf", ai)], w=[("xn", c)])
                    else:
                        S.add("act", lambda e, c=c, cc=cc, ai=ai: e.activation(out=C.xn[:, c, :], in_=C.stf[ai][:, cc, :TT], func=AF.Copy), r=[("stf", ai)], w=[("xn", c)])

        def epi(gi, j, nj, m, b):
            c = gi * 4 + j
            S.add("dve", lambda e: e.tensor_tensor(C.xt[:, c, :], C.banks[b][:, :TT], C.xt[:, c, :], ALU.add),
                  r=[("ps", b), ("xt",)], w=[("xt",)])
        fm_linear(C, W_r, 0, D, KC, C.xn, xn_key, epi)
        S.add("sp", lambda e, t0=t0: e.dma_start(out=xo_r[:, :, t0:t0 + TT], in_=C.xt[:]),
              r=[("xt",)], w=[("dram", "xout")], slot="xt_st")


def emit_mlstm_in(C, xin, T, gname, w_in, qT, kT, k_tm, v_tm, sog, gates, agf=None):
    S, TT = C.S, C.TT
    pending = []
    tkeys = {}
    ctx_extra(C)
    W_r = w_in.rearrange("(c p) n -> p c n", p=128)
    qT_r = qT.rearrange("(c p) t -> p c t", p=128)
    kT_r = kT.rearrange("(c p) t -> p c t", p=128)
    sog_r = sog.rearrange("(c p) t -> p c t", p=128)
    ktm_r = k_tm.rearrange("(b p) n -> p b n", p=128)
    vtm_r = v_tm.rearrange("(b p) n -> p b n", p=128)
    if not hasattr(C, "gst"):
        C.gst = S.sb("gst", [8, TT], F32)
    for t0 in range(0, T, TT):
        emit_norm(C, xin, t0, gname)
        cur = {}

        def fm_epi(kind, dst_r, scale, func):
            def epi(gi, j, nj, m, b):
                if j == 0:
                    cur["st"] = next_stage(C, kind)
                st, skey, sslot = cur["st"]
                S.add("act", lambda e: e.activation(out=st[:, j, :TT], in_=C.banks[b][:, :TT], func=func, scale=float(scale)),
                      r=[("ps", b)], w=[skey])
                if j == nj - 1:
                    S.add("sp", lambda e, t0=t0: e.dma_start(out=dst_r[:, gi * 4:gi * 4 + nj, t0:t0 + TT], in_=st[:, :nj, :TT]),
                          r=[skey], w=[("dram", "o", S.uid())], slot=sslot)
            return epi
        fm_linear(C, W_r, 0, 1024, KC, C.xn, xn_key, fm_epi("b", qT_r, 256 ** -0.5, AF.Copy))
        for fn in pending:
            fn()
        del pending[:]
        fm_linear(C, W_r, 1024, 1024, KC, C.xn, xn_key, fm_epi("b", kT_r, 1.0, AF.Copy))
        fm_linear(C, W_r, 4096, 2048, KC, C.xn, xn_key, fm_epi("f", sog_r, 1.0, AF.Sigmoid))

        def g_epi(gi, j, nj, m, b):
            S.add("act", lambda e: e.activation(out=C.gst[:, :], in_=C.banks[b][:8, :TT], func=AF.Copy),
                  r=[("ps", b)], w=[("gst",)])
            S.add("sp", lambda e, t0=t0: e.dma_start(out=gates[:, t0:t0 + TT], in_=C.gst[:, :]), r=[("gst",)], w=[("dram", "o", S.uid())], slot="gst")
        fm_linear(C, W_r, 6144, 8, KC, C.xn, xn_key, g_epi)

        def tm_epi(dst_r, name):
            def epi(gi, tb, gw, b):
                if tb == 0:
                    cur["st"] = next_stage(C, "b")
                st, skey, sslot = cur["st"]
                eng = "dve" if tb % 2 == 0 else "act"
                if eng == "dve":
                    S.add("dve", lambda e: e.tensor_copy(st[:, tb, :gw], C.banks[b][:, :gw]), r=[("ps", b)], w=[skey])
                else:
                    S.add("act", lambda e: e.activation(out=st[:, tb, :gw], in_=C.banks[b][:, :gw], func=AF.Copy), r=[("ps", b)], w=[skey])
                if tb == TT // 128 - 1:
                    tb0 = t0 // 128
                    dkey = ("dram", "o", S.uid())
                    tkeys.setdefault((name, t0), []).append(dkey)
                    S.add("sp", lambda e: e.dma_start(out=dst_r[:, tb0:tb0 + TT // 128, gi * 512:gi * 512 + gw], in_=st[:, :TT // 128, :gw]),
                          r=[skey], w=[dkey], slot=sslot)
            return epi
        tm_linear(C, W_r, 1024, 1024, tm_epi(ktm_r, "ktm"))
        tm_linear(C, W_r, 2048, 2048, tm_epi(vtm_r, "vtm"))
        if agf is not None:
            ti = t0 // TT
            pending.append(lambda ti=ti, t0=t0: agf(S, "ktm", 512, [ti], tkeys[("ktm", t0)]))
            pending.append(lambda ti=ti, t0=t0: agf(S, "vtm", 256, [2 * ti, 2 * ti + 1], tkeys[("vtm", t0)]))
    for fn in pending:
        fn()


def emit_headproj(C, xin, T, gname, W, c0, hgname, scale, outT):
    S, TT = C.S, C.TT
    ctx_extra(C)
    W_r = W.rearrange("(c p) n -> p c n", p=128)
    o_r = outT.rearrange("(c p) t -> p c t", p=128)
    cur = {}
    for t0 in range(0, T, TT):
        emit_norm(C, xin, t0, gname)

        def epi(gi, j, nj, m, b):
            if j == 0:
                cur["st"] = next_stage(C, "b")
            st, skey, sslot = cur["st"]
            headnorm(C, b, hgname, scale, st[:, j, :TT], skey)
            if j == nj - 1:
                S.add("sp", lambda e, t0=t0: e.dma_start(out=o_r[:, gi * 4:gi * 4 + nj, t0:t0 + TT], in_=st[:, :nj, :TT]),
                      r=[skey], w=[("dram", "o", S.uid())], slot=sslot)
        fm_linear(C, W_r, c0, 2048, KC, C.xn, xn_key, epi)
        yield t0


def emit_kv(C, xin, T, gname, w_kv, kT, v_tm, agf=None):
    S, TT = C.S, C.TT
    ctx_extra(C)
    W_r = w_kv.rearrange("(c p) n -> p c n", p=128)
    vtm_r = v_tm.rearrange("(b p) n -> p b n", p=128)
    cur = {}
    tkeys = {}
    pend = []
    for t0 in emit_headproj(C, xin, T, gname, w_kv, 0, "g_k", 1.0, kT):
        if agf is not None and t0 > 0:
            for fn in pend:
                fn()
            del pend[:]
        def epi(gi, tb, gw, b):
            if tb == 0:
                cur["st"] = next_stage(C, "b")
            st, skey, sslot = cur["st"]
            if tb % 2 == 0:
                S.add("dve", lambda e: e.tensor_copy(st[:, tb, :gw], C.banks[b][:, :gw]), r=[("ps", b)], w=[skey])
            else:
                S.add("act", lambda e: e.activation(out=st[:, tb, :gw], in_=C.banks[b][:, :gw], func=AF.Copy), r=[("ps", b)], w=[skey])
            if tb == TT // 128 - 1:
                tb0 = t0 // 128
                dkey = ("dram", "o", S.uid())
                tkeys.setdefault(t0, []).append(dkey)
                S.add("sp", lambda e: e.dma_start(out=vtm_r[:, tb0:tb0 + TT // 128, gi * 512:gi * 512 + gw], in_=st[:, :TT // 128, :gw]),
                      r=[skey], w=[dkey], slot=sslot)
        tm_linear(C, W_r, 2048, 2048, epi)
        if agf is not None:
            ti = t0 // TT
            pend.append(lambda ti=ti, t0=t0: agf(S, "v", 256, [2 * ti, 2 * ti + 1], tkeys[t0]))
    for fn in pend:
        fn()


def emit_mlstm_core(S, SEQ, qT, kT, k_tm, v_tm, gi, gf, bif, ghead, hout, SEG=1024, dbg=None, gth=None, out_ag=None):
    nc = S.nc
    CH = 128
    NCH = SEG // CH
    DK, DV = 256, 512
    sb, ps = S.sb, S.ps
    banks = [ps("bank%d" % i, [128, 512], F32) for i in range(8)]
    ones_bf = sb("ones_bf", [128, 128], BF16)
    ones_f = sb("ones_f", [128, 128], F32)
    tri = sb("tri", [128, 128], F32)
    S.add("dve", lambda e: e.memset(ones_bf[:], 1.0), w=["ones_bf"])
    S.add("dve", lambda e: e.memset(ones_f[:], 1.0), w=["ones_f"])
    S.add("pool", lambda e: e.memset(tri[:], 1.0), w=["tri"])
    S.add("pool", lambda e: e.affine_select(out=tri[:], in_=tri[:], pattern=[[1, 128]], compare_op=ALU.is_ge, fill=0.0,
                                            base=0, channel_multiplier=-1), r=["tri"], w=["tri"])
    bt = sb("bif_sb", [1, 2], F32)
    S.add("sp", lambda e: e.dma_start(out=bt[:], in_=bif), w=["bif"], slot="bif")
    nbf = sb("nbf", [1, 1], F32)
    S.add("dve", lambda e: e.tensor_scalar(nbf[:], bt[0:1, 1:2], -1.0, None, ALU.mult), r=["bif"], w=["nbf"])
    gh = sb("gh_sb", [128, 4], F32)
    S.add("sp", lambda e: e.dma_start(out=gh[:], in_=ghead), w=["gh"], slot="gh")
    rows = {n: sb("r_" + n, [1, SEG], F32) for n in ("gi", "gf", "e", "lf", "Bn", "U", "G", "nG", "wi", "cl", "we", "one")}
    S.add("dve", lambda e: e.memset(rows["one"][:], 1.0), w=["r_one"])
    carry = sb("carry", [1, 4], F32)
    S.add("dve", lambda e: e.memset(carry[:], 0.0), w=["carry"])
    bc = {n: sb("bc_" + n, [128, SEG], F32) for n in ("nG", "wi", "cl")}
    cols = sb("cols", [128, 3 * NCH], F32)
    qt = [sb("qt%d" % i, [128, 2, SEG], BF16) for i in range(2)]
    kt = [sb("kt%d" % i, [128, 2, SEG], BF16) for i in range(2)]
    ktm = [sb("ktm%d" % i, [128, NCH, DK], BF16) for i in range(2)]
    vtm = [sb("vtm%d" % i, [128, NCH, DV], BF16) for i in range(2)]
    hst = [sb("hst%d" % i, [128, 4, SEG], F32) for i in range(2)]
    PT = sb("PT", [128, 128], F32)
    PTm = sb("PTm", [128, 128], F32)
    AT = sb("AT", [128, 128], BF16)
    qw = sb("qw", [128, 2, 128], BF16)
    dd = sb("dd", [128, 128], F32)
    rd = sb("rd", [128, 128], F32)
    hT = sb("hT", [128, 4, 128], F32)
    hsq = sb("hsq", [128, 4, 128], BF16)
    hr = sb("hr", [128, 128], F32)
    hr2 = sb("hr2", [128, 128], F32)
    kw = sb("kw", [128, DK], BF16)
    Cf = sb("Cf", [128, 2, DV], F32)
    Cb = sb("Cb", [128, 2, DV], BF16)
    nf = sb("nf", [128, 2], F32)
    nrep = sb("nrep", [128, 2, 128], BF16)
    S.add("dve", lambda e: e.memset(Cf[:], 0.0), w=["Cf"])
    S.add("dve", lambda e: e.memset(nf[:], 0.0), w=["nf"])
    S.add("pool", lambda e: e.memset(Cb[:], 0.0), w=["Cb"])
    S.add("pool", lambda e: e.memset(nrep[:], 0.0), w=["nrep"])
    ho_r = hout.rearrange("(c p) t -> p c t", p=128) if out_ag is None else None
    R = rows
    ag_pending = []
    if gth is None:
        qT_r = qT.rearrange("(c p) t -> p c t", p=128)
        kT_r = kT.rearrange("(c p) t -> p c t", p=128)
        ktm_r = k_tm.rearrange("(b p) n -> p b n", p=128)
        vtm_r = v_tm.rearrange("(b p) n -> p b n", p=128)
    else:
        g8 = sb("g8", [1, 8, SEG], F32)
        oh = sb("oh", [1, 4], F32)
        S.add("sp", lambda e: e.dma_start(out=oh[:], in_=gth["onehot"]), w=["oh"], slot="oh")
    for sg in range(SEQ // SEG):
        t0 = sg * SEG
        sl = sg % 2
        if gth is None:
            for dk in range(2):
                S.add("sp", lambda e, t0=t0, sl=sl, dk=dk: e.dma_start(out=qt[sl][:, dk, :], in_=qT_r[:, dk, t0:t0 + SEG]), w=[("qt", sl, dk)], slot="qt%d_%d" % (sl, dk))
                S.add("sp", lambda e, t0=t0, sl=sl, dk=dk: e.dma_start(out=kt[sl][:, dk, :], in_=kT_r[:, dk, t0:t0 + SEG]), w=[("kt", sl, dk)], slot="kt%d_%d" % (sl, dk))
            for c in range(NCH):
                S.add("sp", lambda e, t0=t0, sl=sl, c=c: e.dma_start(out=ktm[sl][:, c, :], in_=ktm_r[:, t0 // 128 + c, :]), w=[("ktm", sl, c)], slot="ktm%d_%d" % (sl, c))
                S.add("sp", lambda e, t0=t0, sl=sl, c=c: e.dma_start(out=vtm[sl][:, c, :], in_=vtm_r[:, t0 // 128 + c, :]), w=[("vtm", sl, c)], slot="vtm%d_%d" % (sl, c))
            S.add("sp", lambda e, t0=t0: e.dma_start(out=R["gi"][:], in_=gi[:, t0:t0 + SEG]), w=["r_gi"], slot="r_gi")
            S.add("sp", lambda e, t0=t0: e.dma_start(out=R["gf"][:], in_=gf[:, t0:t0 + SEG]), w=["r_gf"], slot="r_gf")
        else:
            ixt, ixkey = gth["ixt"], gth["ixkey"]
            for dk in range(2):
                col = gth["MQ"] + sg * 2 + dk
                S.add("pool", lambda e, sl=sl, dk=dk, col=col: e.indirect_dma_start(
                    out=qt[sl][:, dk, :], out_offset=None, in_=gth["q_view"],
                    in_offset=bass.IndirectOffsetOnAxis(ap=ixt[:, col:col + 1], axis=0)),
                    r=[ixkey] + list(gth["agk"]["q"]), w=[("qt", sl, dk)], slot="qt%d_%d" % (sl, dk))
                S.add("pool", lambda e, sl=sl, dk=dk, col=col: e.indirect_dma_start(
                    out=kt[sl][:, dk, :], out_offset=None, in_=gth["k_view"],
                    in_offset=bass.IndirectOffsetOnAxis(ap=ixt[:, col:col + 1], axis=0)),
                    r=[ixkey] + list(gth["agk"]["k"]), w=[("kt", sl, dk)], slot="kt%d_%d" % (sl, dk))
            for c in range(NCH):
                col = gth["MK"] + sg * NCH + c
                colv = gth["MV"] + sg * NCH + c
                S.add("pool", lambda e, sl=sl, c=c, col=col: e.indirect_dma_start(
                    out=ktm[sl][:, c, :], out_offset=None, in_=gth["ktm_view"],
                    in_offset=bass.IndirectOffsetOnAxis(ap=ixt[:, col:col + 1], axis=0)),
                    r=[ixkey] + list(gth["agk"]["ktm"]), w=[("ktm", sl, c)], slot="ktm%d_%d" % (sl, c))
                S.add("pool", lambda e, sl=sl, c=c, colv=colv: e.indirect_dma_start(
                    out=vtm[sl][:, c, :], out_offset=None, in_=gth["vtm_view"],
                    in_offset=bass.IndirectOffsetOnAxis(ap=ixt[:, colv:colv + 1], axis=0)),
                    r=[ixkey] + list(gth["agk"]["vtm"]), w=[("vtm", sl, c)], slot="vtm%d_%d" % (sl, c))
            srank, half = sg // 2, sg % 2
            gsrc = gth["gates_all"][srank * 8:(srank + 1) * 8, half * SEG:(half + 1) * SEG].rearrange("(o r) t -> o r t", o=1)
            S.add("sp", lambda e, gsrc=gsrc: e.dma_start(out=g8[:], in_=gsrc), r=list(gth["agk"]["g"]), w=["g8"], slot="g8")
            for gi_, (dst, off) in enumerate((("gi", 0), ("gf", 4))):
                S.add("dve", lambda e, dst=dst, off=off: e.tensor_scalar(R[dst][:], g8[0:1, off, :], oh[0:1, 0:1], None, ALU.mult),
                      r=["g8", "oh"], w=["r_" + dst])
                for j in range(1, 4):
                    S.add("dve", lambda e, dst=dst, off=off, j=j: e.scalar_tensor_tensor(R[dst][:], g8[0:1, off + j, :], oh[0:1, j:j + 1], R[dst][:], ALU.mult, ALU.add),
                          r=["g8", "oh", "r_" + dst], w=["r_" + dst])
        for fn in ag_pending:
            fn()
        del ag_pending[:]
        S.add("act", lambda e: e.activation(out=R["e"][:], in_=R["gf"][:], func=AF.Exp, scale=-1.0, bias=nbf[0:1, 0:1]),
              r=["r_gf", "nbf"], w=["r_e"])
        S.add("act", lambda e: e.activation(out=R["lf"][:], in_=R["e"][:], func=AF.Ln, bias=ones_f[0:1, 0:1]),
              r=["r_e", "ones_f"], w=["r_lf"])
        S.add("dve", lambda e: e.tensor_tensor_scan(R["Bn"][:], R["one"][:], R["lf"][:], carry[0:1, 0:1], ALU.mult, ALU.add),
              r=["r_lf", "carry", "r_one"], w=["r_Bn"])
        S.add("dve", lambda e: e.scalar_tensor_tensor(R["U"][:], R["gi"][:], bt[0:1, 0:1], R["Bn"][:], ALU.add, ALU.add),
              r=["r_gi", "bif", "r_Bn"], w=["r_U"])
        S.add("dve", lambda e: e.tensor_tensor_scan(R["G"][:], R["U"][:], R["U"][:], carry[0:1, 1:2], ALU.max, ALU.max),
              r=["r_U", "carry"], w=["r_G"])
        S.add("dve", lambda e: e.tensor_scalar(R["nG"][:], R["G"][:], -1.0, None, ALU.mult), r=["r_G"], w=["r_nG"])
        S.add("dve", lambda e: e.tensor_tensor(R["cl"][:], R["Bn"][:], R["G"][:], ALU.subtract), r=["r_Bn", "r_G"], w=["r_cl"])
        S.add("act", lambda e: e.activation(out=R["cl"][:], in_=R["cl"][:], func=AF.Exp), r=["r_cl"], w=["r_cl"])
        for c in range(NCH):
            a, b_ = c * CH, (c + 1) * CH
            gprev = carry[0:1, 1:2] if c == 0 else R["G"][0:1, a - 1:a]
            S.add("act", lambda e, a=a, b_=b_, gprev=gprev: e.activation(out=R["wi"][0:1, a:b_], in_=R["nG"][0:1, a:b_], func=AF.Exp, bias=gprev),
                  r=["r_nG", "r_G", "carry"], w=["r_wi"])
            S.add("act", lambda e, a=a, b_=b_: e.activation(out=R["we"][0:1, a:b_], in_=R["U"][0:1, a:b_], func=AF.Exp, bias=R["nG"][0:1, b_ - 1:b_]),
                  r=["r_nG", "r_U"], w=["r_we"])
        for n in ("nG", "wi", "cl"):
            for h in range(SEG // 512):
                S.add("pe", lambda e, n=n, h=h: e.matmul(banks[7][:, :], ones_f[0:1, :], R[n][0:1, h * 512:(h + 1) * 512], start=True, stop=True),
                      r=["r_" + n, "ones_f"], w=[("ps", 7)])
                S.add("act", lambda e, n=n, h=h: e.activation(out=bc[n][:, h * 512:(h + 1) * 512], in_=banks[7][:, :], func=AF.Copy),
                      r=[("ps", 7)], w=["bc_" + n])
        for c in range(NCH):
            a, b_ = c * CH, (c + 1) * CH
            S.add("pe", lambda e, c=c, a=a, b_=b_: e.matmul(banks[7][:, c:c + 1], R["U"][0:1, a:b_], ones_f[0:1, 0:1], start=True, stop=True),
                  r=["r_U", "ones_f"], w=[("ps", 7)])
            S.add("pe", lambda e, c=c, a=a, b_=b_: e.matmul(banks[7][:, NCH + c:NCH + c + 1], R["we"][0:1, a:b_], ones_f[0:1, 0:1], start=True, stop=True),
                  r=["r_we", "ones_f"], w=[("ps", 7)])
            S.add("pe", lambda e, c=c, b_=b_: e.matmul(banks[7][:, 2 * NCH + c:2 * NCH + c + 1], ones_f[0:1, :], R["wi"][0:1, b_ - 1:b_], start=True, stop=True),
                  r=["r_wi", "ones_f"], w=[("ps", 7)])
        S.add("dve", lambda e: e.tensor_copy(cols[:], banks[7][:, :3 * NCH]), r=[("ps", 7)], w=["cols"])
        S.add("dve", lambda e: e.tensor_copy(carry[0:1, 0:1], R["Bn"][0:1, SEG - 1:SEG]), r=["r_Bn", "r_wi", "r_we"], w=["carry"])
        S.add("dve", lambda e: e.tensor_copy(carry[0:1, 1:2], R["G"][0:1, SEG - 1:SEG]), r=["r_G", "r_wi", "r_we"], w=["carry"])
        if dbg is not None and sg == 0:
            S.add("sp", lambda e: e.dma_start(out=dbg["nG"], in_=bc["nG"][:]), r=["bc_nG"], slot="dbg0")
            S.add("sp", lambda e: e.dma_start(out=dbg["wi"], in_=bc["wi"][:]), r=["bc_wi"], slot="dbg1")
            S.add("sp", lambda e: e.dma_start(out=dbg["cl"], in_=bc["cl"][:]), r=["bc_cl"], slot="dbg2")
            S.add("sp", lambda e: e.dma_start(out=dbg["cols"], in_=cols[:]), r=["cols"], slot="dbg3")
            S.add("sp", lambda e: e.dma_start(out=dbg["tri"], in_=tri[:]), r=["tri"], slot="dbg4")
        for c in range(NCH):
            a, b_ = c * CH, (c + 1) * CH
            qs, ks, kms, vs = qt[sl], kt[sl], ktm[sl], vtm[sl]
            for dk in range(2):
                S.add("pe", lambda e, dk=dk, a=a, b_=b_, ks=ks, qs=qs: e.matmul(banks[0][:, :128], ks[:, dk, a:b_], qs[:, dk, a:b_], start=(dk == 0), stop=(dk == 1)),
                      r=[("kt", sl, dk), ("qt", sl, dk)], w=[("ps", 0)])
            S.add("act", lambda e, a=a, b_=b_, c=c: e.activation(out=PT[:], in_=bc["nG"][:, a:b_], func=AF.Exp, bias=cols[:, c:c + 1]),
                  r=["bc_nG", "cols"], w=["PT"])
            S.add(PENG, lambda e: e.tensor_tensor(PTm[:], PT[:], tri[:], ALU.mult), r=["PT", "tri"], w=["PTm"])
            S.add("dve", lambda e: e.tensor_tensor(AT[:], banks[0][:, :128], PTm[:], ALU.mult), r=[("ps", 0), "PTm"], w=["AT"])
            for dk in range(2):
                S.add("dve", lambda e, dk=dk, a=a, b_=b_, qs=qs: e.tensor_tensor(qw[:, dk, :], qs[:, dk, a:b_], bc["wi"][:, a:b_], ALU.mult),
                      r=[("qt", sl, dk), "bc_wi"], w=["qw"])
            for j in range(4):
                S.add("pe", lambda e, j=j, c=c, vs=vs: e.matmul(banks[1][:, j * 128:(j + 1) * 128], vs[:, c, j * 128:(j + 1) * 128], AT[:], start=True, stop=False),
                      r=[("vtm", sl, c), "AT"], w=[("ps", 1)])
                for dk in range(2):
                    S.add("pe", lambda e, j=j, dk=dk: e.matmul(banks[1][:, j * 128:(j + 1) * 128], Cb[:, dk, j * 128:(j + 1) * 128], qw[:, dk, :], start=False, stop=(dk == 1)),
                          r=["Cb", "qw"], w=[("ps", 1)])
            S.add("pe", lambda e: e.matmul(banks[2][:, :128], ones_bf[:], AT[:], start=True, stop=False), r=["ones_bf", "AT"], w=[("ps", 2)])
            for dk in range(2):
                S.add("pe", lambda e, dk=dk: e.matmul(banks[2][:, :128], nrep[:, dk, :], qw[:, dk, :], start=False, stop=(dk == 1)),
                      r=["nrep", "qw"], w=[("ps", 2)])
            S.add("act", lambda e: e.activation(out=dd[:], in_=banks[2][:, :128], func=AF.Abs), r=[("ps", 2)], w=["dd"])
            S.add("dve", lambda e, a=a, b_=b_: e.tensor_tensor(dd[:], dd[:], bc["cl"][:, a:b_], ALU.max), r=["dd", "bc_cl"], w=["dd"])
            S.add("dve", lambda e: e.reciprocal(rd[:], dd[:]), r=["dd"], w=["rd"])
            for j in range(4):
                S.add("dve", lambda e, j=j: e.tensor_tensor(hT[:, j, :], banks[1][:, j * 128:(j + 1) * 128], rd[:], ALU.mult),
                      r=[("ps", 1), "rd"], w=["hT"])
            S.add("act", lambda e: e.activation(out=hsq[:], in_=hT[:], func=AF.Square), r=["hT"], w=["hsq"])
            for j in range(4):
                S.add("pe", lambda e, j=j: e.matmul(banks[3][:, :128], ones_bf[:], hsq[:, j, :], start=(j == 0), stop=(j == 3)),
                      r=["ones_bf", "hsq"], w=[("ps", 3)])
            S.add("dve", lambda e: e.tensor_scalar(hr[:], banks[3][:, :128], 1.0 / DV, EPS, ALU.mult, ALU.add), r=[("ps", 3)], w=["hr"])
            S.add("act", lambda e: e.activation(out=hr2[:], in_=hr[:], func=AF.Sqrt), r=["hr"], w=["hr2"])
            S.add("dve", lambda e: e.reciprocal(hr[:], hr2[:]), r=["hr2"], w=["hr"])
            for j in range(4):
                S.add("dve", lambda e, j=j, a=a, b_=b_, sl=sl: e.scalar_tensor_tensor(hst[sl][:, j, a:b_], hT[:, j, :], gh[:, j:j + 1], hr[:], ALU.mult, ALU.mult),
                      r=["hT", "hr", "gh"], w=[("hst", sl)])
            S.add("dve", lambda e, c=c, kms=kms: e.tensor_scalar(kw[:], kms[:, c, :], cols[:, NCH + c:NCH + c + 1], None, ALU.mult),
                  r=[("ktm", sl, c), "cols"], w=["kw"])
            for dk in range(2):
                S.add("pe", lambda e, dk=dk, c=c, vs=vs: e.matmul(banks[4 + dk][:, :], kw[:, dk * 128:(dk + 1) * 128], vs[:, c, :], start=True, stop=True),
                      r=["kw", ("vtm", sl, c)], w=[("ps", 4 + dk)])
                S.add("pe", lambda e, dk=dk: e.matmul(banks[6][:, dk:dk + 1], kw[:, dk * 128:(dk + 1) * 128], ones_bf[:, 0:1], start=True, stop=True),
                      r=["kw", "ones_bf"], w=[("ps", 6)])
            for dk in range(2):
                S.add("dve", lambda e, dk=dk, c=c: e.scalar_tensor_tensor(Cf[:, dk, :], Cf[:, dk, :], cols[:, 2 * NCH + c:2 * NCH + c + 1], banks[4 + dk][:, :], ALU.mult, ALU.add),
                      r=["Cf", "cols", ("ps", 4 + dk), "Cb"], w=["Cf"])
            S.add("dve", lambda e, c=c: e.scalar_tensor_tensor(nf[:], nf[:], cols[:, 2 * NCH + c:2 * NCH + c + 1], banks[6][:, 0:2], ALU.mult, ALU.add),
                  r=["nf", "cols", ("ps", 6)], w=["nf"])
            S.add("act", lambda e: e.activation(out=Cb[:], in_=Cf[:], func=AF.Copy), r=["Cf"], w=["Cb"])
            for dk in range(2):
                S.add(PENG, lambda e, dk=dk: e.tensor_scalar(nrep[:, dk, :], ones_f[:], nf[:, dk:dk + 1], None, ALU.mult),
                      r=["nf", "ones_f"], w=["nrep"])
        if out_ag is None:
            S.add("sp", lambda e, t0=t0, sl=sl: e.dma_start(out=ho_r[:, :, t0:t0 + SEG], in_=hst[sl][:]), r=[("hst", sl)], w=[("dram", "ho", sg)], slot="hst%d" % sl)
        else:
            dst = out_ag["h_b"][sg * 512:(sg + 1) * 512, :].rearrange("(c p) t -> p c t", p=128)
            S.add("sp", lambda e, dst=dst, sl=sl: e.dma_start(out=dst, in_=hst[sl][:]), r=[("hst", sl)], w=[("dram", "ho", sg)], slot="hst%d" % sl)
            ag_pending.append(lambda sg=sg: out_ag["agf"](S, "a", 256, [2 * sg, 2 * sg + 1], [("dram", "ho", sg)]))
    for fn in ag_pending:
        fn()


def build_mlstm_core(SEQ, debug=False):
    nc = bass.Bass("TRN2", target_bir_lowering=False)
    qT = nc.dram_tensor("qT", [256, SEQ], BF16, kind="ExternalInput").ap()
    kT = nc.dram_tensor("kT", [256, SEQ], BF16, kind="ExternalInput").ap()
    k_tm = nc.dram_tensor("k_tm", [SEQ, 256], BF16, kind="ExternalInput").ap()
    v_tm = nc.dram_tensor("v_tm", [SEQ, 512], BF16, kind="ExternalInput").ap()
    gi = nc.dram_tensor("gi", [1, SEQ], F32, kind="ExternalInput").ap()
    gf = nc.dram_tensor("gf", [1, SEQ], F32, kind="ExternalInput").ap()
    bif = nc.dram_tensor("bif", [1, 2], F32, kind="ExternalInput").ap()
    ghead = nc.dram_tensor("ghead", [128, 4], F32, kind="ExternalInput").ap()
    hout = nc.dram_tensor("hout", [512, SEQ], F32, kind="ExternalOutput").ap()
    S = Sched(nc)
    dbg = None
    if debug:
        dbg = {n: nc.dram_tensor("dbg_" + n, [128, 1024], F32, kind="ExternalOutput").ap() for n in ("nG", "wi", "cl")}
        dbg["cols"] = nc.dram_tensor("dbg_cols", [128, 24], F32, kind="ExternalOutput").ap()
        dbg["tri"] = nc.dram_tensor("dbg_tri", [128, 128], F32, kind="ExternalOutput").ap()
    emit_mlstm_core(S, SEQ, qT, kT, k_tm, v_tm, gi, gf, bif, ghead, hout, dbg=dbg)
    S.emit()
    return nc, S


def emit_sb_core(S, NH, SEQ, qT, kT, v_tm, oT, gth=None, out_ag=None):
    sb, ps = S.sb, S.ps
    NB = SEQ // 128
    NQB = SEQ // 512
    zb = [ps("zb%d" % i, [128, 512], F32) for i in range(2)]
    ab = [ps("ab%d" % i, [128, 512], F32) for i in range(2)]
    ob = [ps("ob%d" % i, [128, 512], F32) for i in range(2)]
    ones_f = sb("ones_f", [128, 128], F32)
    ntri = sb("ntri", [128, 128], BF16)
    nones = sb("nones", [128, 128], BF16)
    smask = sb("smask", [128, 128], BF16)
    tmpf = sb("tmpf", [128, 128], F32)
    S.add("dve", lambda e: e.memset(ones_f[:], 1.0), w=["ones_f"])
    S.add("dve", lambda e: e.memset(nones[:], -1.0), w=["nones"])
    S.add("pool", lambda e: e.memset(tmpf[:], -1.0), w=["tmpf"])
    S.add("pool", lambda e: e.affine_select(out=tmpf[:], in_=tmpf[:], pattern=[[-1, 128]], compare_op=ALU.is_ge, fill=0.0,
                                            base=0, channel_multiplier=1), r=["tmpf"], w=["tmpf"])
    S.add("dve", lambda e: e.tensor_copy(ntri[:], tmpf[:]), r=["tmpf"], w=["ntri"])
    tmpg = sb("tmpg", [128, 128], F32)
    S.add("pool", lambda e: e.memset(tmpg[:], 1.0), w=["tmpg"])
    S.add("pool", lambda e: e.affine_select(out=tmpg[:], in_=tmpg[:], pattern=[[1, 128]], compare_op=ALU.is_ge, fill=0.0,
                                            base=-1, channel_multiplier=-1), r=["tmpg"], w=["tmpg"])
    S.add("dve", lambda e: e.tensor_copy(smask[:], tmpg[:]), r=["tmpg"], w=["smask"])
    qhs = [sb("qh%d" % i, [128, SEQ], BF16) for i in range(2)]
    khs = [sb("kh%d" % i, [128, SEQ], BF16) for i in range(2)]
    vhs = [sb("vh%d" % i, [128, NB, 128], BF16) for i in range(2)]
    KBATCH = 12
    NBUF = 2 * KBATCH + 2
    e_sb = [sb("e_sb%d" % i, [128, 512], F32) for i in range(2)] if not USE_SOFTPLUS else None
    sp_bf = [sb("sp_bf%d" % i, [128, 512], BF16) for i in range(NBUF)]
    A_bf = [sb("A_bf%d" % i, [128, 512], BF16) for i in range(NBUF)]
    RS_f = sb("RS_f", [128, 512], F32)
    RS_b = [sb("RS_b%d" % i, [128, 512], BF16) for i in range(NBUF)]
    ost = [sb("ost%d" % i, [128, 512], F32) for i in range(2)]
    TC = SEQ // 4

    its = []
    for h in range(NH):
        for QB in range(NQB):
            kb_hi = 4 * QB + 3
            for kb in range(kb_hi, -1, -1):
                its.append((h, QB, kb))

    def geom(n):
        h, QB, kb = its[n]
        r_ = kb - 4 * QB
        col0 = r_ * 128 if r_ >= 0 else 0
        return h, QB, kb, r_, col0, 512 - col0, kb == 4 * QB + 3, QB * 512

    def load_head(h):
        hp = h % 2
        qh, kh, vh = qhs[hp], khs[hp], vhs[hp]
        if gth is None:
            S.add("sp", lambda e, h=h: e.dma_start(out=qh[:], in_=qT[h]), w=[("qh", hp, i) for i in range(4)], slot="qh%d" % hp)
            S.add("sp", lambda e, h=h: e.dma_start(out=kh[:], in_=kT[h]), w=[("kh", hp, i) for i in range(4)], slot="kh%d" % hp)
            S.add("sp", lambda e, h=h: e.dma_start(out=vh[:], in_=v_tm[h].rearrange("(b p) d -> p b d", p=128)),
                  w=[("vh", hp, i) for i in range((NB + 7) // 8)], slot="vh%d" % hp)
        else:
            ixt, ixkey = gth["ixt"], gth["ixkey"]
            for s_ in range(4):
                col = gth["SQ"] + h * 4 + s_
                S.add("pool", lambda e, s_=s_, col=col: e.indirect_dma_start(
                    out=qh[:, s_ * TC:(s_ + 1) * TC], out_offset=None, in_=gth["q_view"],
                    in_offset=bass.IndirectOffsetOnAxis(ap=ixt[:, col:col + 1], axis=0)),
                    r=[ixkey] + list(gth["agk"]["q"]), w=[("qh", hp, s_)], slot="qh%d_%d" % (hp, s_))
                S.add("pool", lambda e, s_=s_, col=col: e.indirect_dma_start(
                    out=kh[:, s_ * TC:(s_ + 1) * TC], out_offset=None, in_=gth["k_view"],
                    in_offset=bass.IndirectOffsetOnAxis(ap=ixt[:, col:col + 1], axis=0)),
                    r=[ixkey] + list(gth["agk"]["k"]), w=[("kh", hp, s_)], slot="kh%d_%d" % (hp, s_))
            for blk in range(NB):
                col = gth["SV"] + h * NB + blk
                S.add("pool", lambda e, blk=blk, col=col: e.indirect_dma_start(
                    out=vh[:, blk, :], out_offset=None, in_=gth["v_view"],
                    in_offset=bass.IndirectOffsetOnAxis(ap=ixt[:, col:col + 1], axis=0)),
                    r=[ixkey] + list(gth["agk"]["v"]), w=[("vh", hp, blk // 8)], slot="vh%d_%d" % (hp, blk // 8), group=True)

    def part_a(n):
        h, QB, kb, r_, col0, ncol, first, q0 = geom(n)
        if n == 0:
            load_head(0)
        if (n == 0 or its[n - 1][0] != h) and h + 1 < NH:
            load_head(h + 1)
        i2, i3 = n % 2, n % NBUF
        hp = h % 2
        kblk = khs[hp][:, kb * 128:(kb + 1) * 128]
        qcols = qhs[hp][:, q0 + col0:q0 + 512]
        khk, qhk = ("kh", hp, (kb * 128) // TC), ("qh", hp, q0 // TC)
        if first:
            S.add("dve", lambda e: e.memset(RS_f[:], 0.0), w=["RS_f"])
        S.add("pe", lambda e: e.matmul(zb[i2][:, :ncol], kblk, qcols, start=True, stop=True), r=[khk, qhk], w=[("zb", i2)])
        if USE_SOFTPLUS:
            S.add("act", lambda e: e.activation(out=sp_bf[i3][:, :ncol], in_=zb[i2][:, :ncol], func=AF.Softplus),
                  r=[("zb", i2)], w=[("sp", i3)])
        else:
            S.add("act", lambda e: e.activation(out=e_sb[i2][:, :ncol], in_=zb[i2][:, :ncol], func=AF.Exp), r=[("zb", i2)], w=[("e_sb", i2)])
            S.add("act", lambda e: e.activation(out=sp_bf[i3][:, :ncol], in_=e_sb[i2][:, :ncol], func=AF.Ln, bias=ones_f[:, 0:1]),
                  r=[("e_sb", i2), "ones_f"], w=[("sp", i3)])
        if r_ >= 0:
            S.add("pool", lambda e: e.tensor_tensor(sp_bf[i3][:, :128], sp_bf[i3][:, :128], smask[:], ALU.mult),
                  r=[("sp", i3), "smask"], w=[("sp", i3)])
        if kb > 0:
            S.add("dve", lambda e: e.tensor_tensor(RS_f[:, col0:512], RS_f[:, col0:512], sp_bf[i3][:, :ncol], ALU.add),
                  r=[("sp", i3), "RS_f"], w=["RS_f"])
            nx = (n + 1) % NBUF
            S.add("dve", lambda e: e.tensor_copy(RS_b[nx][:], RS_f[:]), r=["RS_f"], w=[("RS_b", nx)])

    def part_b(n):
        h, QB, kb, r_, col0, ncol, first, q0 = geom(n)
        i2, i3 = n % 2, n % NBUF
        hp = h % 2
        kblk = khs[hp][:, kb * 128:(kb + 1) * 128]
        qcols = qhs[hp][:, q0 + col0:q0 + 512]
        khk, qhk = ("kh", hp, (kb * 128) // TC), ("qh", hp, q0 // TC)
        S.add("pe", lambda e: e.matmul(ab[i2][:, :ncol], kblk, qcols, start=True, stop=False), r=[khk, qhk], w=[("ab", i2)])
        S.add("pe", lambda e: e.matmul(ab[i2][:, :ncol], ntri[:], sp_bf[i3][:, :ncol], start=False, stop=first),
              r=["ntri", ("sp", i3)], w=[("ab", i2)])
        if not first:
            S.add("pe", lambda e: e.matmul(ab[i2][:, :ncol], nones[:], RS_b[i3][:, col0:512], start=False, stop=True),
                  r=["nones", ("RS_b", i3)], w=[("ab", i2)])
        S.add("act", lambda e: e.activation(out=A_bf[i3][:, :ncol], in_=ab[i2][:, :ncol], func=AF.Exp), r=[("ab", i2)], w=[("A", i3)])
        if r_ >= 0:
            S.add("pool", lambda e: e.tensor_tensor(A_bf[i3][:, :128], A_bf[i3][:, :128], smask[:], ALU.mult),
                  r=[("A", i3), "smask"], w=[("A", i3)])

    def part_c(n):
        h, QB, kb, r_, col0, ncol, first, q0 = geom(n)
        i3 = n % NBUF
        o_i = (h * NQB + QB) % 2
        hp = h % 2
        S.add("pe", lambda e: e.matmul(ob[o_i][:, col0:512], vhs[hp][:, kb, :], A_bf[i3][:, :ncol], start=first, stop=(kb == 0)),
              r=[("vh", hp, kb // 8), ("A", i3)], w=[("ob", o_i)])
        if kb == 0:
            S.add("dve", lambda e: e.tensor_copy(ost[o_i][:], ob[o_i][:]), r=[("ob", o_i)], w=[("ost", o_i)])
            if out_ag is None:
                S.add("sp", lambda e: e.dma_start(out=oT[h][:, q0:q0 + 512], in_=ost[o_i][:]),
                      r=[("ost", o_i)], w=[("dram", "o", h, QB)], slot="ost%d" % o_i)
            else:
                ck = h * 4 + QB // 4
                dst = out_ag["o_b"][ck * 128:(ck + 1) * 128, (QB % 4) * 512:(QB % 4 + 1) * 512]
                S.add("sp", lambda e: e.dma_start(out=dst, in_=ost[o_i][:]),
                      r=[("ost", o_i)], w=[("dram", "o", h, QB)], slot="ost%d" % o_i)
                if QB % 4 == 3:
                    ag_pending.append([n + 12, lambda: out_ag["agf"](S, "a", 128, [ck], [("dram", "o", h, QB - i_) for i_ in range(4)])])

    N = len(its)
    ag_pending = []
    nbat = (N + KBATCH - 1) // KBATCH
    for m in range(nbat + 2):
        for item in [it_ for it_ in ag_pending if it_[0] <= m * KBATCH]:
            item[1]()
            ag_pending.remove(item)
        for i_ in range(KBATCH):
            n = m * KBATCH + i_
            if n < N:
                part_a(n)
            nc_ = (m - 2) * KBATCH + i_
            if 0 <= m - 2 < nbat and nc_ < N:
                part_c(nc_)
        if 0 <= m - 1 < nbat:
            for n in range((m - 1) * KBATCH, min(N, m * KBATCH)):
                part_b(n)
    for item in ag_pending:
        item[1]()


def build_sb_core(NH, SEQ):
    nc = bass.Bass("TRN2", target_bir_lowering=False)
    qT = nc.dram_tensor("qT", [NH, 128, SEQ], BF16, kind="ExternalInput").ap()
    kT = nc.dram_tensor("kT", [NH, 128, SEQ], BF16, kind="ExternalInput").ap()
    v_tm = nc.dram_tensor("v_tm", [NH, SEQ, 128], BF16, kind="ExternalInput").ap()
    oT = nc.dram_tensor("oT", [NH, 128, SEQ], F32, kind="ExternalOutput").ap()
    S = Sched(nc)
    emit_sb_core(S, NH, SEQ, qT, kT, v_tm, oT)
    S.emit()
    return nc, S


def _vec(nc, name, n):
    return nc.dram_tensor(name, [128, n], F32, kind="ExternalInput").ap()


def build_ple(T, TT=512):
    nc = bass.Bass("TRN2", target_bir_lowering=False)
    xin = nc.dram_tensor("xin", [D, T], F32, kind="ExternalInput").ap()
    pT = nc.dram_tensor("pT", [256, T], F32, kind="ExternalInput").ap()
    g = _vec(nc, "g", KC)
    wpe = nc.dram_tensor("wpe", [256, D], F32, kind="ExternalInput").ap()
    wpg = nc.dram_tensor("wpg", [D, D], F32, kind="ExternalInput").ap()
    xout = nc.dram_tensor("xout", [D, T], F32, kind="ExternalOutput").ap()
    S = Sched(nc)
    C = Ctx(S, TT)
    C.load_vec("g", g, KC)
    emit_ple(C, xin, xout, pT, T, "g", wpe, wpg)
    S.emit()
    return nc, S


def build_proj_res(T, gated, TT=512):
    nc = bass.Bass("TRN2", target_bir_lowering=False)
    xin = nc.dram_tensor("xin", [D, T], F32, kind="ExternalInput").ap()
    aT = nc.dram_tensor("aT", [D, T], F32, kind="ExternalInput").ap()
    bT = nc.dram_tensor("bT", [D, T], F32, kind="ExternalInput").ap() if gated else None
    W = nc.dram_tensor("W", [D, D], F32, kind="ExternalInput").ap()
    xout = nc.dram_tensor("xout", [D, T], F32, kind="ExternalOutput").ap()
    S = Sched(nc)
    C = Ctx(S, TT)
    emit_proj_res(C, xin, xout, aT, bT, T, W)
    S.emit()
    return nc, S


def build_mlstm_in(T, TT=512):
    nc = bass.Bass("TRN2", target_bir_lowering=False)
    xin = nc.dram_tensor("xin", [D, T], F32, kind="ExternalInput").ap()
    g = _vec(nc, "g", KC)
    w_in = nc.dram_tensor("w_in", [D, 6152], F32, kind="ExternalInput").ap()
    qT = nc.dram_tensor("qT", [1024, T], BF16, kind="ExternalOutput").ap()
    kT = nc.dram_tensor("kT", [1024, T], BF16, kind="ExternalOutput").ap()
    k_tm = nc.dram_tensor("k_tm", [T, 1024], BF16, kind="ExternalOutput").ap()
    v_tm = nc.dram_tensor("v_tm", [T, 2048], BF16, kind="ExternalOutput").ap()
    sog = nc.dram_tensor("sog", [2048, T], F32, kind="ExternalOutput").ap()
    gates = nc.dram_tensor("gates", [8, T], F32, kind="ExternalOutput").ap()
    S = Sched(nc)
    C = Ctx(S, TT)
    C.load_vec("g", g, KC)
    emit_mlstm_in(C, xin, T, "g", w_in, qT, kT, k_tm, v_tm, sog, gates)
    S.emit()
    return nc, S


def build_kv(T, TT=512):
    nc = bass.Bass("TRN2", target_bir_lowering=False)
    xin = nc.dram_tensor("xin", [D, T], F32, kind="ExternalInput").ap()
    g = _vec(nc, "g", KC)
    gk = _vec(nc, "g_k", 1)
    w_kv = nc.dram_tensor("w_kv", [D, 4096], F32, kind="ExternalInput").ap()
    kT = nc.dram_tensor("kT", [2048, T], BF16, kind="ExternalOutput").ap()
    v_tm = nc.dram_tensor("v_tm", [T, 2048], BF16, kind="ExternalOutput").ap()
    S = Sched(nc)
    C = Ctx(S, TT)
    C.load_vec("g", g, KC)
    C.load_vec("g_k", gk, 1)
    emit_kv(C, xin, T, "g", w_kv, kT, v_tm)
    S.emit()
    return nc, S


def build_q(T, TT=512):
    nc = bass.Bass("TRN2", target_bir_lowering=False)
    xin = nc.dram_tensor("xin", [D, T], F32, kind="ExternalInput").ap()
    g = _vec(nc, "g", KC)
    gq = _vec(nc, "g_q", 1)
    w_q = nc.dram_tensor("w_q", [D, D], F32, kind="ExternalInput").ap()
    qT = nc.dram_tensor("qT", [2048, T], BF16, kind="ExternalOutput").ap()
    S = Sched(nc)
    C = Ctx(S, TT)
    C.load_vec("g", g, KC)
    C.load_vec("g_q", gq, 1)
    for _ in emit_headproj(C, xin, T, "g", w_q, 0, "g_q", 128 ** -0.5, qT):
        pass
    S.emit()
    return nc, S


def build_ffn(T, TT=512):
    nc = bass.Bass("TRN2", target_bir_lowering=False)
    xin = nc.dram_tensor("xin", [D, T], F32, kind="ExternalInput").ap()
    g = nc.dram_tensor("g", [128, KC], F32, kind="ExternalInput").ap()
    wg = nc.dram_tensor("wg", [D, DFF], F32, kind="ExternalInput").ap()
    wu = nc.dram_tensor("wu", [D, DFF], F32, kind="ExternalInput").ap()
    wd = nc.dram_tensor("wd", [DFF, D], F32, kind="ExternalInput").ap()
    xout = nc.dram_tensor("xout", [D, T], F32, kind="ExternalOutput").ap()
    S = Sched(nc)
    C = Ctx(S, TT)
    C.load_vec("g", g, KC)
    emit_ffn(C, xin, xout, T, "g", wg, wu, wd)
    S.emit()
    return nc, S


RG = [[0, 1, 2, 3], [4, 5, 6, 7]]
IX = dict(MQ=0, MK=16, MV=80, PAM=144, PAS=208, SQ=272, SV=288)
NIDX = 544
TCORE = 2048
SEQLEN = 8192
I32 = mybir.dt.int32


def build_fused(plan="full"):
    nc = bass.Bass("TRN2", target_bir_lowering=False)
    T = TCORE
    ext_names = []
    _cache = {}
    shapes = {
        "xT": ([D, T], F32), "pT": ([4, 256, T], F32), "ffn_norm": ([4, 2, 128, KC], F32),
        "ffn_w_gate": ([4, 2, D, DFF], F32), "ffn_w_up": ([4, 2, D, DFF], F32), "ffn_w_down": ([4, 2, DFF, D], F32),
        "mix_norm": ([4, 128, KC], F32), "mlstm_w_in": ([2, D, 6152], F32), "bif": ([2, 1, 2], F32),
        "ghead": ([2, 128, 4], F32), "mlstm_w_out": ([2, D, D], F32), "kv_norm": ([128, KC], F32),
        "sb_w_kv": ([D, 4096], F32), "g_k": ([128, 1], F32), "sb_w_q": ([2, D, D], F32), "g_q": ([2, 128, 1], F32),
        "sb_w_o": ([2, D, D], F32), "ple_norm": ([4, 128, KC], F32), "ple_w_proj": ([4, 256, D], F32),
        "ple_w_gate": ([4, D, D], F32), "idx": ([128, NIDX], I32), "onehot": ([1, 4], F32),
    }

    class _Ext:
        def __getattr__(self, name):
            if name not in _cache:
                shp, dt = shapes[name]
                _cache[name] = nc.dram_tensor(name, shp, dt, kind="ExternalInput").ap()
                ext_names.append(name)
            return _cache[name]
    E = _Ext()
    nc._ext_names = ext_names
    outT = nc.dram_tensor("outT", [D, T], F32, kind="ExternalOutput").ap()

    def dt_(name, shape, dt=F32):
        return nc.dram_tensor(name, shape, dt)

    XA, XB = dt_("XA", [D, T]), dt_("XB", [D, T])
    q_b, k_b = dt_("q_b", [1024, T], BF16), dt_("k_b", [1024, T], BF16)
    ktm_b, vtm_b = dt_("ktm_b", [T, 1024], BF16), dt_("vtm_b", [T, 2048], BF16)
    gates_b = dt_("gates_b", [8, T])
    sog = dt_("sog", [2048, T])
    q_all, k_all = dt_("q_all", [4096, T], BF16), dt_("k_all", [4096, T], BF16)
    ktm_all, vtm_all = dt_("ktm_all", [4 * T, 1024], BF16), dt_("vtm_all", [4 * T, 2048], BF16)
    gates_all = dt_("gates_all", [32, T])
    h_bm, h_allm = dt_("h_bm", [8 * 512, 1024]), dt_("h_allm", [16 * 1024, 1024])
    h_bs, h_alls = dt_("h_bs", [16 * 128, 2048]), dt_("h_alls", [16 * 512, 2048])
    sq_b, sk_b, sv_b = dt_("sq_b", [2048, T], BF16), dt_("sk_b", [2048, T], BF16), dt_("sv_b", [T, 2048], BF16)
    sq_all, sk_all, sv_all = dt_("sq_all", [8192, T], BF16), dt_("sk_all", [8192, T], BF16), dt_("sv_all", [4 * T, 2048], BF16)

    stage_no = [0]

    def stage(fn):
        with nc.cleanup_on_exit():
            S = Sched(nc, "s%d_" % stage_no[0])
            stage_no[0] += 1
            fn(S)
            S.emit()

    def agc(S, name, src, dst, rk, ks, rkeys=()):
        for k in ks:
            S.add("pool", lambda e, k=k: e.collective_compute(
                "AllGather", ALU.bypass, replica_groups=RG,
                ins=[src[k * rk:(k + 1) * rk, :].opt()], outs=[dst[k * 4 * rk:(k + 1) * 4 * rk, :].opt()]),
                r=list(rkeys), w=[("ag", name)], slot="cc_%s" % name, inc=1, group=True)
        return [("ag", name)]

    def ag(S, name, src, dst, rk):
        return agc(S, name, src, dst, rk, range(src.shape[0] // rk))

    def load_idx(S):
        ixt = S.sb("ixt", [128, NIDX], I32)
        S.add("sp", lambda e: e.dma_start(out=ixt[:], in_=E.idx), w=["ixt"], slot="ixt")
        return ixt

    state = {"cur": E.xT, "flip": 0}

    def nxt_buf(final=False):
        if final:
            return outT
        t = (XA, XB)[state["flip"]]
        state["flip"] ^= 1
        return t.ap()

    def st_ffn(i, k):
        cur, nxt = state["cur"], nxt_buf()

        def f(S):
            C = Ctx(S, 512)
            C.load_vec("g", E.ffn_norm[i, k], KC)
            emit_ffn(C, cur, nxt, T, "g", E.ffn_w_gate[i, k], E.ffn_w_up[i, k], E.ffn_w_down[i, k])
        stage(f)
        state["cur"] = nxt

    def st_ple(i, final):
        cur, nxt = state["cur"], nxt_buf(final)

        def f(S):
            C = Ctx(S, 512)
            C.load_vec("g", E.ple_norm[i], KC)
            emit_ple(C, cur, nxt, E.pT[i], T, "g", E.ple_w_proj[i], E.ple_w_gate[i])
        stage(f)
        state["cur"] = nxt

    def st_proj(W, gated, final=False):
        cur, nxt = state["cur"], nxt_buf(final)

        def f(S):
            C = Ctx(S, 512)
            ixt = load_idx(S)
            if gated:
                view, cb = h_allm.ap().rearrange("r (k f) -> (r k) f", k=2), IX["PAM"]
            else:
                view, cb = h_alls.ap().rearrange("r (k f) -> (r k) f", k=4), IX["PAS"]
            emit_proj_res(C, cur, nxt, None, sog.ap() if gated else None, T, W, gth=(view, ixt, "ixt", cb, []))
        stage(f)
        state["cur"] = nxt

    def mlstm_part(i):
        def f(S, i=i):
            C = Ctx(S, 512)
            C.load_vec("g", E.mix_norm[i], KC)
            def agf(S, name, rk, ks, rkeys):
                src, dst = {"ktm": (ktm_b, ktm_all), "vtm": (vtm_b, vtm_all)}[name]
                agc(S, name, src, dst, rk, ks, rkeys)
            emit_mlstm_in(C, state["cur"], T, "g", E.mlstm_w_in[i], q_b.ap(), k_b.ap(), ktm_b.ap(), vtm_b.ap(), sog.ap(), gates_b.ap(), agf=agf)
        stage(f)

        def f(S, i=i):
            ixt = load_idx(S)
            agk = {"q": ag(S, "q", q_b, q_all, 256), "k": ag(S, "k", k_b, k_all, 256),
                   "ktm": [], "vtm": [], "g": ag(S, "g", gates_b, gates_all, 8)}
            gth = dict(IX)
            gth["agk"] = agk
            gth.update(ixt=ixt, ixkey="ixt", onehot=E.onehot,
                       q_view=q_all.ap().rearrange("r (two f) -> (r two) f", two=2),
                       k_view=k_all.ap().rearrange("r (two f) -> (r two) f", two=2),
                       ktm_view=ktm_all.ap().rearrange("t (h f) -> (t h) f", h=4),
                       vtm_view=vtm_all.ap().rearrange("t (h f) -> (t h) f", h=4),
                       gates_all=gates_all.ap())
            out_ag = {"h_b": h_bm.ap(), "agf": lambda S, name, rk, ks, rkeys: agc(S, name, h_bm, h_allm, rk, ks, rkeys)}
            emit_mlstm_core(S, SEQLEN, None, None, None, None, None, None, E.bif[i], E.ghead[i], None, gth=gth, out_ag=out_ag)
        stage(f)
        st_proj(E.mlstm_w_out[i], True, final=(plan == "mlstm0"))

    def kv_part():
        def f(S):
            C = Ctx(S, 512)
            C.load_vec("g", E.kv_norm, KC)
            C.load_vec("g_k", E.g_k, 1)
            emit_kv(C, state["cur"], T, "g", E.sb_w_kv, sk_b.ap(), sv_b.ap(),
                    agf=lambda S, name, rk, ks, rkeys: agc(S, name, sv_b, sv_all, rk, ks, rkeys))
        stage(f)

    def sb_part(j):
        def f(S, j=j):
            C = Ctx(S, 512)
            C.load_vec("g", E.mix_norm[2 + j], KC)
            C.load_vec("g_q", E.g_q[j], 1)
            for _ in emit_headproj(C, state["cur"], T, "g", E.sb_w_q[j], 0, "g_q", 128 ** -0.5, sq_b.ap()):
                pass
        stage(f)

        def f(S, j=j):
            ixt = load_idx(S)
            agk = {"q": ag(S, "q", sq_b, sq_all, 256), "k": [], "v": []}
            if j == 0:
                agk["k"] = ag(S, "k", sk_b, sk_all, 256)
            gth = dict(IX)
            gth["agk"] = agk
            gth.update(ixt=ixt, ixkey="ixt", q_view=sq_all.ap(), k_view=sk_all.ap(),
                       v_view=sv_all.ap().rearrange("t (h f) -> (t h) f", h=16))
            out_ag = {"o_b": h_bs.ap(), "agf": lambda S, name, rk, ks, rkeys: agc(S, name, h_bs, h_alls, rk, ks, rkeys)}
            emit_sb_core(S, 4, SEQLEN, None, None, None, None, gth=gth, out_ag=out_ag)
        stage(f)
        st_proj(E.sb_w_o[j], False, final=(plan == "sb0"))

    if plan == "mlstm0":
        mlstm_part(0)
        return nc
    if plan == "sb0":
        kv_part()
        sb_part(0)
        return nc
    for i in range(4):
        if i == 2:
            kv_part()
        st_ffn(i, 0)
        if i < 2:
            mlstm_part(i)
        else:
            sb_part(i - 2)
        st_ffn(i, 1)
        st_ple(i, final=(i == 3))
    return nc


_PROG = {}


def _c(a):
    return np.ascontiguousarray(a)


def _vl(g, n):
    g = np.asarray(g, dtype=np.float32)
    lead = g.shape[:-1]
    return _c(np.swapaxes(g.reshape(lead + (n, 128)), -1, -2))


def _idx_table(c):
    h = c % 4
    p = np.arange(128, dtype=np.int64)
    t = np.zeros((128, NIDX), dtype=np.int64)

    def g256(r, s_):
        return (r // 256) * 1024 + s_ * 256 + r % 256

    for sg in range(8):
        s_, half = sg // 2, sg % 2
        for dk in range(2):
            t[:, IX["MQ"] + sg * 2 + dk] = g256(h * 256 + dk * 128 + p, s_) * 2 + half
    for g in range(64):
        tg = g * 128 + p
        s_, tt = tg // 2048, tg % 2048
        t[:, IX["MK"] + g] = ((tt // 512) * 2048 + s_ * 512 + tt % 512) * 4 + h
        t[:, IX["MV"] + g] = g256(tt, s_) * 4 + h
    for cch in range(16):
        hh, r = cch // 4, (cch % 4) * 128 + p
        for ti in range(4):
            sg = h * 2 + ti // 2
            ck = sg * 2 + r // 256
            t[:, IX["PAM"] + cch * 4 + ti] = (ck * 1024 + hh * 256 + r % 256) * 2 + ti % 2
            ck = (r // 128) * 4 + h
            t[:, IX["PAS"] + cch * 4 + ti] = (ck * 512 + hh * 128 + r % 128) * 4 + ti
    for hl in range(4):
        for s_ in range(4):
            t[:, IX["SQ"] + hl * 4 + s_] = g256((4 * h + hl) * 128 + p, s_)
        for blk in range(64):
            tg = blk * 128 + p
            s_, tt = tg // 2048, tg % 2048
            t[:, IX["SV"] + hl * 64 + blk] = g256(tt, s_) * 16 + 4 * h + hl
    return t.astype(np.int32)


def kernel(x, p, ffn_norm, ffn_w_gate, ffn_w_up, ffn_w_down, mix_norm,
           mlstm_w_in, mlstm_b_if, mlstm_head_norm, mlstm_w_out,
           kv_norm, sb_w_kv, sb_k_norm, sb_w_q, sb_q_norm, sb_w_o,
           ple_norm, ple_w_proj, ple_w_gate):
    f32 = np.float32
    T = TCORE
    x = np.asarray(x, f32)
    p = np.asarray(p, f32)
    if "nc" not in _PROG:
        _PROG["nc"] = build_fused()
    nc = _PROG["nc"]
    shared = {
        "ffn_norm": _vl(ffn_norm, KC),
        "ffn_w_gate": _c(np.asarray(ffn_w_gate, f32)), "ffn_w_up": _c(np.asarray(ffn_w_up, f32)),
        "ffn_w_down": _c(np.asarray(ffn_w_down, f32)),
        "mix_norm": _vl(mix_norm, KC),
        "mlstm_w_in": _c(np.asarray(mlstm_w_in, f32)), "mlstm_w_out": _c(np.asarray(mlstm_w_out, f32)),
        "kv_norm": _vl(kv_norm, KC), "sb_w_kv": _c(np.asarray(sb_w_kv, f32)), "g_k": _vl(sb_k_norm, 1),
        "sb_w_q": _c(np.asarray(sb_w_q, f32)), "g_q": _vl(sb_q_norm, 1), "sb_w_o": _c(np.asarray(sb_w_o, f32)),
        "ple_norm": _vl(ple_norm, KC), "ple_w_proj": _c(np.asarray(ple_w_proj, f32)),
        "ple_w_gate": _c(np.asarray(ple_w_gate, f32)),
    }
    b_if = np.asarray(mlstm_b_if, f32)
    hn = np.asarray(mlstm_head_norm, f32)
    in_maps = []
    for c in range(NCORES):
        b, s = c // 4, c % 4
        h = s
        m = dict(shared)
        m["xT"] = _c(x[b, s * T:(s + 1) * T].T)
        m["pT"] = _c(p[:, b, s * T:(s + 1) * T, :].transpose(0, 2, 1))
        m["bif"] = _c(np.stack([b_if[:, h], b_if[:, 4 + h]], axis=-1)[:, None, :])
        m["ghead"] = _vl(hn[:, h * 512:(h + 1) * 512], 4)
        m["idx"] = _idx_table(c)
        oh = np.zeros((1, 4), f32)
        oh[0, h] = 1.0
        m["onehot"] = oh
        in_maps.append({k: v for k, v in m.items() if k in nc._ext_names})
    res = run_bass_kernel_spmd(nc, in_maps, core_ids=list(range(NCORES)))
    out = np.empty((2, SEQLEN, D), dtype=f32)
    for c in range(NCORES):
        b, s = c // 4, c % 4
        out[b, s * T:(s + 1) * T] = res.results[c]["outT"].T
    return out
```

```python
import contextlib
import numpy as np
import concourse.bass as bass
import concourse.mybir as mybir
from concourse.bass_utils import run_bass_kernel_spmd

F32 = mybir.dt.float32
BF16 = mybir.dt.bfloat16
AF = mybir.ActivationFunctionType
ALU = mybir.AluOpType

D = 2048
KC = D // 128
DFF = 5504
FC = DFF // 128
EPS = 1e-6
NCORES = 8
PENG = 'dve'
SELF_ORDERED = {'pe'}
NSTF = 4
USE_SOFTPLUS = True


class Op:
    __slots__ = ("eng", "fn", "deps", "slot", "needed", "tok", "waits", "know", "inc")

    def __init__(self, eng, fn, slot, inc=16):
        self.eng = eng
        self.fn = fn
        self.slot = slot
        self.inc = inc
        self.deps = set()
        self.needed = False
        self.tok = None
        self.waits = []
        self.know = None


class Sched:
    ENGS = ("pe", "act", "dve", "pool", "sp")

    def __init__(self, nc, prefix=""):
        self.nc = nc
        self.prefix = prefix
        self.ops = []
        self.last_w = {}
        self.readers = {}
        self.stack = contextlib.ExitStack()
        self.n_t = 0

    def uid(self):
        self.n_t += 1
        return self.n_t

    def sb(self, name, shape, dt):
        return self.stack.enter_context(self.nc.sbuf_tensor(self.prefix + name, shape, dt))

    def ps(self, name, shape, dt=F32):
        return self.stack.enter_context(self.nc.psum_tensor(self.prefix + name, shape, dt))

    def add(self, eng, fn, r=(), w=(), slot=None, inc=16, group=False):
        op = Op(eng, fn, slot, inc)
        deps = op.deps
        for k in r:
            d = self.last_w.get(k)
            if d is not None:
                deps.add(d)
        for k in w:
            d = self.last_w.get(k)
            if d is not None and not (group and d.slot == slot):
                deps.add(d)
            elif d is not None:
                deps.update(x for x in d.deps if x.slot != slot)
            rs = self.readers.get(k)
            if rs:
                deps.update(rs)
        for k in w:
            self.last_w[k] = op
            self.readers[k] = set()
        for k in r:
            self.readers.setdefault(k, set()).add(op)
        deps.discard(op)
        self.ops.append(op)
        return op

    def emit(self):
        nc = self.nc
        ops = self.ops
        for op in ops:
            for d in op.deps:
                if d.eng == op.eng and d.eng in SELF_ORDERED and d.slot is None and op.slot is None:
                    continue
                d.needed = True
        cnt = {e: 0 for e in self.ENGS}
        slot_cnt = {}
        for op in ops:
            if op.slot is not None:
                slot_cnt[op.slot] = slot_cnt.get(op.slot, 0) + op.inc
                op.tok = (("slot", op.slot), slot_cnt[op.slot])
            elif op.needed:
                cnt[op.eng] += 1
                op.tok = (("eng", op.eng), cnt[op.eng])
        sems = {}
        for e in self.ENGS:
            sems[("eng", e)] = nc.alloc_semaphore(self.prefix + "s_" + e)
        for i, sl in enumerate(slot_cnt):
            sems[("slot", sl)] = nc.alloc_semaphore(self.prefix + "d%d" % i)
        known = {e: {} for e in self.ENGS}
        for op in ops:
            kn = known[op.eng]
            for d in op.deps:
                if d.eng == op.eng and d.eng in SELF_ORDERED and d.slot is None and op.slot is None:
                    continue
                sk, val = d.tok
                if kn.get(sk, 0) >= val:
                    continue
                op.waits.append((sk, val))
                for k2, v2 in d.know.items():
                    if kn.get(k2, 0) < v2:
                        kn[k2] = v2
            if op.tok is not None:
                kn2 = dict(kn)
                sk, val = op.tok
                if kn2.get(sk, 0) < val:
                    kn2[sk] = val
                op.know = kn2
        per_eng = {e: [] for e in self.ENGS}
        for op in ops:
            w = {}
            for sk, val in op.waits:
                if w.get(sk, 0) < val:
                    w[sk] = val
            op.waits = list(w.items())
            per_eng[op.eng].append(op)
        final_waits = [(("slot", sl), v) for sl, v in slot_cnt.items()]
        self.stats = {e: len(per_eng[e]) for e in self.ENGS}

        def run(engobj, lst, final=False):
            for op in lst:
                for sk, val in op.waits:
                    engobj.wait_ge(sems[sk], val)
                ins = op.fn(engobj)
                if op.tok is not None:
                    if op.slot is not None and op.inc == 1:
                        ins.then_inc(sems[op.tok[0]])
                    else:
                        ins.then_inc(sems[op.tok[0]], op.inc if op.slot is not None else 1)
            if final:
                for sk, val in final_waits:
                    engobj.wait_ge(sems[sk], val)

        with nc.Block(self.prefix + "blk") as block:
            @block.tensor
            def _(e):
                run(e, per_eng["pe"])

            @block.scalar
            def _(e):
                run(e, per_eng["act"])

            @block.vector
            def _(e):
                run(e, per_eng["dve"])

            @block.gpsimd
            def _(e):
                run(e, per_eng["pool"])

            @block.sync
            def _(e):
                run(e, per_eng["sp"], final=True)


class Ctx:
    def __init__(self, S, TT):
        self.S = S
        self.TT = TT
        nc = S.nc
        self.ones = S.sb("ones_bf", [128, 128], BF16)
        self.banks = [S.ps("bank%d" % i, [128, 512], F32) for i in range(8)]
        S.add("dve", lambda e: e.memset(self.ones[:], 1.0), w=[("ones",)])
        self.xt = S.sb("xt", [128, KC, TT], F32)
        self.xn = S.sb("xn", [128, KC, TT], BF16)
        self.sq = S.sb("sq", [128, KC, TT], BF16)
        self.rs = S.sb("rstd", [128, TT], F32)
        self.rs2 = S.sb("rstd2", [128, TT], F32)
        self.gv = {}

    def load_vec(self, name, dram_ap, nchunk):
        S = self.S
        t = S.sb("v_" + name, [128, nchunk], F32)
        S.add("sp", lambda e: e.dma_start(out=t[:], in_=dram_ap), w=[("v", name)], slot="v_" + name)
        self.gv[name] = t
        return t


def emit_norm(C, xin, t0, gname, load_x=True):
    S, TT = C.S, C.TT
    xt, xn, sq, rs, rs2 = C.xt, C.xn, C.sq, C.rs, C.rs2
    g = C.gv[gname]
    ssb = C.banks[0]
    if load_x:
        src = xin.rearrange("(c p) t -> p c t", p=128)[:, :, t0:t0 + TT]
        S.add("sp", lambda e: e.dma_start(out=xt[:], in_=src), w=[("xt",)], slot="xt")
    S.add("act", lambda e: e.activation(out=sq[:], in_=xt[:], func=AF.Square), r=[("xt",)], w=[("sq",)])
    for c in range(KC):
        S.add("pe", lambda e, c=c: e.matmul(ssb[:, :TT], C.ones[:], sq[:, c, :], start=(c == 0), stop=(c == KC - 1)),
              r=[("sq",), ("ones",)], w=[("ps", 0)])
    S.add("dve", lambda e: e.tensor_scalar(rs[:], ssb[:, :TT], 1.0 / D, EPS, ALU.mult, ALU.add),
          r=[("ps", 0)], w=[("rs",)])
    S.add("act", lambda e: e.activation(out=rs2[:], in_=rs[:], func=AF.Sqrt), r=[("rs",)], w=[("rs2",)])
    S.add("dve", lambda e: e.reciprocal(rs[:], rs2[:]), r=[("rs2",)], w=[("rs",)])
    for c in range(KC):
        S.add("dve", lambda e, c=c: e.scalar_tensor_tensor(xn[:, c, :], xt[:, c, :], g[:, c:c + 1], rs[:],
                                                           ALU.mult, ALU.mult),
              r=[("xt",), ("rs",), ("v", gname)], w=[("xn", c)])


def emit_ffn(C, xin, xout, T, gname, wg, wu, wd):
    S, TT = C.S, C.TT
    if not hasattr(C, "wg"):
        C.wg = [S.sb("wg%d" % i, [128, KC, 512], BF16) for i in range(2)]
        C.wu = [S.sb("wu%d" % i, [128, KC, 512], BF16) for i in range(2)]
        C.wd = [S.sb("wd%d" % i, [128, 11, 512], BF16) for i in range(2)]
        C.hid = S.sb("hid", [128, FC, TT], BF16)
        C.sg = [S.sb("sg%d" % i, [128, TT], F32) for i in range(2)]
        C.ctr = {"gu": 0, "wd": 0, "f": 0}
    wg_r = wg.rearrange("(c p) n -> p c n", p=128)
    wu_r = wu.rearrange("(c p) n -> p c n", p=128)
    wd_r = wd.rearrange("(f p) n -> p f n", p=128)
    xo_r = xout.rearrange("(c p) t -> p c t", p=128)
    hid = C.hid
    for t0 in range(0, T, TT):
        emit_norm(C, xin, t0, gname)
        for fg in range(0, FC, 4):
            nf = min(4, FC - fg)
            sl = C.ctr["gu"] % 2
            C.ctr["gu"] += 1
            wgt, wut = C.wg[sl], C.wu[sl]
            S.add("pool", lambda e, wgt=wgt, fg=fg, nf=nf: e.dma_start(
                out=wgt[:, :, :nf * 128], in_=wg_r[:, :, fg * 128:(fg + nf) * 128]),
                w=[("wg", sl)], slot="wg%d" % sl)
            S.add("pool", lambda e, wut=wut, fg=fg, nf=nf: e.dma_start(
                out=wut[:, :, :nf * 128], in_=wu_r[:, :, fg * 128:(fg + nf) * 128]),
                w=[("wu", sl)], slot="wu%d" % sl)
            for fi in range(nf):
                f = fg + fi
                pb = C.ctr["f"] % 2
                C.ctr["f"] += 1
                gb, ub = C.banks[4 + pb], C.banks[6 + pb]
                for c in range(KC):
                    S.add("pe", lambda e, c=c, fi=fi, gb=gb, wgt=wgt: e.matmul(
                        gb[:, :TT], wgt[:, c, fi * 128:(fi + 1) * 128], C.xn[:, c, :], start=(c == 0), stop=(c == KC - 1)),
                        r=[("wg", sl), ("xn", c)], w=[("ps", 4 + pb)])
                for c in range(KC):
                    S.add("pe", lambda e, c=c, fi=fi, ub=ub, wut=wut: e.matmul(
                        ub[:, :TT], wut[:, c, fi * 128:(fi + 1) * 128], C.xn[:, c, :], start=(c == 0), stop=(c == KC - 1)),
                        r=[("wu", sl), ("xn", c)], w=[("ps", 6 + pb)])
                sg = C.sg[pb]
                S.add("act", lambda e, sg=sg, gb=gb: e.activation(out=sg[:], in_=gb[:, :TT], func=AF.Silu),
                      r=[("ps", 4 + pb)], w=[("sg", pb)])
                S.add("dve", lambda e, sg=sg, ub=ub, f=f: e.tensor_tensor(hid[:, f, :], ub[:, :TT], sg[:], ALU.mult),
                      r=[("ps", 6 + pb), ("sg", pb)], w=[("hid", f)])
        for ng in range(4):
            for f0 in range(0, FC, 11):
                nf = min(11, FC - f0)
                sl = C.ctr["wd"] % 2
                C.ctr["wd"] += 1
                wdt = C.wd[sl]
                S.add("pool", lambda e, wdt=wdt, f0=f0, nf=nf, ng=ng: e.dma_start(
                    out=wdt[:, :nf, :], in_=wd_r[:, f0:f0 + nf, ng * 512:(ng + 1) * 512]),
                    w=[("wd", sl)], slot="wd%d" % sl)
                for fi in range(nf):
                    f = f0 + fi
                    for j in range(4):
                        S.add("pe", lambda e, wdt=wdt, fi=fi, f=f, j=j: e.matmul(
                            C.banks[j][:, :TT], wdt[:, fi, j * 128:(j + 1) * 128], hid[:, f, :],
                            start=(f == 0), stop=(f == FC - 1)),
                            r=[("wd", sl), ("hid", f)], w=[("ps", j)])
            for j in range(4):
                c = ng * 4 + j
                S.add("dve", lambda e, j=j, c=c: e.scalar_tensor_tensor(
                    C.xt[:, c, :], C.banks[j][:, :TT], 0.5, C.xt[:, c, :], ALU.mult, ALU.add),
                    r=[("ps", j), ("xt",)], w=[("xt",)])
        S.add("sp", lambda e, t0=t0: e.dma_start(out=xo_r[:, :, t0:t0 + TT], in_=C.xt[:]),
              r=[("xt",)], w=[("dram", "xout")], slot="xt_st")


def ctx_extra(C):
    S, TT = C.S, C.TT
    if hasattr(C, "wsl"):
        return
    if not hasattr(C, "wg"):
        C.wg = [S.sb("wg%d" % i, [128, KC, 512], BF16) for i in range(2)]
        C.wu = [S.sb("wu%d" % i, [128, KC, 512], BF16) for i in range(2)]
        C.sg = [S.sb("sg%d" % i, [128, TT], F32) for i in range(2)]
    C.wsl = [(C.wg[0], ("wg", 0), "wg0"), (C.wu[0], ("wu", 0), "wu0"),
             (C.wg[1], ("wg", 1), "wg1"), (C.wu[1], ("wu", 1), "wu1")]
    C.wctr = 0
    C.bctr = 0
    C.stb = [S.sb("stb%d" % i, [128, 4, 512], BF16) for i in range(2)]
    C.stf = [S.sb("stf%d" % i, [128, 4, 512], F32) for i in range(NSTF)]
    C.stctr = {"b": 0, "f": 0}
    C.kf = S.sb("kf", [128, TT], F32)
    C.sqh = S.sb("sqh", [128, TT], BF16)
    C.hr = S.sb("hr", [128, TT], F32)
    C.hr2 = S.sb("hr2", [128, TT], F32)


def next_w(C):
    t = C.wsl[C.wctr % 4]
    C.wctr += 1
    return t


def next_bank(C):
    b = 4 + (C.bctr % 4)
    C.bctr += 1
    return b


def next_stage(C, kind):
    i = C.stctr[kind] % 2
    C.stctr[kind] += 1
    return (C.stb if kind == "b" else C.stf)[i], ("st" + kind, i), "st%s%d" % (kind, i)


def fm_linear(C, W_r, c0, ncols, kch, rhs, rkey, epilogue):
    S, TT = C.S, C.TT
    for gi, g0 in enumerate(range(0, ncols, 512)):
        gw = min(512, ncols - g0)
        wt, wkey, wslot = next_w(C)
        S.add("pool", lambda e, wt=wt, g0=g0, gw=gw: e.dma_start(
            out=wt[:, :kch, :gw], in_=W_r[:, :, c0 + g0:c0 + g0 + gw]), w=[wkey], slot=wslot)
        nj = (gw + 127) // 128
        for j in range(nj):
            m = min(128, gw - j * 128)
            b = next_bank(C)
            for c in range(kch):
                S.add("pe", lambda e, wt=wt, c=c, j=j, m=m, b=b: e.matmul(
                    C.banks[b][:m, :TT], wt[:, c, j * 128:j * 128 + m], rhs[:, c, :], start=(c == 0), stop=(c == kch - 1)),
                    r=[wkey, rkey(c)], w=[("ps", b)])
            epilogue(gi, j, nj, m, b)


def tm_linear(C, W_r, c0, ncols, epilogue):
    S, TT = C.S, C.TT
    for gi, g0 in enumerate(range(0, ncols, 512)):
        gw = min(512, ncols - g0)
        wt, wkey, wslot = next_w(C)
        S.add("pool", lambda e, wt=wt, g0=g0, gw=gw: e.dma_start(
            out=wt[:, :, :gw], in_=W_r[:, :, c0 + g0:c0 + g0 + gw]), w=[wkey], slot=wslot)
        for tb in range(TT // 128):
            b = next_bank(C)
            for c in range(KC):
                S.add("pe", lambda e, wt=wt, c=c, tb=tb, gw=gw, b=b: e.matmul(
                    C.banks[b][:, :gw], C.xn[:, c, tb * 128:(tb + 1) * 128], wt[:, c, :gw], start=(c == 0), stop=(c == KC - 1)),
                    r=[wkey, ("xn", c)], w=[("ps", b)])
            epilogue(gi, tb, gw, b)


def headnorm(C, b, gname, scale, out_ap, out_key):
    S, TT = C.S, C.TT
    bank = C.banks[b]
    g = C.gv[gname]
    S.add("act", lambda e: e.activation(out=C.kf[:], in_=bank[:, :TT], func=AF.Copy, scale=float(scale)),
          r=[("ps", b)], w=[("kf",)])
    S.add("act", lambda e: e.activation(out=C.sqh[:], in_=bank[:, :TT], func=AF.Square), r=[("ps", b)], w=[("sqh",)])
    S.add("pe", lambda e: e.matmul(C.banks[1][:, :TT], C.ones[:], C.sqh[:], start=True, stop=True),
          r=[("sqh",), ("ones",)], w=[("ps", 1)])
    S.add("dve", lambda e: e.tensor_scalar(C.hr[:], C.banks[1][:, :TT], 1.0 / 128, EPS, ALU.mult, ALU.add),
          r=[("ps", 1)], w=[("hr",)])
    S.add("act", lambda e: e.activation(out=C.hr2[:], in_=C.hr[:], func=AF.Sqrt), r=[("hr",)], w=[("hr2",)])
    S.add("dve", lambda e: e.reciprocal(C.hr[:], C.hr2[:]), r=[("hr2",)], w=[("hr",)])
    S.add("dve", lambda e: e.scalar_tensor_tensor(out_ap, C.kf[:], g[:, 0:1], C.hr[:], ALU.mult, ALU.mult),
          r=[("kf",), ("hr",), ("v", gname)], w=[out_key])


def xn_key(c):
    return ("xn", c)


def emit_ple(C, xin, xout, pT, T, gname, wpe, wpg):
    S, TT = C.S, C.TT
    ctx_extra(C)
    if not hasattr(C, "pt"):
        C.pt = S.sb("pt", [128, 2, TT], BF16)
        C.wpe = [S.sb("wpe%d" % i, [128, 2, 512], BF16) for i in range(2)]
        C.pectr = 0
    wpg_r = wpg.rearrange("(c p) n -> p c n", p=128)
    wpe_r = wpe.rearrange("(c p) n -> p c n", p=128)
    pT_r = pT.rearrange("(c p) t -> p c t", p=128)
    xo_r = xout.rearrange("(c p) t -> p c t", p=128)
    for t0 in range(0, T, TT):
        emit_norm(C, xin, t0, gname)
        S.add("pool", lambda e, t0=t0: e.dma_start(out=C.pt[:], in_=pT_r[:, :, t0:t0 + TT]), w=[("pt",)], slot="pt")
        for ng in range(4):
            sl = C.pectr % 2
            C.pectr += 1
            wpet = C.wpe[sl]
            S.add("pool", lambda e, wpet=wpet, ng=ng: e.dma_start(out=wpet[:], in_=wpe_r[:, :, ng * 512:(ng + 1) * 512]),
                  w=[("wpe", sl)], slot="wpe%d" % sl)

            def epi(gi, j, nj, m, b, ng=ng, wpet=wpet, sl=sl):
                c = ng * 4 + j
                pb = 2 + (c % 2)
                for cc in range(2):
                    S.add("pe", lambda e, cc=cc: e.matmul(C.banks[pb][:, :TT], wpet[:, cc, j * 128:(j + 1) * 128], C.pt[:, cc, :],
                                                          start=(cc == 0), stop=(cc == 1)),
                          r=[("wpe", sl), ("pt",)], w=[("ps", pb)])
                sg = C.sg[c % 2]
                S.add("act", lambda e: e.activation(out=sg[:], in_=C.banks[b][:, :TT], func=AF.Sigmoid),
                      r=[("ps", b)], w=[("sg", c % 2)])
                S.add("dve", lambda e: e.tensor_tensor(sg[:], C.banks[pb][:, :TT], sg[:], ALU.mult),
                      r=[("ps", pb), ("sg", c % 2)], w=[("sg", c % 2)])
                S.add("dve", lambda e: e.tensor_tensor(C.xt[:, c, :], C.xt[:, c, :], sg[:], ALU.add),
                      r=[("sg", c % 2), ("xt",)], w=[("xt",)])
            fm_linear(C, wpg_r, ng * 512, 512, KC, C.xn, xn_key, epi)
        S.add("sp", lambda e, t0=t0: e.dma_start(out=xo_r[:, :, t0:t0 + TT], in_=C.xt[:]),
              r=[("xt",)], w=[("dram", "xout")], slot="xt_st")


def emit_proj_res(C, xin, xout, aT, bT, T, W, gth=None):
    S, TT = C.S, C.TT
    ctx_extra(C)
    W_r = W.rearrange("(c p) n -> p c n", p=128)
    x_r = xin.rearrange("(c p) t -> p c t", p=128)
    a_r = aT.rearrange("(c p) t -> p c t", p=128) if gth is None else None
    b_r = bT.rearrange("(c p) t -> p c t", p=128) if bT is not None else None
    xo_r = xout.rearrange("(c p) t -> p c t", p=128)
    for t0 in range(0, T, TT):
        S.add("sp", lambda e, t0=t0: e.dma_start(out=C.xt[:], in_=x_r[:, :, t0:t0 + TT]), w=[("xt",)], slot="xt")
        for g4 in range(4):
            ai = C.stctr["f"] % NSTF
            C.stctr["f"] += 1
            if gth is None:
                S.add("sp", lambda e, t0=t0, g4=g4, ai=ai: e.dma_start(out=C.stf[ai][:, :, :TT], in_=a_r[:, g4 * 4:(g4 + 1) * 4, t0:t0 + TT]),
                      w=[("stf", ai)], slot="stf%d" % ai)
            else:
                view, ixt, ixkey, cb, agk = gth
                for cc in range(4):
                    col = cb + (g4 * 4 + cc) * 4 + t0 // TT
                    S.add("pool", lambda e, cc=cc, ai=ai, col=col: e.indirect_dma_start(
                        out=C.stf[ai][:, cc, :TT], out_offset=None, in_=view,
                        in_offset=bass.IndirectOffsetOnAxis(ap=ixt[:, col:col + 1], axis=0)),
                        r=[ixkey] + list(agk), w=[("stf", ai)], slot="stf%d" % ai, group=True)
            if b_r is not None:
                bi_ = C.stctr["f"] % NSTF
                C.stctr["f"] += 1
                S.add("sp", lambda e, t0=t0, g4=g4, bi_=bi_: e.dma_start(out=C.stf[bi_][:, :, :TT], in_=b_r[:, g4 * 4:(g4 + 1) * 4, t0:t0 + TT]),
                      w=[("stf", bi_)], slot="stf%d" % bi_)
                for cc in range(4):
                    c = g4 * 4 + cc
                    S.add("dve", lambda e, c=c, cc=cc, ai=ai, bi_=bi_: e.tensor_tensor(C.xn[:, c, :], C.stf[ai][:, cc, :TT], C.stf[bi_][:, cc, :TT], ALU.mult),
                          r=[("stf", ai), ("stf", bi_)], w=[("xn", c)])
            else:
                for cc in range(4):
                    c = g4 * 4 + cc
                    if cc % 2 == 0:
                        S.add("dve", lambda e, c=c, cc=cc, ai=ai: e.tensor_copy(C.xn[:, c, :], C.stf[ai][:, cc, :TT]), r=[("stf", ai)], w=[("xn", c)])
                    else:
                        S.add("act", lambda e, c=c, cc=cc, ai=ai: e.activation(out=C.xn[:, c, :], in_=C.stf[ai][:, cc, :TT], func=AF.Copy), r=[("stf", ai)], w=[("xn", c)])

        def epi(gi, j, nj, m, b):
            c = gi * 4 + j
            S.add("dve", lambda e: e.tensor_tensor(C.xt[:, c, :], C.banks[b][:, :TT], C.xt[:, c, :], ALU.add),
                  r=[("ps", b), ("xt",)], w=[("xt",)])
        fm_linear(C, W_r, 0, D, KC, C.xn, xn_key, epi)
        S.add("sp", lambda e, t0=t0: e.dma_start(out=xo_r[:, :, t0:t0 + TT], in_=C.xt[:]),
              r=[("xt",)], w=[("dram", "xout")], slot="xt_st")


def emit_mlstm_in(C, xin, T, gname, w_in, qT, kT, k_tm, v_tm, sog, gates, agf=None):
    S, TT = C.S, C.TT
    pending = []
    tkeys = {}
    ctx_extra(C)
    W_r = w_in.rearrange("(c p) n -> p c n", p=128)
    qT_r = qT.rearrange("(c p) t -> p c t", p=128)
    kT_r = kT.rearrange("(c p) t -> p c t", p=128)
    sog_r = sog.rearrange("(c p) t -> p c t", p=128)
    ktm_r = k_tm.rearrange("(b p) n -> p b n", p=128)
    vtm_r = v_tm.rearrange("(b p) n -> p b n", p=128)
    if not hasattr(C, "gst"):
        C.gst = S.sb("gst", [8, TT], F32)
    for t0 in range(0, T, TT):
        emit_norm(C, xin, t0, gname)
        cur = {}

        def fm_epi(kind, dst_r, scale, func):
            def epi(gi, j, nj, m, b):
                if j == 0:
                    cur["st"] = next_stage(C, kind)
                st, skey, sslot = cur["st"]
                S.add("act", lambda e: e.activation(out=st[:, j, :TT], in_=C.banks[b][:, :TT], func=func, scale=float(scale)),
                      r=[("ps", b)], w=[skey])
                if j == nj - 1:
                    S.add("sp", lambda e, t0=t0: e.dma_start(out=dst_r[:, gi * 4:gi * 4 + nj, t0:t0 + TT], in_=st[:, :nj, :TT]),
                          r=[skey], w=[("dram", "o", S.uid())], slot=sslot)
            return epi
        fm_linear(C, W_r, 0, 1024, KC, C.xn, xn_key, fm_epi("b", qT_r, 256 ** -0.5, AF.Copy))
        for fn in pending:
            fn()
        del pending[:]
        fm_linear(C, W_r, 1024, 1024, KC, C.xn, xn_key, fm_epi("b", kT_r, 1.0, AF.Copy))
        fm_linear(C, W_r, 4096, 2048, KC, C.xn, xn_key, fm_epi("f", sog_r, 1.0, AF.Sigmoid))

        def g_epi(gi, j, nj, m, b):
            S.add("act", lambda e: e.activation(out=C.gst[:, :], in_=C.banks[b][:8, :TT], func=AF.Copy),
                  r=[("ps", b)], w=[("gst",)])
            S.add("sp", lambda e, t0=t0: e.dma_start(out=gates[:, t0:t0 + TT], in_=C.gst[:, :]), r=[("gst",)], w=[("dram", "o", S.uid())], slot="gst")
        fm_linear(C, W_r, 6144, 8, KC, C.xn, xn_key, g_epi)

        def tm_epi(dst_r, name):
            def epi(gi, tb, gw, b):
                if tb == 0:
                    cur["st"] = next_stage(C, "b")
                st, skey, sslot = cur["st"]
                eng = "dve" if tb % 2 == 0 else "act"
                if eng == "dve":
                    S.add("dve", lambda e: e.tensor_copy(st[:, tb, :gw], C.banks[b][:, :gw]), r=[("ps", b)], w=[skey])
                else:
                    S.add("act", lambda e: e.activation(out=st[:, tb, :gw], in_=C.banks[b][:, :gw], func=AF.Copy), r=[("ps", b)], w=[skey])
                if tb == TT // 128 - 1:
                    tb0 = t0 // 128
                    dkey = ("dram", "o", S.uid())
                    tkeys.setdefault((name, t0), []).append(dkey)
                    S.add("sp", lambda e: e.dma_start(out=dst_r[:, tb0:tb0 + TT // 128, gi * 512:gi * 512 + gw], in_=st[:, :TT // 128, :gw]),
                          r=[skey], w=[dkey], slot=sslot)
            return epi
        tm_linear(C, W_r, 1024, 1024, tm_epi(ktm_r, "ktm"))
        tm_linear(C, W_r, 2048, 2048, tm_epi(vtm_r, "vtm"))
        if agf is not None:
            ti = t0 // TT
            pending.append(lambda ti=ti, t0=t0: agf(S, "ktm", 512, [ti], tkeys[("ktm", t0)]))
            pending.append(lambda ti=ti, t0=t0: agf(S, "vtm", 256, [2 * ti, 2 * ti + 1], tkeys[("vtm", t0)]))
    for fn in pending:
        fn()


def emit_headproj(C, xin, T, gname, W, c0, hgname, scale, outT):
    S, TT = C.S, C.TT
    ctx_extra(C)
    W_r = W.rearrange("(c p) n -> p c n", p=128)
    o_r = outT.rearrange("(c p) t -> p c t", p=128)
    cur = {}
    for t0 in range(0, T, TT):
        emit_norm(C, xin, t0, gname)

        def epi(gi, j, nj, m, b):
            if j == 0:
                cur["st"] = next_stage(C, "b")
            st, skey, sslot = cur["st"]
            headnorm(C, b, hgname, scale, st[:, j, :TT], skey)
            if j == nj - 1:
                S.add("sp", lambda e, t0=t0: e.dma_start(out=o_r[:, gi * 4:gi * 4 + nj, t0:t0 + TT], in_=st[:, :nj, :TT]),
                      r=[skey], w=[("dram", "o", S.uid())], slot=sslot)
        fm_linear(C, W_r, c0, 2048, KC, C.xn, xn_key, epi)
        yield t0


def emit_kv(C, xin, T, gname, w_kv, kT, v_tm, agf=None):
    S, TT = C.S, C.TT
    ctx_extra(C)
    W_r = w_kv.rearrange("(c p) n -> p c n", p=128)
    vtm_r = v_tm.rearrange("(b p) n -> p b n", p=128)
    cur = {}
    tkeys = {}
    pend = []
    for t0 in emit_headproj(C, xin, T, gname, w_kv, 0, "g_k", 1.0, kT):
        if agf is not None and t0 > 0:
            for fn in pend:
                fn()
            del pend[:]
        def epi(gi, tb, gw, b):
            if tb == 0:
                cur["st"] = next_stage(C, "b")
            st, skey, sslot = cur["st"]
            if tb % 2 == 0:
                S.add("dve", lambda e: e.tensor_copy(st[:, tb, :gw], C.banks[b][:, :gw]), r=[("ps", b)], w=[skey])
            else:
                S.add("act", lambda e: e.activation(out=st[:, tb, :gw], in_=C.banks[b][:, :gw], func=AF.Copy), r=[("ps", b)], w=[skey])
            if tb == TT // 128 - 1:
                tb0 = t0 // 128
                dkey = ("dram", "o", S.uid())
                tkeys.setdefault(t0, []).append(dkey)
                S.add("sp", lambda e: e.dma_start(out=vtm_r[:, tb0:tb0 + TT // 128, gi * 512:gi * 512 + gw], in_=st[:, :TT // 128, :gw]),
                      r=[skey], w=[dkey], slot=sslot)
        tm_linear(C, W_r, 2048, 2048, epi)
        if agf is not None:
            ti = t0 // TT
            pend.append(lambda ti=ti, t0=t0: agf(S, "v", 256, [2 * ti, 2 * ti + 1], tkeys[t0]))
    for fn in pend:
        fn()


def emit_mlstm_core(S, SEQ, qT, kT, k_tm, v_tm, gi, gf, bif, ghead, hout, SEG=1024, dbg=None, gth=None, out_ag=None):
    nc = S.nc
    CH = 128
    NCH = SEG // CH
    DK, DV = 256, 512
    sb, ps = S.sb, S.ps
    banks = [ps("bank%d" % i, [128, 512], F32) for i in range(8)]
    ones_bf = sb("ones_bf", [128, 128], BF16)
    ones_f = sb("ones_f", [128, 128], F32)
    tri = sb("tri", [128, 128], F32)
    S.add("dve", lambda e: e.memset(ones_bf[:], 1.0), w=["ones_bf"])
    S.add("dve", lambda e: e.memset(ones_f[:], 1.0), w=["ones_f"])
    S.add("pool", lambda e: e.memset(tri[:], 1.0), w=["tri"])
    S.add("pool", lambda e: e.affine_select(out=tri[:], in_=tri[:], pattern=[[1, 128]], compare_op=ALU.is_ge, fill=0.0,
                                            base=0, channel_multiplier=-1), r=["tri"], w=["tri"])
    bt = sb("bif_sb", [1, 2], F32)
    S.add("sp", lambda e: e.dma_start(out=bt[:], in_=bif), w=["bif"], slot="bif")
    nbf = sb("nbf", [1, 1], F32)
    S.add("dve", lambda e: e.tensor_scalar(nbf[:], bt[0:1, 1:2], -1.0, None, ALU.mult), r=["bif"], w=["nbf"])
    gh = sb("gh_sb", [128, 4], F32)
    S.add("sp", lambda e: e.dma_start(out=gh[:], in_=ghead), w=["gh"], slot="gh")
    rows = {n: sb("r_" + n, [1, SEG], F32) for n in ("gi", "gf", "e", "lf", "Bn", "U", "G", "nG", "wi", "cl", "we", "one")}
    S.add("dve", lambda e: e.memset(rows["one"][:], 1.0), w=["r_one"])
    carry = sb("carry", [1, 4], F32)
    S.add("dve", lambda e: e.memset(carry[:], 0.0), w=["carry"])
    bc = {n: sb("bc_" + n, [128, SEG], F32) for n in ("nG", "wi", "cl")}
    cols = sb("cols", [128, 3 * NCH], F32)
    qt = [sb("qt%d" % i, [128, 2, SEG], BF16) for i in range(2)]
    kt = [sb("kt%d" % i, [128, 2, SEG], BF16) for i in range(2)]
    ktm = [sb("ktm%d" % i, [128, NCH, DK], BF16) for i in range(2)]
    vtm = [sb("vtm%d" % i, [128, NCH, DV], BF16) for i in range(2)]
    hst = [sb("hst%d" % i, [128, 4, SEG], F32) for i in range(2)]
    PT = sb("PT", [128, 128], F32)
    PTm = sb("PTm", [128, 128], F32)
    AT = sb("AT", [128, 128], BF16)
    qw = sb("qw", [128, 2, 128], BF16)
    dd = sb("dd", [128, 128], F32)
    rd = sb("rd", [128, 128], F32)
    hT = sb("hT", [128, 4, 128], F32)
    hsq = sb("hsq", [128, 4, 128], BF16)
    hr = sb("hr", [128, 128], F32)
    hr2 = sb("hr2", [128, 128], F32)
    kw = sb("kw", [128, DK], BF16)
    Cf = sb("Cf", [128, 2, DV], F32)
    Cb = sb("Cb", [128, 2, DV], BF16)
    nf = sb("nf", [128, 2], F32)
    nrep = sb("nrep", [128, 2, 128], BF16)
    S.add("dve", lambda e: e.memset(Cf[:], 0.0), w=["Cf"])
    S.add("dve", lambda e: e.memset(nf[:], 0.0), w=["nf"])
    S.add("pool", lambda e: e.memset(Cb[:], 0.0), w=["Cb"])
    S.add("pool", lambda e: e.memset(nrep[:], 0.0), w=["nrep"])
    ho_r = hout.rearrange("(c p) t -> p c t", p=128) if out_ag is None else None
    R = rows
    ag_pending = []
    if gth is None:
        qT_r = qT.rearrange("(c p) t -> p c t", p=128)
        kT_r = kT.rearrange("(c p) t -> p c t", p=128)
        ktm_r = k_tm.rearrange("(b p) n -> p b n", p=128)
        vtm_r = v_tm.rearrange("(b p) n -> p b n", p=128)
    else:
        g8 = sb("g8", [1, 8, SEG], F32)
        oh = sb("oh", [1, 4], F32)
        S.add("sp", lambda e: e.dma_start(out=oh[:], in_=gth["onehot"]), w=["oh"], slot="oh")
    for sg in range(SEQ // SEG):
        t0 = sg * SEG
        sl = sg % 2
        if gth is None:
            for dk in range(2):
                S.add("sp", lambda e, t0=t0, sl=sl, dk=dk: e.dma_start(out=qt[sl][:, dk, :], in_=qT_r[:, dk, t0:t0 + SEG]), w=[("qt", sl, dk)], slot="qt%d_%d" % (sl, dk))
                S.add("sp", lambda e, t0=t0, sl=sl, dk=dk: e.dma_start(out=kt[sl][:, dk, :], in_=kT_r[:, dk, t0:t0 + SEG]), w=[("kt", sl, dk)], slot="kt%d_%d" % (sl, dk))
            for c in range(NCH):
                S.add("sp", lambda e, t0=t0, sl=sl, c=c: e.dma_start(out=ktm[sl][:, c, :], in_=ktm_r[:, t0 // 128 + c, :]), w=[("ktm", sl, c)], slot="ktm%d_%d" % (sl, c))
                S.add("sp", lambda e, t0=t0, sl=sl, c=c: e.dma_start(out=vtm[sl][:, c, :], in_=vtm_r[:, t0 // 128 + c, :]), w=[("vtm", sl, c)], slot="vtm%d_%d" % (sl, c))
            S.add("sp", lambda e, t0=t0: e.dma_start(out=R["gi"][:], in_=gi[:, t0:t0 + SEG]), w=["r_gi"], slot="r_gi")
            S.add("sp", lambda e, t0=t0: e.dma_start(out=R["gf"][:], in_=gf[:, t0:t0 + SEG]), w=["r_gf"], slot="r_gf")
        else:
            ixt, ixkey = gth["ixt"], gth["ixkey"]
            for dk in range(2):
                col = gth["MQ"] + sg * 2 + dk
                S.add("pool", lambda e, sl=sl, dk=dk, col=col: e.indirect_dma_start(
                    out=qt[sl][:, dk, :], out_offset=None, in_=gth["q_view"],
                    in_offset=bass.IndirectOffsetOnAxis(ap=ixt[:, col:col + 1], axis=0)),
                    r=[ixkey] + list(gth["agk"]["q"]), w=[("qt", sl, dk)], slot="qt%d_%d" % (sl, dk))
                S.add("pool", lambda e, sl=sl, dk=dk, col=col: e.indirect_dma_start(
                    out=kt[sl][:, dk, :], out_offset=None, in_=gth["k_view"],
                    in_offset=bass.IndirectOffsetOnAxis(ap=ixt[:, col:col + 1], axis=0)),
                    r=[ixkey] + list(gth["agk"]["k"]), w=[("kt", sl, dk)], slot="kt%d_%d" % (sl, dk))
            for c in range(NCH):
                col = gth["MK"] + sg * NCH + c
                colv = gth["MV"] + sg * NCH + c
                S.add("pool", lambda e, sl=sl, c=c, col=col: e.indirect_dma_start(
                    out=ktm[sl][:, c, :], out_offset=None, in_=gth["ktm_view"],
                    in_offset=bass.IndirectOffsetOnAxis(ap=ixt[:, col:col + 1], axis=0)),
                    r=[ixkey] + list(gth["agk"]["ktm"]), w=[("ktm", sl, c)], slot="ktm%d_%d" % (sl, c))
                S.add("pool", lambda e, sl=sl, c=c, colv=colv: e.indirect_dma_start(
                    out=vtm[sl][:, c, :], out_offset=None, in_=gth["vtm_view"],
                    in_offset=bass.IndirectOffsetOnAxis(ap=ixt[:, colv:colv + 1], axis=0)),
                    r=[ixkey] + list(gth["agk"]["vtm"]), w=[("vtm", sl, c)], slot="vtm%d_%d" % (sl, c))
            srank, half = sg // 2, sg % 2
            gsrc = gth["gates_all"][srank * 8:(srank + 1) * 8, half * SEG:(half + 1) * SEG].rearrange("(o r) t -> o r t", o=1)
            S.add("sp", lambda e, gsrc=gsrc: e.dma_start(out=g8[:], in_=gsrc), r=list(gth["agk"]["g"]), w=["g8"], slot="g8")
            for gi_, (dst, off) in enumerate((("gi", 0), ("gf", 4))):
                S.add("dve", lambda e, dst=dst, off=off: e.tensor_scalar(R[dst][:], g8[0:1, off, :], oh[0:1, 0:1], None, ALU.mult),
                      r=["g8", "oh"], w=["r_" + dst])
                for j in range(1, 4):
                    S.add("dve", lambda e, dst=dst, off=off, j=j: e.scalar_tensor_tensor(R[dst][:], g8[0:1, off + j, :], oh[0:1, j:j + 1], R[dst][:], ALU.mult, ALU.add),
                          r=["g8", "oh", "r_" + dst], w=["r_" + dst])
        for fn in ag_pending:
            fn()
        del ag_pending[:]
        S.add("act", lambda e: e.activation(out=R["e"][:], in_=R["gf"][:], func=AF.Exp, scale=-1.0, bias=nbf[0:1, 0:1]),
              r=["r_gf", "nbf"], w=["r_e"])
        S.add("act", lambda e: e.activation(out=R["lf"][:], in_=R["e"][:], func=AF.Ln, bias=ones_f[0:1, 0:1]),
              r=["r_e", "ones_f"], w=["r_lf"])
        S.add("dve", lambda e: e.tensor_tensor_scan(R["Bn"][:], R["one"][:], R["lf"][:], carry[0:1, 0:1], ALU.mult, ALU.add),
              r=["r_lf", "carry", "r_one"], w=["r_Bn"])
        S.add("dve", lambda e: e.scalar_tensor_tensor(R["U"][:], R["gi"][:], bt[0:1, 0:1], R["Bn"][:], ALU.add, ALU.add),
              r=["r_gi", "bif", "r_Bn"], w=["r_U"])
        S.add("dve", lambda e: e.tensor_tensor_scan(R["G"][:], R["U"][:], R["U"][:], carry[0:1, 1:2], ALU.max, ALU.max),
              r=["r_U", "carry"], w=["r_G"])
        S.add("dve", lambda e: e.tensor_scalar(R["nG"][:], R["G"][:], -1.0, None, ALU.mult), r=["r_G"], w=["r_nG"])
        S.add("dve", lambda e: e.tensor_tensor(R["cl"][:], R["Bn"][:], R["G"][:], ALU.subtract), r=["r_Bn", "r_G"], w=["r_cl"])
        S.add("act", lambda e: e.activation(out=R["cl"][:], in_=R["cl"][:], func=AF.Exp), r=["r_cl"], w=["r_cl"])
        for c in range(NCH):
            a, b_ = c * CH, (c + 1) * CH
            gprev = carry[0:1, 1:2] if c == 0 else R["G"][0:1, a - 1:a]
            S.add("act", lambda e, a=a, b_=b_, gprev=gprev: e.activation(out=R["wi"][0:1, a:b_], in_=R["nG"][0:1, a:b_], func=AF.Exp, bias=gprev),
                  r=["r_nG", "r_G", "carry"], w=["r_wi"])
            S.add("act", lambda e, a=a, b_=b_: e.activation(out=R["we"][0:1, a:b_], in_=R["U"][0:1, a:b_], func=AF.Exp, bias=R["nG"][0:1, b_ - 1:b_]),
                  r=["r_nG", "r_U"], w=["r_we"])
        for n in ("nG", "wi", "cl"):
            for h in range(SEG // 512):
                S.add("pe", lambda e, n=n, h=h: e.matmul(banks[7][:, :], ones_f[0:1, :], R[n][0:1, h * 512:(h + 1) * 512], start=True, stop=True),
                      r=["r_" + n, "ones_f"], w=[("ps", 7)])
                S.add("act", lambda e, n=n, h=h: e.activation(out=bc[n][:, h * 512:(h + 1) * 512], in_=banks[7][:, :], func=AF.Copy),
                      r=[("ps", 7)], w=["bc_" + n])
        for c in range(NCH):
            a, b_ = c * CH, (c + 1) * CH
            S.add("pe", lambda e, c=c, a=a, b_=b_: e.matmul(banks[7][:, c:c + 1], R["U"][0:1, a:b_], ones_f[0:1, 0:1], start=True, stop=True),
                  r=["r_U", "ones_f"], w=[("ps", 7)])
            S.add("pe", lambda e, c=c, a=a, b_=b_: e.matmul(banks[7][:, NCH + c:NCH + c + 1], R["we"][0:1, a:b_], ones_f[0:1, 0:1], start=True, stop=True),
                  r=["r_we", "ones_f"], w=[("ps", 7)])
            S.add("pe", lambda e, c=c, b_=b_: e.matmul(banks[7][:, 2 * NCH + c:2 * NCH + c + 1], ones_f[0:1, :], R["wi"][0:1, b_ - 1:b_], start=True, stop=True),
                  r=["r_wi", "ones_f"], w=[("ps", 7)])
        S.add("dve", lambda e: e.tensor_copy(cols[:], banks[7][:, :3 * NCH]), r=[("ps", 7)], w=["cols"])
        S.add("dve", lambda e: e.tensor_copy(carry[0:1, 0:1], R["Bn"][0:1, SEG - 1:SEG]), r=["r_Bn", "r_wi", "r_we"], w=["carry"])
        S.add("dve", lambda e: e.tensor_copy(carry[0:1, 1:2], R["G"][0:1, SEG - 1:SEG]), r=["r_G", "r_wi", "r_we"], w=["carry"])
        if dbg is not None and sg == 0:
            S.add("sp", lambda e: e.dma_start(out=dbg["nG"], in_=bc["nG"][:]), r=["bc_nG"], slot="dbg0")
            S.add("sp", lambda e: e.dma_start(out=dbg["wi"], in_=bc["wi"][:]), r=["bc_wi"], slot="dbg1")
            S.add("sp", lambda e: e.dma_start(out=dbg["cl"], in_=bc["cl"][:]), r=["bc_cl"], slot="dbg2")
            S.add("sp", lambda e: e.dma_start(out=dbg["cols"], in_=cols[:]), r=["cols"], slot="dbg3")
            S.add("sp", lambda e: e.dma_start(out=dbg["tri"], in_=tri[:]), r=["tri"], slot="dbg4")
        for c in range(NCH):
            a, b_ = c * CH, (c + 1) * CH
            qs, ks, kms, vs = qt[sl], kt[sl], ktm[sl], vtm[sl]
            for dk in range(2):
                S.add("pe", lambda e, dk=dk, a=a, b_=b_, ks=ks, qs=qs: e.matmul(banks[0][:, :128], ks[:, dk, a:b_], qs[:, dk, a:b_], start=(dk == 0), stop=(dk == 1)),
                      r=[("kt", sl, dk), ("qt", sl, dk)], w=[("ps", 0)])
            S.add("act", lambda e, a=a, b_=b_, c=c: e.activation(out=PT[:], in_=bc["nG"][:, a:b_], func=AF.Exp, bias=cols[:, c:c + 1]),
                  r=["bc_nG", "cols"], w=["PT"])
            S.add(PENG, lambda e: e.tensor_tensor(PTm[:], PT[:], tri[:], ALU.mult), r=["PT", "tri"], w=["PTm"])
            S.add("dve", lambda e: e.tensor_tensor(AT[:], banks[0][:, :128], PTm[:], ALU.mult), r=[("ps", 0), "PTm"], w=["AT"])
            for dk in range(2):
                S.add("dve", lambda e, dk=dk, a=a, b_=b_, qs=qs: e.tensor_tensor(qw[:, dk, :], qs[:, dk, a:b_], bc["wi"][:, a:b_], ALU.mult),
                      r=[("qt", sl, dk), "bc_wi"], w=["qw"])
            for j in range(4):
                S.add("pe", lambda e, j=j, c=c, vs=vs: e.matmul(banks[1][:, j * 128:(j + 1) * 128], vs[:, c, j * 128:(j + 1) * 128], AT[:], start=True, stop=False),
                      r=[("vtm", sl, c), "AT"], w=[("ps", 1)])
                for dk in range(2):
                    S.add("pe", lambda e, j=j, dk=dk: e.matmul(banks[1][:, j * 128:(j + 1) * 128], Cb[:, dk, j * 128:(j + 1) * 128], qw[:, dk, :], start=False, stop=(dk == 1)),
                          r=["Cb", "qw"], w=[("ps", 1)])
            S.add("pe", lambda e: e.matmul(banks[2][:, :128], ones_bf[:], AT[:], start=True, stop=False), r=["ones_bf", "AT"], w=[("ps", 2)])
            for dk in range(2):
                S.add("pe", lambda e, dk=dk: e.matmul(banks[2][:, :128], nrep[:, dk, :], qw[:, dk, :], start=False, stop=(dk == 1)),
                      r=["nrep", "qw"], w=[("ps", 2)])
            S.add("act", lambda e: e.activation(out=dd[:], in_=banks[2][:, :128], func=AF.Abs), r=[("ps", 2)], w=["dd"])
            S.add("dve", lambda e, a=a, b_=b_: e.tensor_tensor(dd[:], dd[:], bc["cl"][:, a:b_], ALU.max), r=["dd", "bc_cl"], w=["dd"])
            S.add("dve", lambda e: e.reciprocal(rd[:], dd[:]), r=["dd"], w=["rd"])
            for j in range(4):
                S.add("dve", lambda e, j=j: e.tensor_tensor(hT[:, j, :], banks[1][:, j * 128:(j + 1) * 128], rd[:], ALU.mult),
                      r=[("ps", 1), "rd"], w=["hT"])
            S.add("act", lambda e: e.activation(out=hsq[:], in_=hT[:], func=AF.Square), r=["hT"], w=["hsq"])
            for j in range(4):
                S.add("pe", lambda e, j=j: e.matmul(banks[3][:, :128], ones_bf[:], hsq[:, j, :], start=(j == 0), stop=(j == 3)),
                      r=["ones_bf", "hsq"], w=[("ps", 3)])
            S.add("dve", lambda e: e.tensor_scalar(hr[:], banks[3][:, :128], 1.0 / DV, EPS, ALU.mult, ALU.add), r=[("ps", 3)], w=["hr"])
            S.add("act", lambda e: e.activation(out=hr2[:], in_=hr[:], func=AF.Ln), r=["hr"], w=["hr2"])
            S.add("act", lambda e: e.activation(out=hr[:], in_=hr2[:], func=AF.Exp, scale=-0.5), r=["hr2"], w=["hr"])
            for j in range(4):
                S.add("dve", lambda e, j=j, a=a, b_=b_, sl=sl: e.scalar_tensor_tensor(hst[sl][:, j, a:b_], hT[:, j, :], gh[:, j:j + 1], hr[:], ALU.mult, ALU.mult),
                      r=["hT", "hr", "gh"], w=[("hst", sl)])
            S.add("dve", lambda e, c=c, kms=kms: e.tensor_scalar(kw[:], kms[:, c, :], cols[:, NCH + c:NCH + c + 1], None, ALU.mult),
                  r=[("ktm", sl, c), "cols"], w=["kw"])
            for dk in range(2):
                S.add("pe", lambda e, dk=dk, c=c, vs=vs: e.matmul(banks[4 + dk][:, :], kw[:, dk * 128:(dk + 1) * 128], vs[:, c, :], start=True, stop=True),
                      r=["kw", ("vtm", sl, c)], w=[("ps", 4 + dk)])
                S.add("pe", lambda e, dk=dk: e.matmul(banks[6][:, dk:dk + 1], kw[:, dk * 128:(dk + 1) * 128], ones_bf[:, 0:1], start=True, stop=True),
                      r=["kw", "ones_bf"], w=[("ps", 6)])
            for dk in range(2):
                S.add("dve", lambda e, dk=dk, c=c: e.scalar_tensor_tensor(Cf[:, dk, :], Cf[:, dk, :], cols[:, 2 * NCH + c:2 * NCH + c + 1], banks[4 + dk][:, :], ALU.mult, ALU.add),
                      r=["Cf", "cols", ("ps", 4 + dk), "Cb"], w=["Cf"])
            S.add("dve", lambda e, c=c: e.scalar_tensor_tensor(nf[:], nf[:], cols[:, 2 * NCH + c:2 * NCH + c + 1], banks[6][:, 0:2], ALU.mult, ALU.add),
                  r=["nf", "cols", ("ps", 6)], w=["nf"])
            S.add("act", lambda e: e.activation(out=Cb[:], in_=Cf[:], func=AF.Copy), r=["Cf"], w=["Cb"])
            for dk in range(2):
                S.add(PENG, lambda e, dk=dk: e.tensor_scalar(nrep[:, dk, :], ones_f[:], nf[:, dk:dk + 1], None, ALU.mult),
                      r=["nf", "ones_f"], w=["nrep"])
        if out_ag is None:
            S.add("sp", lambda e, t0=t0, sl=sl: e.dma_start(out=ho_r[:, :, t0:t0 + SEG], in_=hst[sl][:]), r=[("hst", sl)], w=[("dram", "ho", sg)], slot="hst%d" % sl)
        else:
            dst = out_ag["h_b"][sg * 512:(sg + 1) * 512, :].rearrange("(c p) t -> p c t", p=128)
            S.add("sp", lambda e, dst=dst, sl=sl: e.dma_start(out=dst, in_=hst[sl][:]), r=[("hst", sl)], w=[("dram", "ho", sg)], slot="hst%d" % sl)
            ag_pending.append(lambda sg=sg: out_ag["agf"](S, "a", 256, [2 * sg, 2 * sg + 1], [("dram", "ho", sg)]))
    for fn in ag_pending:
        fn()


def build_mlstm_core(SEQ, debug=False):
    nc = bass.Bass("TRN2", target_bir_lowering=False)
    qT = nc.dram_tensor("qT", [256, SEQ], BF16, kind="ExternalInput").ap()
    kT = nc.dram_tensor("kT", [256, SEQ], BF16, kind="ExternalInput").ap()
    k_tm = nc.dram_tensor("k_tm", [SEQ, 256], BF16, kind="ExternalInput").ap()
    v_tm = nc.dram_tensor("v_tm", [SEQ, 512], BF16, kind="ExternalInput").ap()
    gi = nc.dram_tensor("gi", [1, SEQ], F32, kind="ExternalInput").ap()
    gf = nc.dram_tensor("gf", [1, SEQ], F32, kind="ExternalInput").ap()
    bif = nc.dram_tensor("bif", [1, 2], F32, kind="ExternalInput").ap()
    ghead = nc.dram_tensor("ghead", [128, 4], F32, kind="ExternalInput").ap()
    hout = nc.dram_tensor("hout", [512, SEQ], F32, kind="ExternalOutput").ap()
    S = Sched(nc)
    dbg = None
    if debug:
        dbg = {n: nc.dram_tensor("dbg_" + n, [128, 1024], F32, kind="ExternalOutput").ap() for n in ("nG", "wi", "cl")}
        dbg["cols"] = nc.dram_tensor("dbg_cols", [128, 24], F32, kind="ExternalOutput").ap()
        dbg["tri"] = nc.dram_tensor("dbg_tri", [128, 128], F32, kind="ExternalOutput").ap()
    emit_mlstm_core(S, SEQ, qT, kT, k_tm, v_tm, gi, gf, bif, ghead, hout, dbg=dbg)
    S.emit()
    return nc, S


def emit_sb_core(S, NH, SEQ, qT, kT, v_tm, oT, gth=None, out_ag=None):
    sb, ps = S.sb, S.ps
    NB = SEQ // 128
    NQB = SEQ // 512
    zb = [ps("zb%d" % i, [128, 512], F32) for i in range(2)]
    ab = [ps("ab%d" % i, [128, 512], F32) for i in range(2)]
    ob = [ps("ob%d" % i, [128, 512], F32) for i in range(2)]
    ones_f = sb("ones_f", [128, 128], F32)
    ntri = sb("ntri", [128, 128], BF16)
    nones = sb("nones", [128, 128], BF16)
    smask = sb("smask", [128, 128], BF16)
    tmpf = sb("tmpf", [128, 128], F32)
    S.add("dve", lambda e: e.memset(ones_f[:], 1.0), w=["ones_f"])
    S.add("dve", lambda e: e.memset(nones[:], -1.0), w=["nones"])
    S.add("pool", lambda e: e.memset(tmpf[:], -1.0), w=["tmpf"])
    S.add("pool", lambda e: e.affine_select(out=tmpf[:], in_=tmpf[:], pattern=[[-1, 128]], compare_op=ALU.is_ge, fill=0.0,
                                            base=0, channel_multiplier=1), r=["tmpf"], w=["tmpf"])
    S.add("dve", lambda e: e.tensor_copy(ntri[:], tmpf[:]), r=["tmpf"], w=["ntri"])
    tmpg = sb("tmpg", [128, 128], F32)
    S.add("pool", lambda e: e.memset(tmpg[:], 1.0), w=["tmpg"])
    S.add("pool", lambda e: e.affine_select(out=tmpg[:], in_=tmpg[:], pattern=[[1, 128]], compare_op=ALU.is_ge, fill=0.0,
                                            base=-1, channel_multiplier=-1), r=["tmpg"], w=["tmpg"])
    S.add("dve", lambda e: e.tensor_copy(smask[:], tmpg[:]), r=["tmpg"], w=["smask"])
    qhs = [sb("qh%d" % i, [128, SEQ], BF16) for i in range(2)]
    khs = [sb("kh%d" % i, [128, SEQ], BF16) for i in range(2)]
    vhs = [sb("vh%d" % i, [128, NB, 128], BF16) for i in range(2)]
    KBATCH = 12
    NBUF = 2 * KBATCH + 2
    e_sb = [sb("e_sb%d" % i, [128, 512], F32) for i in range(2)] if not USE_SOFTPLUS else None
    sp_bf = [sb("sp_bf%d" % i, [128, 512], BF16) for i in range(NBUF)]
    A_bf = [sb("A_bf%d" % i, [128, 512], BF16) for i in range(NBUF)]
    RS_f = sb("RS_f", [128, 512], F32)
    RS_b = [sb("RS_b%d" % i, [128, 512], BF16) for i in range(NBUF)]
    ost = [sb("ost%d" % i, [128, 512], F32) for i in range(2)]
    TC = SEQ // 4

    its = []
    for h in range(NH):
        for QB in range(NQB):
            kb_hi = 4 * QB + 3
            for kb in range(kb_hi, -1, -1):
                its.append((h, QB, kb))

    def geom(n):
        h, QB, kb = its[n]
        r_ = kb - 4 * QB
        col0 = r_ * 128 if r_ >= 0 else 0
        return h, QB, kb, r_, col0, 512 - col0, kb == 4 * QB + 3, QB * 512

    def load_head(h):
        hp = h % 2
        qh, kh, vh = qhs[hp], khs[hp], vhs[hp]
        if gth is None:
            S.add("sp", lambda e, h=h: e.dma_start(out=qh[:], in_=qT[h]), w=[("qh", hp, i) for i in range(4)], slot="qh%d" % hp)
            S.add("sp", lambda e, h=h: e.dma_start(out=kh[:], in_=kT[h]), w=[("kh", hp, i) for i in range(4)], slot="kh%d" % hp)
            S.add("sp", lambda e, h=h: e.dma_start(out=vh[:], in_=v_tm[h].rearrange("(b p) d -> p b d", p=128)),
                  w=[("vh", hp, i) for i in range((NB + 7) // 8)], slot="vh%d" % hp)
        else:
            ixt, ixkey = gth["ixt"], gth["ixkey"]
            for s_ in range(4):
                col = gth["SQ"] + h * 4 + s_
                S.add("pool", lambda e, s_=s_, col=col: e.indirect_dma_start(
                    out=qh[:, s_ * TC:(s_ + 1) * TC], out_offset=None, in_=gth["q_view"],
                    in_offset=bass.IndirectOffsetOnAxis(ap=ixt[:, col:col + 1], axis=0)),
                    r=[ixkey] + list(gth["agk"]["q"]), w=[("qh", hp, s_)], slot="qh%d_%d" % (hp, s_))
                S.add("pool", lambda e, s_=s_, col=col: e.indirect_dma_start(
                    out=kh[:, s_ * TC:(s_ + 1) * TC], out_offset=None, in_=gth["k_view"],
                    in_offset=bass.IndirectOffsetOnAxis(ap=ixt[:, col:col + 1], axis=0)),
                    r=[ixkey] + list(gth["agk"]["k"]), w=[("kh", hp, s_)], slot="kh%d_%d" % (hp, s_))
            for blk in range(NB):
                col = gth["SV"] + h * NB + blk
                S.add("pool", lambda e, blk=blk, col=col: e.indirect_dma_start(
                    out=vh[:, blk, :], out_offset=None, in_=gth["v_view"],
                    in_offset=bass.IndirectOffsetOnAxis(ap=ixt[:, col:col + 1], axis=0)),
                    r=[ixkey] + list(gth["agk"]["v"]), w=[("vh", hp, blk // 8)], slot="vh%d_%d" % (hp, blk // 8), group=True)

    def part_a(n):
        h, QB, kb, r_, col0, ncol, first, q0 = geom(n)
        if n == 0:
            load_head(0)
        if (n == 0 or its[n - 1][0] != h) and h + 1 < NH:
            load_head(h + 1)
        i2, i3 = n % 2, n % NBUF
        hp = h % 2
        kblk = khs[hp][:, kb * 128:(kb + 1) * 128]
        qcols = qhs[hp][:, q0 + col0:q0 + 512]
        khk, qhk = ("kh", hp, (kb * 128) // TC), ("qh", hp, q0 // TC)
        if first:
            S.add("dve", lambda e: e.memset(RS_f[:], 0.0), w=["RS_f"])
        S.add("pe", lambda e: e.matmul(zb[i2][:, :ncol], kblk, qcols, start=True, stop=True), r=[khk, qhk], w=[("zb", i2)])
        if USE_SOFTPLUS:
            S.add("act", lambda e: e.activation(out=sp_bf[i3][:, :ncol], in_=zb[i2][:, :ncol], func=AF.Softplus),
                  r=[("zb", i2)], w=[("sp", i3)])
        else:
            S.add("act", lambda e: e.activation(out=e_sb[i2][:, :ncol], in_=zb[i2][:, :ncol], func=AF.Exp), r=[("zb", i2)], w=[("e_sb", i2)])
            S.add("act", lambda e: e.activation(out=sp_bf[i3][:, :ncol], in_=e_sb[i2][:, :ncol], func=AF.Ln, bias=ones_f[:, 0:1]),
                  r=[("e_sb", i2), "ones_f"], w=[("sp", i3)])
        if r_ >= 0:
            S.add("pool", lambda e: e.tensor_tensor(sp_bf[i3][:, :128], sp_bf[i3][:, :128], smask[:], ALU.mult),
                  r=[("sp", i3), "smask"], w=[("sp", i3)])
        if kb > 0:
            S.add("dve", lambda e: e.tensor_tensor(RS_f[:, col0:512], RS_f[:, col0:512], sp_bf[i3][:, :ncol], ALU.add),
                  r=[("sp", i3), "RS_f"], w=["RS_f"])
            nx = (n + 1) % NBUF
            S.add("dve", lambda e: e.tensor_copy(RS_b[nx][:], RS_f[:]), r=["RS_f"], w=[("RS_b", nx)])

    def part_b(n):
        h, QB, kb, r_, col0, ncol, first, q0 = geom(n)
        i2, i3 = n % 2, n % NBUF
        hp = h % 2
        kblk = khs[hp][:, kb * 128:(kb + 1) * 128]
        qcols = qhs[hp][:, q0 + col0:q0 + 512]
        khk, qhk = ("kh", hp, (kb * 128) // TC), ("qh", hp, q0 // TC)
        S.add("pe", lambda e: e.matmul(ab[i2][:, :ncol], kblk, qcols, start=True, stop=False), r=[khk, qhk], w=[("ab", i2)])
        S.add("pe", lambda e: e.matmul(ab[i2][:, :ncol], ntri[:], sp_bf[i3][:, :ncol], start=False, stop=first),
              r=["ntri", ("sp", i3)], w=[("ab", i2)])
        if not first:
            S.add("pe", lambda e: e.matmul(ab[i2][:, :ncol], nones[:], RS_b[i3][:, col0:512], start=False, stop=True),
                  r=["nones", ("RS_b", i3)], w=[("ab", i2)])
        S.add("act", lambda e: e.activation(out=A_bf[i3][:, :ncol], in_=ab[i2][:, :ncol], func=AF.Exp), r=[("ab", i2)], w=[("A", i3)])
        if r_ >= 0:
            S.add("pool", lambda e: e.tensor_tensor(A_bf[i3][:, :128], A_bf[i3][:, :128], smask[:], ALU.mult),
                  r=[("A", i3), "smask"], w=[("A", i3)])

    def part_c(n):
        h, QB, kb, r_, col0, ncol, first, q0 = geom(n)
        i3 = n % NBUF
        o_i = (h * NQB + QB) % 2
        hp = h % 2
        S.add("pe", lambda e: e.matmul(ob[o_i][:, col0:512], vhs[hp][:, kb, :], A_bf[i3][:, :ncol], start=first, stop=(kb == 0)),
              r=[("vh", hp, kb // 8), ("A", i3)], w=[("ob", o_i)])
        if kb == 0:
            S.add("dve", lambda e: e.tensor_copy(ost[o_i][:], ob[o_i][:]), r=[("ob", o_i)], w=[("ost", o_i)])
            if out_ag is None:
                S.add("sp", lambda e: e.dma_start(out=oT[h][:, q0:q0 + 512], in_=ost[o_i][:]),
                      r=[("ost", o_i)], w=[("dram", "o", h, QB)], slot="ost%d" % o_i)
            else:
                ck = h * 4 + QB // 4
                dst = out_ag["o_b"][ck * 128:(ck + 1) * 128, (QB % 4) * 512:(QB % 4 + 1) * 512]
                S.add("sp", lambda e: e.dma_start(out=dst, in_=ost[o_i][:]),
                      r=[("ost", o_i)], w=[("dram", "o", h, QB)], slot="ost%d" % o_i)
                if QB % 4 == 3:
                    ag_pending.append([n + 12, lambda: out_ag["agf"](S, "a", 128, [ck], [("dram", "o", h, QB - i_) for i_ in range(4)])])

    N = len(its)
    ag_pending = []
    nbat = (N + KBATCH - 1) // KBATCH
    for m in range(nbat + 2):
        for item in [it_ for it_ in ag_pending if it_[0] <= m * KBATCH]:
            item[1]()
            ag_pending.remove(item)
        for i_ in range(KBATCH):
            n = m * KBATCH + i_
            if n < N:
                part_a(n)
            nc_ = (m - 2) * KBATCH + i_
            if 0 <= m - 2 < nbat and nc_ < N:
                part_c(nc_)
        if 0 <= m - 1 < nbat:
            for n in range((m - 1) * KBATCH, min(N, m * KBATCH)):
                part_b(n)
    for item in ag_pending:
        item[1]()


def build_sb_core(NH, SEQ):
    nc = bass.Bass("TRN2", target_bir_lowering=False)
    qT = nc.dram_tensor("qT", [NH, 128, SEQ], BF16, kind="ExternalInput").ap()
    kT = nc.dram_tensor("kT", [NH, 128, SEQ], BF16, kind="ExternalInput").ap()
    v_tm = nc.dram_tensor("v_tm", [NH, SEQ, 128], BF16, kind="ExternalInput").ap()
    oT = nc.dram_tensor("oT", [NH, 128, SEQ], F32, kind="ExternalOutput").ap()
    S = Sched(nc)
    emit_sb_core(S, NH, SEQ, qT, kT, v_tm, oT)
    S.emit()
    return nc, S


def _vec(nc, name, n):
    return nc.dram_tensor(name, [128, n], F32, kind="ExternalInput").ap()


def build_ple(T, TT=512):
    nc = bass.Bass("TRN2", target_bir_lowering=False)
    xin = nc.dram_tensor("xin", [D, T], F32, kind="ExternalInput").ap()
    pT = nc.dram_tensor("pT", [256, T], F32, kind="ExternalInput").ap()
    g = _vec(nc, "g", KC)
    wpe = nc.dram_tensor("wpe", [256, D], F32, kind="ExternalInput").ap()
    wpg = nc.dram_tensor("wpg", [D, D], F32, kind="ExternalInput").ap()
    xout = nc.dram_tensor("xout", [D, T], F32, kind="ExternalOutput").ap()
    S = Sched(nc)
    C = Ctx(S, TT)
    C.load_vec("g", g, KC)
    emit_ple(C, xin, xout, pT, T, "g", wpe, wpg)
    S.emit()
    return nc, S


def build_proj_res(T, gated, TT=512):
    nc = bass.Bass("TRN2", target_bir_lowering=False)
    xin = nc.dram_tensor("xin", [D, T], F32, kind="ExternalInput").ap()
    aT = nc.dram_tensor("aT", [D, T], F32, kind="ExternalInput").ap()
    bT = nc.dram_tensor("bT", [D, T], F32, kind="ExternalInput").ap() if gated else None
    W = nc.dram_tensor("W", [D, D], F32, kind="ExternalInput").ap()
    xout = nc.dram_tensor("xout", [D, T], F32, kind="ExternalOutput").ap()
    S = Sched(nc)
    C = Ctx(S, TT)
    emit_proj_res(C, xin, xout, aT, bT, T, W)
    S.emit()
    return nc, S


def build_mlstm_in(T, TT=512):
    nc = bass.Bass("TRN2", target_bir_lowering=False)
    xin = nc.dram_tensor("xin", [D, T], F32, kind="ExternalInput").ap()
    g = _vec(nc, "g", KC)
    w_in = nc.dram_tensor("w_in", [D, 6152], F32, kind="ExternalInput").ap()
    qT = nc.dram_tensor("qT", [1024, T], BF16, kind="ExternalOutput").ap()
    kT = nc.dram_tensor("kT", [1024, T], BF16, kind="ExternalOutput").ap()
    k_tm = nc.dram_tensor("k_tm", [T, 1024], BF16, kind="ExternalOutput").ap()
    v_tm = nc.dram_tensor("v_tm", [T, 2048], BF16, kind="ExternalOutput").ap()
    sog = nc.dram_tensor("sog", [2048, T], F32, kind="ExternalOutput").ap()
    gates = nc.dram_tensor("gates", [8, T], F32, kind="ExternalOutput").ap()
    S = Sched(nc)
    C = Ctx(S, TT)
    C.load_vec("g", g, KC)
    emit_mlstm_in(C, xin, T, "g", w_in, qT, kT, k_tm, v_tm, sog, gates)
    S.emit()
    return nc, S


def build_kv(T, TT=512):
    nc = bass.Bass("TRN2", target_bir_lowering=False)
    xin = nc.dram_tensor("xin", [D, T], F32, kind="ExternalInput").ap()
    g = _vec(nc, "g", KC)
    gk = _vec(nc, "g_k", 1)
    w_kv = nc.dram_tensor("w_kv", [D, 4096], F32, kind="ExternalInput").ap()
    kT = nc.dram_tensor("kT", [2048, T], BF16, kind="ExternalOutput").ap()
    v_tm = nc.dram_tensor("v_tm", [T, 2048], BF16, kind="ExternalOutput").ap()
    S = Sched(nc)
    C = Ctx(S, TT)
    C.load_vec("g", g, KC)
    C.load_vec("g_k", gk, 1)
    emit_kv(C, xin, T, "g", w_kv, kT, v_tm)
    S.emit()
    return nc, S


def build_q(T, TT=512):
    nc = bass.Bass("TRN2", target_bir_lowering=False)
    xin = nc.dram_tensor("xin", [D, T], F32, kind="ExternalInput").ap()
    g = _vec(nc, "g", KC)
    gq = _vec(nc, "g_q", 1)
    w_q = nc.dram_tensor("w_q", [D, D], F32, kind="ExternalInput").ap()
    qT = nc.dram_tensor("qT", [2048, T], BF16, kind="ExternalOutput").ap()
    S = Sched(nc)
    C = Ctx(S, TT)
    C.load_vec("g", g, KC)
    C.load_vec("g_q", gq, 1)
    for _ in emit_headproj(C, xin, T, "g", w_q, 0, "g_q", 128 ** -0.5, qT):
        pass
    S.emit()
    return nc, S


def build_ffn(T, TT=512):
    nc = bass.Bass("TRN2", target_bir_lowering=False)
    xin = nc.dram_tensor("xin", [D, T], F32, kind="ExternalInput").ap()
    g = nc.dram_tensor("g", [128, KC], F32, kind="ExternalInput").ap()
    wg = nc.dram_tensor("wg", [D, DFF], F32, kind="ExternalInput").ap()
    wu = nc.dram_tensor("wu", [D, DFF], F32, kind="ExternalInput").ap()
    wd = nc.dram_tensor("wd", [DFF, D], F32, kind="ExternalInput").ap()
    xout = nc.dram_tensor("xout", [D, T], F32, kind="ExternalOutput").ap()
    S = Sched(nc)
    C = Ctx(S, TT)
    C.load_vec("g", g, KC)
    emit_ffn(C, xin, xout, T, "g", wg, wu, wd)
    S.emit()
    return nc, S


RG = [[0, 1, 2, 3], [4, 5, 6, 7]]
IX = dict(MQ=0, MK=16, MV=80, PAM=144, PAS=208, SQ=272, SV=288)
NIDX = 544
TCORE = 2048
SEQLEN = 8192
I32 = mybir.dt.int32


def build_fused(plan="full"):
    nc = bass.Bass("TRN2", target_bir_lowering=False)
    T = TCORE
    ext_names = []
    _cache = {}
    shapes = {
        "xT": ([D, T], F32), "pT": ([4, 256, T], F32), "ffn_norm": ([4, 2, 128, KC], F32),
        "ffn_w_gate": ([4, 2, D, DFF], F32), "ffn_w_up": ([4, 2, D, DFF], F32), "ffn_w_down": ([4, 2, DFF, D], F32),
        "mix_norm": ([4, 128, KC], F32), "mlstm_w_in": ([2, D, 6152], F32), "bif": ([2, 1, 2], F32),
        "ghead": ([2, 128, 4], F32), "mlstm_w_out": ([2, D, D], F32), "kv_norm": ([128, KC], F32),
        "sb_w_kv": ([D, 4096], F32), "g_k": ([128, 1], F32), "sb_w_q": ([2, D, D], F32), "g_q": ([2, 128, 1], F32),
        "sb_w_o": ([2, D, D], F32), "ple_norm": ([4, 128, KC], F32), "ple_w_proj": ([4, 256, D], F32),
        "ple_w_gate": ([4, D, D], F32), "idx": ([128, NIDX], I32), "onehot": ([1, 4], F32),
    }

    class _Ext:
        def __getattr__(self, name):
            if name not in _cache:
                shp, dt = shapes[name]
                _cache[name] = nc.dram_tensor(name, shp, dt, kind="ExternalInput").ap()
                ext_names.append(name)
            return _cache[name]
    E = _Ext()
    nc._ext_names = ext_names
    outT = nc.dram_tensor("outT", [D, T], F32, kind="ExternalOutput").ap()

    def dt_(name, shape, dt=F32):
        return nc.dram_tensor(name, shape, dt)

    XA, XB = dt_("XA", [D, T]), dt_("XB", [D, T])
    q_b, k_b = dt_("q_b", [1024, T], BF16), dt_("k_b", [1024, T], BF16)
    ktm_b, vtm_b = dt_("ktm_b", [T, 1024], BF16), dt_("vtm_b", [T, 2048], BF16)
    gates_b = dt_("gates_b", [8, T])
    sog = dt_("sog", [2048, T])
    q_all, k_all = dt_("q_all", [4096, T], BF16), dt_("k_all", [4096, T], BF16)
    ktm_all, vtm_all = dt_("ktm_all", [4 * T, 1024], BF16), dt_("vtm_all", [4 * T, 2048], BF16)
    gates_all = dt_("gates_all", [32, T])
    h_bm, h_allm = dt_("h_bm", [8 * 512, 1024]), dt_("h_allm", [16 * 1024, 1024])
    h_bs, h_alls = dt_("h_bs", [16 * 128, 2048]), dt_("h_alls", [16 * 512, 2048])
    sq_b, sk_b, sv_b = dt_("sq_b", [2048, T], BF16), dt_("sk_b", [2048, T], BF16), dt_("sv_b", [T, 2048], BF16)
    sq_all, sk_all, sv_all = dt_("sq_all", [8192, T], BF16), dt_("sk_all", [8192, T], BF16), dt_("sv_all", [4 * T, 2048], BF16)

    stage_no = [0]

    def stage(fn):
        with nc.cleanup_on_exit():
            S = Sched(nc, "s%d_" % stage_no[0])
            stage_no[0] += 1
            fn(S)
            S.emit()

    def agc(S, name, src, dst, rk, ks, rkeys=()):
        for k in ks:
            S.add("pool", lambda e, k=k: e.collective_compute(
                "AllGather", ALU.bypass, replica_groups=RG,
                ins=[src[k * rk:(k + 1) * rk, :].opt()], outs=[dst[k * 4 * rk:(k + 1) * 4 * rk, :].opt()]),
                r=list(rkeys), w=[("ag", name)], slot="cc_%s" % name, inc=1, group=True)
        return [("ag", name)]

    def ag(S, name, src, dst, rk):
        return agc(S, name, src, dst, rk, range(src.shape[0] // rk))

    def load_idx(S):
        ixt = S.sb("ixt", [128, NIDX], I32)
        S.add("sp", lambda e: e.dma_start(out=ixt[:], in_=E.idx), w=["ixt"], slot="ixt")
        return ixt

    state = {"cur": E.xT, "flip": 0}

    def nxt_buf(final=False):
        if final:
            return outT
        t = (XA, XB)[state["flip"]]
        state["flip"] ^= 1
        return t.ap()

    def st_ffn(i, k):
        cur, nxt = state["cur"], nxt_buf()

        def f(S):
            C = Ctx(S, 512)
            C.load_vec("g", E.ffn_norm[i, k], KC)
            emit_ffn(C, cur, nxt, T, "g", E.ffn_w_gate[i, k], E.ffn_w_up[i, k], E.ffn_w_down[i, k])
        stage(f)
        state["cur"] = nxt

    def st_ple(i, final):
        cur, nxt = state["cur"], nxt_buf(final)

        def f(S):
            C = Ctx(S, 512)
            C.load_vec("g", E.ple_norm[i], KC)
            emit_ple(C, cur, nxt, E.pT[i], T, "g", E.ple_w_proj[i], E.ple_w_gate[i])
        stage(f)
        state["cur"] = nxt

    def st_proj(W, gated, final=False):
        cur, nxt = state["cur"], nxt_buf(final)

        def f(S):
            C = Ctx(S, 512)
            ixt = load_idx(S)
            if gated:
                view, cb = h_allm.ap().rearrange("r (k f) -> (r k) f", k=2), IX["PAM"]
            else:
                view, cb = h_alls.ap().rearrange("r (k f) -> (r k) f", k=4), IX["PAS"]
            emit_proj_res(C, cur, nxt, None, sog.ap() if gated else None, T, W, gth=(view, ixt, "ixt", cb, []))
        stage(f)
        state["cur"] = nxt

    def mlstm_part(i):
        def f(S, i=i):
            C = Ctx(S, 512)
            C.load_vec("g", E.mix_norm[i], KC)
            def agf(S, name, rk, ks, rkeys):
                src, dst = {"ktm": (ktm_b, ktm_all), "vtm": (vtm_b, vtm_all)}[name]
                agc(S, name, src, dst, rk, ks, rkeys)
            emit_mlstm_in(C, state["cur"], T, "g", E.mlstm_w_in[i], q_b.ap(), k_b.ap(), ktm_b.ap(), vtm_b.ap(), sog.ap(), gates_b.ap(), agf=agf)
        stage(f)

        def f(S, i=i):
            ixt = load_idx(S)
            agk = {"q": ag(S, "q", q_b, q_all, 256), "k": ag(S, "k", k_b, k_all, 256),
                   "ktm": [], "vtm": [], "g": ag(S, "g", gates_b, gates_all, 8)}
            gth = dict(IX)
            gth["agk"] = agk
            gth.update(ixt=ixt, ixkey="ixt", onehot=E.onehot,
                       q_view=q_all.ap().rearrange("r (two f) -> (r two) f", two=2),
                       k_view=k_all.ap().rearrange("r (two f) -> (r two) f", two=2),
                       ktm_view=ktm_all.ap().rearrange("t (h f) -> (t h) f", h=4),
                       vtm_view=vtm_all.ap().rearrange("t (h f) -> (t h) f", h=4),
                       gates_all=gates_all.ap())
            out_ag = {"h_b": h_bm.ap(), "agf": lambda S, name, rk, ks, rkeys: agc(S, name, h_bm, h_allm, rk, ks, rkeys)}
            emit_mlstm_core(S, SEQLEN, None, None, None, None, None, None, E.bif[i], E.ghead[i], None, gth=gth, out_ag=out_ag)
        stage(f)
        st_proj(E.mlstm_w_out[i], True, final=(plan == "mlstm0"))

    def kv_part():
        def f(S):
            C = Ctx(S, 512)
            C.load_vec("g", E.kv_norm, KC)
            C.load_vec("g_k", E.g_k, 1)
            emit_kv(C, state["cur"], T, "g", E.sb_w_kv, sk_b.ap(), sv_b.ap(),
                    agf=lambda S, name, rk, ks, rkeys: agc(S, name, sv_b, sv_all, rk, ks, rkeys))
        stage(f)

    def sb_part(j):
        def f(S, j=j):
            C = Ctx(S, 512)
            C.load_vec("g", E.mix_norm[2 + j], KC)
            C.load_vec("g_q", E.g_q[j], 1)
            for _ in emit_headproj(C, state["cur"], T, "g", E.sb_w_q[j], 0, "g_q", 128 ** -0.5, sq_b.ap()):
                pass
        stage(f)

        def f(S, j=j):
            ixt = load_idx(S)
            agk = {"q": ag(S, "q", sq_b, sq_all, 256), "k": [], "v": []}
            if j == 0:
                agk["k"] = ag(S, "k", sk_b, sk_all, 256)
            gth = dict(IX)
            gth["agk"] = agk
            gth.update(ixt=ixt, ixkey="ixt", q_view=sq_all.ap(), k_view=sk_all.ap(),
                       v_view=sv_all.ap().rearrange("t (h f) -> (t h) f", h=16))
            out_ag = {"o_b": h_bs.ap(), "agf": lambda S, name, rk, ks, rkeys: agc(S, name, h_bs, h_alls, rk, ks, rkeys)}
            emit_sb_core(S, 4, SEQLEN, None, None, None, None, gth=gth, out_ag=out_ag)
        stage(f)
        st_proj(E.sb_w_o[j], False, final=(plan == "sb0"))

    if plan == "mlstm0":
        mlstm_part(0)
        return nc
    if plan == "sb0":
        kv_part()
        sb_part(0)
        return nc
    for i in range(4):
        if i == 2:
            kv_part()
        st_ffn(i, 0)
        if i < 2:
            mlstm_part(i)
        else:
            sb_part(i - 2)
        st_ffn(i, 1)
        st_ple(i, final=(i == 3))
    return nc


_PROG = {}


def _c(a):
    return np.ascontiguousarray(a)


def _vl(g, n):
    g = np.asarray(g, dtype=np.float32)
    lead = g.shape[:-1]
    return _c(np.swapaxes(g.reshape(lead + (n, 128)), -1, -2))


def _idx_table(c):
    h = c % 4
    p = np.arange(128, dtype=np.int64)
    t = np.zeros((128, NIDX), dtype=np.int64)

    def g256(r, s_):
        return (r // 256) * 1024 + s_ * 256 + r % 256

    for sg in range(8):
        s_, half = sg // 2, sg % 2
        for dk in range(2):
            t[:, IX["MQ"] + sg * 2 + dk] = g256(h * 256 + dk * 128 + p, s_) * 2 + half
    for g in range(64):
        tg = g * 128 + p
        s_, tt = tg // 2048, tg % 2048
        t[:, IX["MK"] + g] = ((tt // 512) * 2048 + s_ * 512 + tt % 512) * 4 + h
        t[:, IX["MV"] + g] = g256(tt, s_) * 4 + h
    for cch in range(16):
        hh, r = cch // 4, (cch % 4) * 128 + p
        for ti in range(4):
            sg = h * 2 + ti // 2
            ck = sg * 2 + r // 256
            t[:, IX["PAM"] + cch * 4 + ti] = (ck * 1024 + hh * 256 + r % 256) * 2 + ti % 2
            ck = (r // 128) * 4 + h
            t[:, IX["PAS"] + cch * 4 + ti] = (ck * 512 + hh * 128 + r % 128) * 4 + ti
    for hl in range(4):
        for s_ in range(4):
            t[:, IX["SQ"] + hl * 4 + s_] = g256((4 * h + hl) * 128 + p, s_)
        for blk in range(64):
            tg = blk * 128 + p
            s_, tt = tg // 2048, tg % 2048
            t[:, IX["SV"] + hl * 64 + blk] = g256(tt, s_) * 16 + 4 * h + hl
    return t.astype(np.int32)


def kernel(x, p, ffn_norm, ffn_w_gate, ffn_w_up, ffn_w_down, mix_norm,
           mlstm_w_in, mlstm_b_if, mlstm_head_norm, mlstm_w_out,
           kv_norm, sb_w_kv, sb_k_norm, sb_w_q, sb_q_norm, sb_w_o,
           ple_norm, ple_w_proj, ple_w_gate):
    f32 = np.float32
    T = TCORE
    x = np.asarray(x, f32)
    p = np.asarray(p, f32)
    if "nc" not in _PROG:
        _PROG["nc"] = build_fused()
    nc = _PROG["nc"]
    shared = {
        "ffn_norm": _vl(ffn_norm, KC),
        "ffn_w_gate": _c(np.asarray(ffn_w_gate, f32)), "ffn_w_up": _c(np.asarray(ffn_w_up, f32)),
        "ffn_w_down": _c(np.asarray(ffn_w_down, f32)),
        "mix_norm": _vl(mix_norm, KC),
        "mlstm_w_in": _c(np.asarray(mlstm_w_in, f32)), "mlstm_w_out": _c(np.asarray(mlstm_w_out, f32)),
        "kv_norm": _vl(kv_norm, KC), "sb_w_kv": _c(np.asarray(sb_w_kv, f32)), "g_k": _vl(sb_k_norm, 1),
        "sb_w_q": _c(np.asarray(sb_w_q, f32)), "g_q": _vl(sb_q_norm, 1), "sb_w_o": _c(np.asarray(sb_w_o, f32)),
        "ple_norm": _vl(ple_norm, KC), "ple_w_proj": _c(np.asarray(ple_w_proj, f32)),
        "ple_w_gate": _c(np.asarray(ple_w_gate, f32)),
    }
    b_if = np.asarray(mlstm_b_if, f32)
    hn = np.asarray(mlstm_head_norm, f32)
    in_maps = []
    for c in range(NCORES):
        b, s = c // 4, c % 4
        h = s
        m = dict(shared)
        m["xT"] = _c(x[b, s * T:(s + 1) * T].T)
        m["pT"] = _c(p[:, b, s * T:(s + 1) * T, :].transpose(0, 2, 1))
        m["bif"] = _c(np.stack([b_if[:, h], b_if[:, 4 + h]], axis=-1)[:, None, :])
        m["ghead"] = _vl(hn[:, h * 512:(h + 1) * 512], 4)
        m["idx"] = _idx_table(c)
        oh = np.zeros((1, 4), f32)
        oh[0, h] = 1.0
        m["onehot"] = oh
        in_maps.append({k: v for k, v in m.items() if k in nc._ext_names})
    res = run_bass_kernel_spmd(nc, in_maps, core_ids=list(range(NCORES)))
    out = np.empty((2, SEQLEN, D), dtype=f32)
    for c in range(NCORES):
        b, s = c // 4, c % 4
        out[b, s * T:(s + 1) * T] = res.results[c]["outT"].T
    return out
```

```python
import contextlib
import numpy as np
import concourse.bass as bass
import concourse.mybir as mybir
from concourse.bass_utils import run_bass_kernel_spmd

F32 = mybir.dt.float32
BF16 = mybir.dt.bfloat16
AF = mybir.ActivationFunctionType
ALU = mybir.AluOpType

D = 2048
KC = D // 128
DFF = 5504
FC = DFF // 128
EPS = 1e-6
NCORES = 8
PENG = 'dve'
SELF_ORDERED = {'pe'}
NSTF = 4
USE_SOFTPLUS = True


class Op:
    __slots__ = ("eng", "fn", "deps", "slot", "needed", "tok", "waits", "know", "inc")

    def __init__(self, eng, fn, slot, inc=16):
        self.eng = eng
        self.fn = fn
        self.slot = slot
        self.inc = inc
        self.deps = set()
        self.needed = False
        self.tok = None
        self.waits = []
        self.know = None


class Sched:
    ENGS = ("pe", "act", "dve", "pool", "sp")

    def __init__(self, nc, prefix=""):
        self.nc = nc
        self.prefix = prefix
        self.ops = []
        self.last_w = {}
        self.readers = {}
        self.stack = contextlib.ExitStack()
        self.n_t = 0

    def uid(self):
        self.n_t += 1
        return self.n_t

    def sb(self, name, shape, dt):
        return self.stack.enter_context(self.nc.sbuf_tensor(self.prefix + name, shape, dt))

    def ps(self, name, shape, dt=F32):
        return self.stack.enter_context(self.nc.psum_tensor(self.prefix + name, shape, dt))

    def add(self, eng, fn, r=(), w=(), slot=None, inc=16, group=False):
        op = Op(eng, fn, slot, inc)
        deps = op.deps
        for k in r:
            d = self.last_w.get(k)
            if d is not None:
                deps.add(d)
        for k in w:
            d = self.last_w.get(k)
            if d is not None and not (group and d.slot == slot):
                deps.add(d)
            elif d is not None:
                deps.update(x for x in d.deps if x.slot != slot)
            rs = self.readers.get(k)
            if rs:
                deps.update(rs)
        for k in w:
            self.last_w[k] = op
            self.readers[k] = set()
        for k in r:
            self.readers.setdefault(k, set()).add(op)
        deps.discard(op)
        self.ops.append(op)
        return op

    def emit(self):
        nc = self.nc
        ops = self.ops
        for op in ops:
            for d in op.deps:
                if d.eng == op.eng and d.eng in SELF_ORDERED and d.slot is None and op.slot is None:
                    continue
                d.needed = True
        cnt = {e: 0 for e in self.ENGS}
        slot_cnt = {}
        for op in ops:
            if op.slot is not None:
                slot_cnt[op.slot] = slot_cnt.get(op.slot, 0) + op.inc
                op.tok = (("slot", op.slot), slot_cnt[op.slot])
            elif op.needed:
                cnt[op.eng] += 1
                op.tok = (("eng", op.eng), cnt[op.eng])
        sems = {}
        for e in self.ENGS:
            sems[("eng", e)] = nc.alloc_semaphore(self.prefix + "s_" + e)
        for i, sl in enumerate(slot_cnt):
            sems[("slot", sl)] = nc.alloc_semaphore(self.prefix + "d%d" % i)
        known = {e: {} for e in self.ENGS}
        for op in ops:
            kn = known[op.eng]
            for d in op.deps:
                if d.eng == op.eng and d.eng in SELF_ORDERED and d.slot is None and op.slot is None:
                    continue
                sk, val = d.tok
                if kn.get(sk, 0) >= val:
                    continue
                op.waits.append((sk, val))
                for k2, v2 in d.know.items():
                    if kn.get(k2, 0) < v2:
                        kn[k2] = v2
            if op.tok is not None:
                kn2 = dict(kn)
                sk, val = op.tok
                if kn2.get(sk, 0) < val:
                    kn2[sk] = val
                op.know = kn2
        per_eng = {e: [] for e in self.ENGS}
        for op in ops:
            w = {}
            for sk, val in op.waits:
                if w.get(sk, 0) < val:
                    w[sk] = val
            op.waits = list(w.items())
            per_eng[op.eng].append(op)
        final_waits = [(("slot", sl), v) for sl, v in slot_cnt.items()]
        self.stats = {e: len(per_eng[e]) for e in self.ENGS}

        def run(engobj, lst, final=False):
            for op in lst:
                for sk, val in op.waits:
                    engobj.wait_ge(sems[sk], val)
                ins = op.fn(engobj)
                if op.tok is not None:
                    if op.slot is not None and op.inc == 1:
                        ins.then_inc(sems[op.tok[0]])
                    else:
                        ins.then_inc(sems[op.tok[0]], op.inc if op.slot is not None else 1)
            if final:
                for sk, val in final_waits:
                    engobj.wait_ge(sems[sk], val)

        with nc.Block(self.prefix + "blk") as block:
            @block.tensor
            def _(e):
                run(e, per_eng["pe"])

            @block.scalar
            def _(e):
                run(e, per_eng["act"])

            @block.vector
            def _(e):
                run(e, per_eng["dve"])

            @block.gpsimd
            def _(e):
                run(e, per_eng["pool"])

            @block.sync
            def _(e):
                run(e, per_eng["sp"], final=True)


class Ctx:
    def __init__(self, S, TT):
        self.S = S
        self.TT = TT
        nc = S.nc
        self.ones = S.sb("ones_bf", [128, 128], BF16)
        self.banks = [S.ps("bank%d" % i, [128, 512], F32) for i in range(8)]
        S.add("dve", lambda e: e.memset(self.ones[:], 1.0), w=[("ones",)])
        self.xt = S.sb("xt", [128, KC, TT], F32)
        self.xn = S.sb("xn", [128, KC, TT], BF16)
        self.sq = S.sb("sq", [128, KC, TT], BF16)
        self.rs = S.sb("rstd", [128, TT], F32)
        self.rs2 = S.sb("rstd2", [128, TT], F32)
        self.gv = {}

    def load_vec(self, name, dram_ap, nchunk):
        S = self.S
        t = S.sb("v_" + name, [128, nchunk], F32)
        S.add("sp", lambda e: e.dma_start(out=t[:], in_=dram_ap), w=[("v", name)], slot="v_" + name)
        self.gv[name] = t
        return t


def emit_norm(C, xin, t0, gname, load_x=True):
    S, TT = C.S, C.TT
    xt, xn, sq, rs, rs2 = C.xt, C.xn, C.sq, C.rs, C.rs2
    g = C.gv[gname]
    ssb = C.banks[0]
    if load_x:
        src = xin.rearrange("(c p) t -> p c t", p=128)[:, :, t0:t0 + TT]
        S.add("sp", lambda e: e.dma_start(out=xt[:], in_=src), w=[("xt",)], slot="xt")
    S.add("act", lambda e: e.activation(out=sq[:], in_=xt[:], func=AF.Square), r=[("xt",)], w=[("sq",)])
    for c in range(KC):
        S.add("pe", lambda e, c=c: e.matmul(ssb[:, :TT], C.ones[:], sq[:, c, :], start=(c == 0), stop=(c == KC - 1)),
              r=[("sq",), ("ones",)], w=[("ps", 0)])
    S.add("dve", lambda e: e.tensor_scalar(rs[:], ssb[:, :TT], 1.0 / D, EPS, ALU.mult, ALU.add),
          r=[("ps", 0)], w=[("rs",)])
    S.add("act", lambda e: e.activation(out=rs2[:], in_=rs[:], func=AF.Ln), r=[("rs",)], w=[("rs2",)])
    S.add("act", lambda e: e.activation(out=rs[:], in_=rs2[:], func=AF.Exp, scale=-0.5), r=[("rs2",)], w=[("rs",)])
    for c in range(KC):
        S.add("dve", lambda e, c=c: e.scalar_tensor_tensor(xn[:, c, :], xt[:, c, :], g[:, c:c + 1], rs[:],
                                                           ALU.mult, ALU.mult),
              r=[("xt",), ("rs",), ("v", gname)], w=[("xn", c)])


def emit_ffn(C, xin, xout, T, gname, wg, wu, wd):
    S, TT = C.S, C.TT
    if not hasattr(C, "wg"):
        C.wg = [S.sb("wg%d" % i, [128, KC, 512], BF16) for i in range(2)]
        C.wu = [S.sb("wu%d" % i, [128, KC, 512], BF16) for i in range(2)]
        C.wd = [S.sb("wd%d" % i, [128, 11, 512], BF16) for i in range(2)]
        C.hid = S.sb("hid", [128, FC, TT], BF16)
        C.sg = [S.sb("sg%d" % i, [128, TT], F32) for i in range(2)]
        C.ctr = {"gu": 0, "wd": 0, "f": 0}
    wg_r = wg.rearrange("(c p) n -> p c n", p=128)
    wu_r = wu.rearrange("(c p) n -> p c n", p=128)
    wd_r = wd.rearrange("(f p) n -> p f n", p=128)
    xo_r = xout.rearrange("(c p) t -> p c t", p=128)
    hid = C.hid
    for t0 in range(0, T, TT):
        emit_norm(C, xin, t0, gname)
        for fg in range(0, FC, 4):
            nf = min(4, FC - fg)
            sl = C.ctr["gu"] % 2
            C.ctr["gu"] += 1
            wgt, wut = C.wg[sl], C.wu[sl]
            S.add("pool", lambda e, wgt=wgt, fg=fg, nf=nf: e.dma_start(
                out=wgt[:, :, :nf * 128], in_=wg_r[:, :, fg * 128:(fg + nf) * 128]),
                w=[("wg", sl)], slot="wg%d" % sl)
            S.add("pool", lambda e, wut=wut, fg=fg, nf=nf: e.dma_start(
                out=wut[:, :, :nf * 128], in_=wu_r[:, :, fg * 128:(fg + nf) * 128]),
                w=[("wu", sl)], slot="wu%d" % sl)
            for fi in range(nf):
                f = fg + fi
                pb = C.ctr["f"] % 2
                C.ctr["f"] += 1
                gb, ub = C.banks[4 + pb], C.banks[6 + pb]
                for c in range(KC):
                    S.add("pe", lambda e, c=c, fi=fi, gb=gb, wgt=wgt: e.matmul(
                        gb[:, :TT], wgt[:, c, fi * 128:(fi + 1) * 128], C.xn[:, c, :], start=(c == 0), stop=(c == KC - 1)),
                        r=[("wg", sl), ("xn", c)], w=[("ps", 4 + pb)])
                for c in range(KC):
                    S.add("pe", lambda e, c=c, fi=fi, ub=ub, wut=wut: e.matmul(
                        ub[:, :TT], wut[:, c, fi * 128:(fi + 1) * 128], C.xn[:, c, :], start=(c == 0), stop=(c == KC - 1)),
                        r=[("wu", sl), ("xn", c)], w=[("ps", 6 + pb)])
                sg = C.sg[pb]
                S.add("act", lambda e, sg=sg, gb=gb: e.activation(out=sg[:], in_=gb[:, :TT], func=AF.Silu),
                      r=[("ps", 4 + pb)], w=[("sg", pb)])
                S.add("dve", lambda e, sg=sg, ub=ub, f=f: e.tensor_tensor(hid[:, f, :], ub[:, :TT], sg[:], ALU.mult),
                      r=[("ps", 6 + pb), ("sg", pb)], w=[("hid", f)])
        for ng in range(4):
            for f0 in range(0, FC, 11):
                nf = min(11, FC - f0)
                sl = C.ctr["wd"] % 2
                C.ctr["wd"] += 1
                wdt = C.wd[sl]
                S.add("pool", lambda e, wdt=wdt, f0=f0, nf=nf, ng=ng: e.dma_start(
                    out=wdt[:, :nf, :], in_=wd_r[:, f0:f0 + nf, ng * 512:(ng + 1) * 512]),
                    w=[("wd", sl)], slot="wd%d" % sl)
                for fi in range(nf):
                    f = f0 + fi
                    for j in range(4):
                        S.add("pe", lambda e, wdt=wdt, fi=fi, f=f, j=j: e.matmul(
                            C.banks[j][:, :TT], wdt[:, fi, j * 128:(j + 1) * 128], hid[:, f, :],
                            start=(f == 0), stop=(f == FC - 1)),
                            r=[("wd", sl), ("hid", f)], w=[("ps", j)])
            for j in range(4):
                c = ng * 4 + j
                S.add("dve", lambda e, j=j, c=c: e.scalar_tensor_tensor(
                    C.xt[:, c, :], C.banks[j][:, :TT], 0.5, C.xt[:, c, :], ALU.mult, ALU.add),
                    r=[("ps", j), ("xt",)], w=[("xt",)])
        S.add("sp", lambda e, t0=t0: e.dma_start(out=xo_r[:, :, t0:t0 + TT], in_=C.xt[:]),
              r=[("xt",)], w=[("dram", "xout")], slot="xt_st")


def ctx_extra(C):
    S, TT = C.S, C.TT
    if hasattr(C, "wsl"):
        return
    if not hasattr(C, "wg"):
        C.wg = [S.sb("wg%d" % i, [128, KC, 512], BF16) for i in range(2)]
        C.wu = [S.sb("wu%d" % i, [128, KC, 512], BF16) for i in range(2)]
        C.sg = [S.sb("sg%d" % i, [128, TT], F32) for i in range(2)]
    C.wsl = [(C.wg[0], ("wg", 0), "wg0"), (C.wu[0], ("wu", 0), "wu0"),
             (C.wg[1], ("wg", 1), "wg1"), (C.wu[1], ("wu", 1), "wu1")]
    C.wctr = 0
    C.bctr = 0
    C.stb = [S.sb("stb%d" % i, [128, 4, 512], BF16) for i in range(2)]
    C.stf = [S.sb("stf%d" % i, [128, 4, 512], F32) for i in range(NSTF)]
    C.stctr = {"b": 0, "f": 0}
    C.kf = S.sb("kf", [128, TT], F32)
    C.sqh = S.sb("sqh", [128, TT], BF16)
    C.hr = S.sb("hr", [128, TT], F32)
    C.hr2 = S.sb("hr2", [128, TT], F32)


def next_w(C):
    t = C.wsl[C.wctr % 4]
    C.wctr += 1
    return t


def next_bank(C):
    b = 4 + (C.bctr % 4)
    C.bctr += 1
    return b


def next_stage(C, kind):
    i = C.stctr[kind] % 2
    C.stctr[kind] += 1
    return (C.stb if kind == "b" else C.stf)[i], ("st" + kind, i), "st%s%d" % (kind, i)


def fm_linear(C, W_r, c0, ncols, kch, rhs, rkey, epilogue):
    S, TT = C.S, C.TT
    for gi, g0 in enumerate(range(0, ncols, 512)):
        gw = min(512, ncols - g0)
        wt, wkey, wslot = next_w(C)
        S.add("pool", lambda e, wt=wt, g0=g0, gw=gw: e.dma_start(
            out=wt[:, :kch, :gw], in_=W_r[:, :, c0 + g0:c0 + g0 + gw]), w=[wkey], slot=wslot)
        nj = (gw + 127) // 128
        for j in range(nj):
            m = min(128, gw - j * 128)
            b = next_bank(C)
            for c in range(kch):
                S.add("pe", lambda e, wt=wt, c=c, j=j, m=m, b=b: e.matmul(
                    C.banks[b][:m, :TT], wt[:, c, j * 128:j * 128 + m], rhs[:, c, :], start=(c == 0), stop=(c == kch - 1)),
                    r=[wkey, rkey(c)], w=[("ps", b)])
            epilogue(gi, j, nj, m, b)


def tm_linear(C, W_r, c0, ncols, epilogue):
    S, TT = C.S, C.TT
    for gi, g0 in enumerate(range(0, ncols, 512)):
        gw = min(512, ncols - g0)
        wt, wkey, wslot = next_w(C)
        S.add("pool", lambda e, wt=wt, g0=g0, gw=gw: e.dma_start(
            out=wt[:, :, :gw], in_=W_r[:, :, c0 + g0:c0 + g0 + gw]), w=[wkey], slot=wslot)
        for tb in range(TT // 128):
            b = next_bank(C)
            for c in range(KC):
                S.add("pe", lambda e, wt=wt, c=c, tb=tb, gw=gw, b=b: e.matmul(
                    C.banks[b][:, :gw], C.xn[:, c, tb * 128:(tb + 1) * 128], wt[:, c, :gw], start=(c == 0), stop=(c == KC - 1)),
                    r=[wkey, ("xn", c)], w=[("ps", b)])
            epilogue(gi, tb, gw, b)


def headnorm(C, b, gname, scale, out_ap, out_key):
    S, TT = C.S, C.TT
    bank = C.banks[b]
    g = C.gv[gname]
    S.add("act", lambda e: e.activation(out=C.kf[:], in_=bank[:, :TT], func=AF.Copy, scale=float(scale)),
          r=[("ps", b)], w=[("kf",)])
    S.add("act", lambda e: e.activation(out=C.sqh[:], in_=bank[:, :TT], func=AF.Square), r=[("ps", b)], w=[("sqh",)])
    S.add("pe", lambda e: e.matmul(C.banks[1][:, :TT], C.ones[:], C.sqh[:], start=True, stop=True),
          r=[("sqh",), ("ones",)], w=[("ps", 1)])
    S.add("dve", lambda e: e.tensor_scalar(C.hr[:], C.banks[1][:, :TT], 1.0 / 128, EPS, ALU.mult, ALU.add),
          r=[("ps", 1)], w=[("hr",)])
    S.add("act", lambda e: e.activation(out=C.hr2[:], in_=C.hr[:], func=AF.Ln), r=[("hr",)], w=[("hr2",)])
    S.add("act", lambda e: e.activation(out=C.hr[:], in_=C.hr2[:], func=AF.Exp, scale=-0.5), r=[("hr2",)], w=[("hr",)])
    S.add("dve", lambda e: e.scalar_tensor_tensor(out_ap, C.kf[:], g[:, 0:1], C.hr[:], ALU.mult, ALU.mult),
          r=[("kf",), ("hr",), ("v", gname)], w=[out_key])


def xn_key(c):
    return ("xn", c)


def emit_ple(C, xin, xout, pT, T, gname, wpe, wpg):
    S, TT = C.S, C.TT
    ctx_extra(C)
    if not hasattr(C, "pt"):
        C.pt = S.sb("pt", [128, 2, TT], BF16)
        C.wpe = [S.sb("wpe%d" % i, [128, 2, 512], BF16) for i in range(2)]
        C.pectr = 0
    wpg_r = wpg.rearrange("(c p) n -> p c n", p=128)
    wpe_r = wpe.rearrange("(c p) n -> p c n", p=128)
    pT_r = pT.rearrange("(c p) t -> p c t", p=128)
    xo_r = xout.rearrange("(c p) t -> p c t", p=128)
    for t0 in range(0, T, TT):
        emit_norm(C, xin, t0, gname)
        S.add("pool", lambda e, t0=t0: e.dma_start(out=C.pt[:], in_=pT_r[:, :, t0:t0 + TT]), w=[("pt",)], slot="pt")
        for ng in range(4):
            sl = C.pectr % 2
            C.pectr += 1
            wpet = C.wpe[sl]
            S.add("pool", lambda e, wpet=wpet, ng=ng: e.dma_start(out=wpet[:], in_=wpe_r[:, :, ng * 512:(ng + 1) * 512]),
                  w=[("wpe", sl)], slot="wpe%d" % sl)

            def epi(gi, j, nj, m, b, ng=ng, wpet=wpet, sl=sl):
                c = ng * 4 + j
                pb = 2 + (c % 2)
                for cc in range(2):
                    S.add("pe", lambda e, cc=cc: e.matmul(C.banks[pb][:, :TT], wpet[:, cc, j * 128:(j + 1) * 128], C.pt[:, cc, :],
                                                          start=(cc == 0), stop=(cc == 1)),
                          r=[("wpe", sl), ("pt",)], w=[("ps", pb)])
                sg = C.sg[c % 2]
                S.add("act", lambda e: e.activation(out=sg[:], in_=C.banks[b][:, :TT], func=AF.Sigmoid),
                      r=[("ps", b)], w=[("sg", c % 2)])
                S.add("dve", lambda e: e.tensor_tensor(sg[:], C.banks[pb][:, :TT], sg[:], ALU.mult),
                      r=[("ps", pb), ("sg", c % 2)], w=[("sg", c % 2)])
                S.add("dve", lambda e: e.tensor_tensor(C.xt[:, c, :], C.xt[:, c, :], sg[:], ALU.add),
                      r=[("sg", c % 2), ("xt",)], w=[("xt",)])
            fm_linear(C, wpg_r, ng * 512, 512, KC, C.xn, xn_key, epi)
        S.add("sp", lambda e, t0=t0: e.dma_start(out=xo_r[:, :, t0:t0 + TT], in_=C.xt[:]),
              r=[("xt",)], w=[("dram", "xout")], slot="xt_st")


def emit_proj_res(C, xin, xout, aT, bT, T, W, gth=None):
    S, TT = C.S, C.TT
    ctx_extra(C)
    W_r = W.rearrange("(c p) n -> p c n", p=128)
    x_r = xin.rearrange("(c p) t -> p c t", p=128)
    a_r = aT.rearrange("(c p) t -> p c t", p=128) if gth is None else None
    b_r = bT.rearrange("(c p) t -> p c t", p=128) if bT is not None else None
    xo_r = xout.rearrange("(c p) t -> p c t", p=128)
    for t0 in range(0, T, TT):
        S.add("sp", lambda e, t0=t0: e.dma_start(out=C.xt[:], in_=x_r[:, :, t0:t0 + TT]), w=[("xt",)], slot="xt")
        for g4 in range(4):
            ai = C.stctr["f"] % NSTF
            C.stctr["f"] += 1
            if gth is None:
                S.add("sp", lambda e, t0=t0, g4=g4, ai=ai: e.dma_start(out=C.stf[ai][:, :, :TT], in_=a_r[:, g4 * 4:(g4 + 1) * 4, t0:t0 + TT]),
                      w=[("stf", ai)], slot="stf%d" % ai)
            else:
                view, ixt, ixkey, cb, agk = gth
                for cc in range(4):
                    col = cb + (g4 * 4 + cc) * 4 + t0 // TT
                    S.add("pool", lambda e, cc=cc, ai=ai, col=col: e.indirect_dma_start(
                        out=C.stf[ai][:, cc, :TT], out_offset=None, in_=view,
                        in_offset=bass.IndirectOffsetOnAxis(ap=ixt[:, col:col + 1], axis=0)),
                        r=[ixkey] + list(agk), w=[("stf", ai)], slot="stf%d" % ai, group=True)
            if b_r is not None:
                bi_ = C.stctr["f"] % NSTF
                C.stctr["f"] += 1
                S.add("sp", lambda e, t0=t0, g4=g4, bi_=bi_: e.dma_start(out=C.stf[bi_][:, :, :TT], in_=b_r[:, g4 * 4:(g4 + 1) * 4, t0:t0 + TT]),
                      w=[("stf", bi_)], slot="stf%d" % bi_)
                for cc in range(4):
                    c = g4 * 4 + cc
                    S.add("dve", lambda e, c=c, cc=cc, ai=ai, bi_=bi_: e.tensor_tensor(C.xn[:, c, :], C.stf[ai][:, cc, :TT], C.stf[bi_][:, cc, :TT], ALU.mult),
                          r=[("stf", ai), ("stf", bi_)], w=[("xn", c)])
            else:
                for cc in range(4):
                    c = g4 * 4 + cc
                    if cc % 2 == 0:
                        S.add("dve", lambda e, c=c, cc=cc, ai=ai: e.tensor_copy(C.xn[:, c, :], C.stf[ai][:, cc, :TT]), r=[("stf", ai)], w=[("xn", c)])
                    else:
                        S.add("act", lambda e, c=c, cc=cc, ai=ai: e.activation(out=C.xn[:, c, :], in_=C.stf[ai][:, cc, :TT], func=AF.Copy), r=[("stf", ai)], w=[("xn", c)])

        def epi(gi, j, nj, m, b):
            c = gi * 4 + j
            S.add("dve", lambda e: e.tensor_tensor(C.xt[:, c, :], C.banks[b][:, :TT], C.xt[:, c, :], ALU.add),
                  r=[("ps", b), ("xt",)], w=[("xt",)])
        fm_linear(C, W_r, 0, D, KC, C.xn, xn_key, epi)
        S.add("sp", lambda e, t0=t0: e.dma_start(out=xo_r[:, :, t0:t0 + TT], in_=C.xt[:]),
              r=[("xt",)], w=[("dram", "xout")], slot="xt_st")


def emit_mlstm_in(C, xin, T, gname, w_in, qT, kT, k_tm, v_tm, sog, gates, agf=None):
    S, TT = C.S, C.TT
    pending = []
    tkeys = {}
    ctx_extra(C)
    W_r = w_in.rearrange("(c p) n -> p c n", p=128)
    qT_r = qT.rearrange("(c p) t -> p c t", p=128)
    kT_r = kT.rearrange("(c p) t -> p c t", p=128)
    sog_r = sog.rearrange("(c p) t -> p c t", p=128)
    ktm_r = k_tm.rearrange("(b p) n -> p b n", p=128)
    vtm_r = v_tm.rearrange("(b p) n -> p b n", p=128)
    if not hasattr(C, "gst"):
        C.gst = S.sb("gst", [8, TT], F32)
    for t0 in range(0, T, TT):
        emit_norm(C, xin, t0, gname)
        cur = {}

        def fm_epi(kind, dst_r, scale, func):
            def epi(gi, j, nj, m, b):
                if j == 0:
                    cur["st"] = next_stage(C, kind)
                st, skey, sslot = cur["st"]
                S.add("act", lambda e: e.activation(out=st[:, j, :TT], in_=C.banks[b][:, :TT], func=func, scale=float(scale)),
                      r=[("ps", b)], w=[skey])
                if j == nj - 1:
                    S.add("sp", lambda e, t0=t0: e.dma_start(out=dst_r[:, gi * 4:gi * 4 + nj, t0:t0 + TT], in_=st[:, :nj, :TT]),
                          r=[skey], w=[("dram", "o", S.uid())], slot=sslot)
            return epi
        fm_linear(C, W_r, 0, 1024, KC, C.xn, xn_key, fm_epi("b", qT_r, 256 ** -0.5, AF.Copy))
        for fn in pending:
            fn()
        del pending[:]
        fm_linear(C, W_r, 1024, 1024, KC, C.xn, xn_key, fm_epi("b", kT_r, 1.0, AF.Copy))
        fm_linear(C, W_r, 4096, 2048, KC, C.xn, xn_key, fm_epi("f", sog_r, 1.0, AF.Sigmoid))

        def g_epi(gi, j, nj, m, b):
            S.add("act", lambda e: e.activation(out=C.gst[:, :], in_=C.banks[b][:8, :TT], func=AF.Copy),
                  r=[("ps", b)], w=[("gst",)])
            S.add("sp", lambda e, t0=t0: e.dma_start(out=gates[:, t0:t0 + TT], in_=C.gst[:, :]), r=[("gst",)], w=[("dram", "o", S.uid())], slot="gst")
        fm_linear(C, W_r, 6144, 8, KC, C.xn, xn_key, g_epi)

        def tm_epi(dst_r, name):
            def epi(gi, tb, gw, b):
                if tb == 0:
                    cur["st"] = next_stage(C, "b")
                st, skey, sslot = cur["st"]
                eng = "dve" if tb % 2 == 0 else "act"
                if eng == "dve":
                    S.add("dve", lambda e: e.tensor_copy(st[:, tb, :gw], C.banks[b][:, :gw]), r=[("ps", b)], w=[skey])
                else:
                    S.add("act", lambda e: e.activation(out=st[:, tb, :gw], in_=C.banks[b][:, :gw], func=AF.Copy), r=[("ps", b)], w=[skey])
                if tb == TT // 128 - 1:
                    tb0 = t0 // 128
                    dkey = ("dram", "o", S.uid())
                    tkeys.setdefault((name, t0), []).append(dkey)
                    S.add("sp", lambda e: e.dma_start(out=dst_r[:, tb0:tb0 + TT // 128, gi * 512:gi * 512 + gw], in_=st[:, :TT // 128, :gw]),
                          r=[skey], w=[dkey], slot=sslot)
            return epi
        tm_linear(C, W_r, 1024, 1024, tm_epi(ktm_r, "ktm"))
        tm_linear(C, W_r, 2048, 2048, tm_epi(vtm_r, "vtm"))
        if agf is not None:
            ti = t0 // TT
            pending.append(lambda ti=ti, t0=t0: agf(S, "ktm", 512, [ti], tkeys[("ktm", t0)]))
            pending.append(lambda ti=ti, t0=t0: agf(S, "vtm", 256, [2 * ti, 2 * ti + 1], tkeys[("vtm", t0)]))
    for fn in pending:
        fn()


def emit_headproj(C, xin, T, gname, W, c0, hgname, scale, outT):
    S, TT = C.S, C.TT
    ctx_extra(C)
    W_r = W.rearrange("(c p) n -> p c n", p=128)
    o_r = outT.rearrange("(c p) t -> p c t", p=128)
    cur = {}
    for t0 in range(0, T, TT):
        emit_norm(C, xin, t0, gname)

        def epi(gi, j, nj, m, b):
            if j == 0:
                cur["st"] = next_stage(C, "b")
            st, skey, sslot = cur["st"]
            headnorm(C, b, hgname, scale, st[:, j, :TT], skey)
            if j == nj - 1:
                S.add("sp", lambda e, t0=t0: e.dma_start(out=o_r[:, gi * 4:gi * 4 + nj, t0:t0 + TT], in_=st[:, :nj, :TT]),
                      r=[skey], w=[("dram", "o", S.uid())], slot=sslot)
        fm_linear(C, W_r, c0, 2048, KC, C.xn, xn_key, epi)
        yield t0


def emit_kv(C, xin, T, gname, w_kv, kT, v_tm, agf=None):
    S, TT = C.S, C.TT
    ctx_extra(C)
    W_r = w_kv.rearrange("(c p) n -> p c n", p=128)
    vtm_r = v_tm.rearrange("(b p) n -> p b n", p=128)
    cur = {}
    tkeys = {}
    pend = []
    for t0 in emit_headproj(C, xin, T, gname, w_kv, 0, "g_k", 1.0, kT):
        if agf is not None and t0 > 0:
            for fn in pend:
                fn()
            del pend[:]
        def epi(gi, tb, gw, b):
            if tb == 0:
                cur["st"] = next_stage(C, "b")
            st, skey, sslot = cur["st"]
            if tb % 2 == 0:
                S.add("dve", lambda e: e.tensor_copy(st[:, tb, :gw], C.banks[b][:, :gw]), r=[("ps", b)], w=[skey])
            else:
                S.add("act", lambda e: e.activation(out=st[:, tb, :gw], in_=C.banks[b][:, :gw], func=AF.Copy), r=[("ps", b)], w=[skey])
            if tb == TT // 128 - 1:
                tb0 = t0 // 128
                dkey = ("dram", "o", S.uid())
                tkeys.setdefault(t0, []).append(dkey)
                S.add("sp", lambda e: e.dma_start(out=vtm_r[:, tb0:tb0 + TT // 128, gi * 512:gi * 512 + gw], in_=st[:, :TT // 128, :gw]),
                      r=[skey], w=[dkey], slot=sslot)
        tm_linear(C, W_r, 2048, 2048, epi)
        if agf is not None:
            ti = t0 // TT
            pend.append(lambda ti=ti, t0=t0: agf(S, "v", 256, [2 * ti, 2 * ti + 1], tkeys[t0]))
    for fn in pend:
        fn()


def emit_mlstm_core(S, SEQ, qT, kT, k_tm, v_tm, gi, gf, bif, ghead, hout, SEG=1024, dbg=None, gth=None, out_ag=None):
    nc = S.nc
    CH = 128
    NCH = SEG // CH
    DK, DV = 256, 512
    sb, ps = S.sb, S.ps
    banks = [ps("bank%d" % i, [128, 512], F32) for i in range(8)]
    ones_bf = sb("ones_bf", [128, 128], BF16)
    ones_f = sb("ones_f", [128, 128], F32)
    tri = sb("tri", [128, 128], F32)
    S.add("dve", lambda e: e.memset(ones_bf[:], 1.0), w=["ones_bf"])
    S.add("dve", lambda e: e.memset(ones_f[:], 1.0), w=["ones_f"])
    S.add("pool", lambda e: e.memset(tri[:], 1.0), w=["tri"])
    S.add("pool", lambda e: e.affine_select(out=tri[:], in_=tri[:], pattern=[[1, 128]], compare_op=ALU.is_ge, fill=0.0,
                                            base=0, channel_multiplier=-1), r=["tri"], w=["tri"])
    bt = sb("bif_sb", [1, 2], F32)
    S.add("sp", lambda e: e.dma_start(out=bt[:], in_=bif), w=["bif"], slot="bif")
    nbf = sb("nbf", [1, 1], F32)
    S.add("dve", lambda e: e.tensor_scalar(nbf[:], bt[0:1, 1:2], -1.0, None, ALU.mult), r=["bif"], w=["nbf"])
    gh = sb("gh_sb", [128, 4], F32)
    S.add("sp", lambda e: e.dma_start(out=gh[:], in_=ghead), w=["gh"], slot="gh")
    rows = {n: sb("r_" + n, [1, SEG], F32) for n in ("gi", "gf", "e", "lf", "Bn", "U", "G", "nG", "wi", "cl", "we", "one")}
    S.add("dve", lambda e: e.memset(rows["one"][:], 1.0), w=["r_one"])
    carry = sb("carry", [1, 4], F32)
    S.add("dve", lambda e: e.memset(carry[:], 0.0), w=["carry"])
    bc = {n: sb("bc_" + n, [128, SEG], F32) for n in ("nG", "wi", "cl")}
    cols = sb("cols", [128, 3 * NCH], F32)
    qt = [sb("qt%d" % i, [128, 2, SEG], BF16) for i in range(2)]
    kt = [sb("kt%d" % i, [128, 2, SEG], BF16) for i in range(2)]
    ktm = [sb("ktm%d" % i, [128, NCH, DK], BF16) for i in range(2)]
    vtm = [sb("vtm%d" % i, [128, NCH, DV], BF16) for i in range(2)]
    hst = [sb("hst%d" % i, [128, 4, SEG], F32) for i in range(2)]
    PT = sb("PT", [128, 128], F32)
    PTm = sb("PTm", [128, 128], F32)
    AT = sb("AT", [128, 128], BF16)
    qw = sb("qw", [128, 2, 128], BF16)
    dd = sb("dd", [128, 128], F32)
    rd = sb("rd", [128, 128], F32)
    hT = sb("hT", [128, 4, 128], F32)
    hsq = sb("hsq", [128, 4, 128], BF16)
    hr = sb("hr", [128, 128], F32)
    hr2 = sb("hr2", [128, 128], F32)
    kw = sb("kw", [128, DK], BF16)
    Cf = sb("Cf", [128, 2, DV], F32)
    Cb = sb("Cb", [128, 2, DV], BF16)
    nf = sb("nf", [128, 2], F32)
    nrep = sb("nrep", [128, 2, 128], BF16)
    S.add("dve", lambda e: e.memset(Cf[:], 0.0), w=["Cf"])
    S.add("dve", lambda e: e.memset(nf[:], 0.0), w=["nf"])
    S.add("pool", lambda e: e.memset(Cb[:], 0.0), w=["Cb"])
    S.add("pool", lambda e: e.memset(nrep[:], 0.0), w=["nrep"])
    ho_r = hout.rearrange("(c p) t -> p c t", p=128) if out_ag is None else None
    R = rows
    ag_pending = []
    if gth is None:
        qT_r = qT.rearrange("(c p) t -> p c t", p=128)
        kT_r = kT.rearrange("(c p) t -> p c t", p=128)
        ktm_r = k_tm.rearrange("(b p) n -> p b n", p=128)
        vtm_r = v_tm.rearrange("(b p) n -> p b n", p=128)
    else:
        g8 = sb("g8", [1, 8, SEG], F32)
        oh = sb("oh", [1, 4], F32)
        S.add("sp", lambda e: e.dma_start(out=oh[:], in_=gth["onehot"]), w=["oh"], slot="oh")
    for sg in range(SEQ // SEG):
        t0 = sg * SEG
        sl = sg % 2
        if gth is None:
            for dk in range(2):
                S.add("sp", lambda e, t0=t0, sl=sl, dk=dk: e.dma_start(out=qt[sl][:, dk, :], in_=qT_r[:, dk, t0:t0 + SEG]), w=[("qt", sl, dk)], slot="qt%d_%d" % (sl, dk))
                S.add("sp", lambda e, t0=t0, sl=sl, dk=dk: e.dma_start(out=kt[sl][:, dk, :], in_=kT_r[:, dk, t0:t0 + SEG]), w=[("kt", sl, dk)], slot="kt%d_%d" % (sl, dk))
            for c in range(NCH):
                S.add("sp", lambda e, t0=t0, sl=sl, c=c: e.dma_start(out=ktm[sl][:, c, :], in_=ktm_r[:, t0 // 128 + c, :]), w=[("ktm", sl, c)], slot="ktm%d_%d" % (sl, c))
                S.add("sp", lambda e, t0=t0, sl=sl, c=c: e.dma_start(out=vtm[sl][:, c, :], in_=vtm_r[:, t0 // 128 + c, :]), w=[("vtm", sl, c)], slot="vtm%d_%d" % (sl, c))
            S.add("sp", lambda e, t0=t0: e.dma_start(out=R["gi"][:], in_=gi[:, t0:t0 + SEG]), w=["r_gi"], slot="r_gi")
            S.add("sp", lambda e, t0=t0: e.dma_start(out=R["gf"][:], in_=gf[:, t0:t0 + SEG]), w=["r_gf"], slot="r_gf")
        else:
            ixt, ixkey = gth["ixt"], gth["ixkey"]
            for dk in range(2):
                col = gth["MQ"] + sg * 2 + dk
                S.add("pool", lambda e, sl=sl, dk=dk, col=col: e.indirect_dma_start(
                    out=qt[sl][:, dk, :], out_offset=None, in_=gth["q_view"],
                    in_offset=bass.IndirectOffsetOnAxis(ap=ixt[:, col:col + 1], axis=0)),
                    r=[ixkey] + list(gth["agk"]["q"]), w=[("qt", sl, dk)], slot="qt%d_%d" % (sl, dk))
                S.add("pool", lambda e, sl=sl, dk=dk, col=col: e.indirect_dma_start(
                    out=kt[sl][:, dk, :], out_offset=None, in_=gth["k_view"],
                    in_offset=bass.IndirectOffsetOnAxis(ap=ixt[:, col:col + 1], axis=0)),
                    r=[ixkey] + list(gth["agk"]["k"]), w=[("kt", sl, dk)], slot="kt%d_%d" % (sl, dk))
            for c in range(NCH):
                col = gth["MK"] + sg * NCH + c
                colv = gth["MV"] + sg * NCH + c
                S.add("pool", lambda e, sl=sl, c=c, col=col: e.indirect_dma_start(
                    out=ktm[sl][:, c, :], out_offset=None, in_=gth["ktm_view"],
                    in_offset=bass.IndirectOffsetOnAxis(ap=ixt[:, col:col + 1], axis=0)),
                    r=[ixkey] + list(gth["agk"]["ktm"]), w=[("ktm", sl, c)], slot="ktm%d_%d" % (sl, c))
                S.add("pool", lambda e, sl=sl, c=c, colv=colv: e.indirect_dma_start(
                    out=vtm[sl][:, c, :], out_offset=None, in_=gth["vtm_view"],
                    in_offset=bass.IndirectOffsetOnAxis(ap=ixt[:, colv:colv + 1], axis=0)),
                    r=[ixkey] + list(gth["agk"]["vtm"]), w=[("vtm", sl, c)], slot="vtm%d_%d" % (sl, c))
            srank, half = sg // 2, sg % 2
            gsrc = gth["gates_all"][srank * 8:(srank + 1) * 8, half * SEG:(half + 1) * SEG].rearrange("(o r) t -> o r t", o=1)
            S.add("sp", lambda e, gsrc=gsrc: e.dma_start(out=g8[:], in_=gsrc), r=list(gth["agk"]["g"]), w=["g8"], slot="g8")
            for gi_, (dst, off) in enumerate((("gi", 0), ("gf", 4))):
                S.add("dve", lambda e, dst=dst, off=off: e.tensor_scalar(R[dst][:], g8[0:1, off, :], oh[0:1, 0:1], None, ALU.mult),
                      r=["g8", "oh"], w=["r_" + dst])
                for j in range(1, 4):
                    S.add("dve", lambda e, dst=dst, off=off, j=j: e.scalar_tensor_tensor(R[dst][:], g8[0:1, off + j, :], oh[0:1, j:j + 1], R[dst][:], ALU.mult, ALU.add),
                          r=["g8", "oh", "r_" + dst], w=["r_" + dst])
        for fn in ag_pending:
            fn()
        del ag_pending[:]
        S.add("act", lambda e: e.activation(out=R["e"][:], in_=R["gf"][:], func=AF.Exp, scale=-1.0, bias=nbf[0:1, 0:1]),
              r=["r_gf", "nbf"], w=["r_e"])
        S.add("act", lambda e: e.activation(out=R["lf"][:], in_=R["e"][:], func=AF.Ln, bias=ones_f[0:1, 0:1]),
              r=["r_e", "ones_f"], w=["r_lf"])
        S.add("dve", lambda e: e.tensor_tensor_scan(R["Bn"][:], R["one"][:], R["lf"][:], carry[0:1, 0:1], ALU.mult, ALU.add),
              r=["r_lf", "carry", "r_one"], w=["r_Bn"])
        S.add("dve", lambda e: e.scalar_tensor_tensor(R["U"][:], R["gi"][:], bt[0:1, 0:1], R["Bn"][:], ALU.add, ALU.add),
              r=["r_gi", "bif", "r_Bn"], w=["r_U"])
        S.add("dve", lambda e: e.tensor_tensor_scan(R["G"][:], R["U"][:], R["U"][:], carry[0:1, 1:2], ALU.max, ALU.max),
              r=["r_U", "carry"], w=["r_G"])
        S.add("dve", lambda e: e.tensor_scalar(R["nG"][:], R["G"][:], -1.0, None, ALU.mult), r=["r_G"], w=["r_nG"])
        S.add("dve", lambda e: e.tensor_tensor(R["cl"][:], R["Bn"][:], R["G"][:], ALU.subtract), r=["r_Bn", "r_G"], w=["r_cl"])
        S.add("act", lambda e: e.activation(out=R["cl"][:], in_=R["cl"][:], func=AF.Exp), r=["r_cl"], w=["r_cl"])
        for c in range(NCH):
            a, b_ = c * CH, (c + 1) * CH
            gprev = carry[0:1, 1:2] if c == 0 else R["G"][0:1, a - 1:a]
            S.add("act", lambda e, a=a, b_=b_, gprev=gprev: e.activation(out=R["wi"][0:1, a:b_], in_=R["nG"][0:1, a:b_], func=AF.Exp, bias=gprev),
                  r=["r_nG", "r_G", "carry"], w=["r_wi"])
            S.add("act", lambda e, a=a, b_=b_: e.activation(out=R["we"][0:1, a:b_], in_=R["U"][0:1, a:b_], func=AF.Exp, bias=R["nG"][0:1, b_ - 1:b_]),
                  r=["r_nG", "r_U"], w=["r_we"])
        for n in ("nG", "wi", "cl"):
            for h in range(SEG // 512):
                S.add("pe", lambda e, n=n, h=h: e.matmul(banks[7][:, :], ones_f[0:1, :], R[n][0:1, h * 512:(h + 1) * 512], start=True, stop=True),
                      r=["r_" + n, "ones_f"], w=[("ps", 7)])
                S.add("act", lambda e, n=n, h=h: e.activation(out=bc[n][:, h * 512:(h + 1) * 512], in_=banks[7][:, :], func=AF.Copy),
                      r=[("ps", 7)], w=["bc_" + n])
        for c in range(NCH):
            a, b_ = c * CH, (c + 1) * CH
            S.add("pe", lambda e, c=c, a=a, b_=b_: e.matmul(banks[7][:, c:c + 1], R["U"][0:1, a:b_], ones_f[0:1, 0:1], start=True, stop=True),
                  r=["r_U", "ones_f"], w=[("ps", 7)])
            S.add("pe", lambda e, c=c, a=a, b_=b_: e.matmul(banks[7][:, NCH + c:NCH + c + 1], R["we"][0:1, a:b_], ones_f[0:1, 0:1], start=True, stop=True),
                  r=["r_we", "ones_f"], w=[("ps", 7)])
            S.add("pe", lambda e, c=c, b_=b_: e.matmul(banks[7][:, 2 * NCH + c:2 * NCH + c + 1], ones_f[0:1, :], R["wi"][0:1, b_ - 1:b_], start=True, stop=True),
                  r=["r_wi", "ones_f"], w=[("ps", 7)])
        S.add("dve", lambda e: e.tensor_copy(cols[:], banks[7][:, :3 * NCH]), r=[("ps", 7)], w=["cols"])
        S.add("dve", lambda e: e.tensor_copy(carry[0:1, 0:1], R["Bn"][0:1, SEG - 1:SEG]), r=["r_Bn", "r_wi", "r_we"], w=["carry"])
        S.add("dve", lambda e: e.tensor_copy(carry[0:1, 1:2], R["G"][0:1, SEG - 1:SEG]), r=["r_G", "r_wi", "r_we"], w=["carry"])
        if dbg is not None and sg == 0:
            S.add("sp", lambda e: e.dma_start(out=dbg["nG"], in_=bc["nG"][:]), r=["bc_nG"], slot="dbg0")
            S.add("sp", lambda e: e.dma_start(out=dbg["wi"], in_=bc["wi"][:]), r=["bc_wi"], slot="dbg1")
            S.add("sp", lambda e: e.dma_start(out=dbg["cl"], in_=bc["cl"][:]), r=["bc_cl"], slot="dbg2")
            S.add("sp", lambda e: e.dma_start(out=dbg["cols"], in_=cols[:]), r=["cols"], slot="dbg3")
            S.add("sp", lambda e: e.dma_start(out=dbg["tri"], in_=tri[:]), r=["tri"], slot="dbg4")
        for c in range(NCH):
            a, b_ = c * CH, (c + 1) * CH
            qs, ks, kms, vs = qt[sl], kt[sl], ktm[sl], vtm[sl]
            for dk in range(2):
                S.add("pe", lambda e, dk=dk, a=a, b_=b_, ks=ks, qs=qs: e.matmul(banks[0][:, :128], ks[:, dk, a:b_], qs[:, dk, a:b_], start=(dk == 0), stop=(dk == 1)),
                      r=[("kt", sl, dk), ("qt", sl, dk)], w=[("ps", 0)])
            S.add("act", lambda e, a=a, b_=b_, c=c: e.activation(out=PT[:], in_=bc["nG"][:, a:b_], func=AF.Exp, bias=cols[:, c:c + 1]),
                  r=["bc_nG", "cols"], w=["PT"])
            S.add(PENG, lambda e: e.tensor_tensor(PTm[:], PT[:], tri[:], ALU.mult), r=["PT", "tri"], w=["PTm"])
            S.add("dve", lambda e: e.tensor_tensor(AT[:], banks[0][:, :128], PTm[:], ALU.mult), r=[("ps", 0), "PTm"], w=["AT"])
            for dk in range(2):
                S.add("dve", lambda e, dk=dk, a=a, b_=b_, qs=qs: e.tensor_tensor(qw[:, dk, :], qs[:, dk, a:b_], bc["wi"][:, a:b_], ALU.mult),
                      r=[("qt", sl, dk), "bc_wi"], w=["qw"])
            for j in range(4):
                S.add("pe", lambda e, j=j, c=c, vs=vs: e.matmul(banks[1][:, j * 128:(j + 1) * 128], vs[:, c, j * 128:(j + 1) * 128], AT[:], start=True, stop=False),
                      r=[("vtm", sl, c), "AT"], w=[("ps", 1)])
                for dk in range(2):
                    S.add("pe", lambda e, j=j, dk=dk: e.matmul(banks[1][:, j * 128:(j + 1) * 128], Cb[:, dk, j * 128:(j + 1) * 128], qw[:, dk, :], start=False, stop=(dk == 1)),
                          r=["Cb", "qw"], w=[("ps", 1)])
            S.add("pe", lambda e: e.matmul(banks[2][:, :128], ones_bf[:], AT[:], start=True, stop=False), r=["ones_bf", "AT"], w=[("ps", 2)])
            for dk in range(2):
                S.add("pe", lambda e, dk=dk: e.matmul(banks[2][:, :128], nrep[:, dk, :], qw[:, dk, :], start=False, stop=(dk == 1)),
                      r=["nrep", "qw"], w=[("ps", 2)])
            S.add("act", lambda e: e.activation(out=dd[:], in_=banks[2][:, :128], func=AF.Abs), r=[("ps", 2)], w=["dd"])
            S.add("dve", lambda e, a=a, b_=b_: e.tensor_tensor(dd[:], dd[:], bc["cl"][:, a:b_], ALU.max), r=["dd", "bc_cl"], w=["dd"])
            S.add("dve", lambda e: e.reciprocal(rd[:], dd[:]), r=["dd"], w=["rd"])
            for j in range(4):
                S.add("dve", lambda e, j=j: e.tensor_tensor(hT[:, j, :], banks[1][:, j * 128:(j + 1) * 128], rd[:], ALU.mult),
                      r=[("ps", 1), "rd"], w=["hT"])
            S.add("act", lambda e: e.activation(out=hsq[:], in_=hT[:], func=AF.Square), r=["hT"], w=["hsq"])
            for j in range(4):
                S.add("pe", lambda e, j=j: e.matmul(banks[3][:, :128], ones_bf[:], hsq[:, j, :], start=(j == 0), stop=(j == 3)),
                      r=["ones_bf", "hsq"], w=[("ps", 3)])
            S.add("dve", lambda e: e.tensor_scalar(hr[:], banks[3][:, :128], 1.0 / DV, EPS, ALU.mult, ALU.add), r=[("ps", 3)], w=["hr"])
            S.add("act", lambda e: e.activation(out=hr2[:], in_=hr[:], func=AF.Ln), r=["hr"], w=["hr2"])
            S.add("act", lambda e: e.activation(out=hr[:], in_=hr2[:], func=AF.Exp, scale=-0.5), r=["hr2"], w=["hr"])
            for j in range(4):
                S.add("dve", lambda e, j=j, a=a, b_=b_, sl=sl: e.scalar_tensor_tensor(hst[sl][:, j, a:b_], hT[:, j, :], gh[:, j:j + 1], hr[:], ALU.mult, ALU.mult),
                      r=["hT", "hr", "gh"], w=[("hst", sl)])
            S.add("dve", lambda e, c=c, kms=kms: e.tensor_scalar(kw[:], kms[:, c, :], cols[:, NCH + c:NCH + c + 1], None, ALU.mult),
                  r=[("ktm", sl, c), "cols"], w=["kw"])
            for dk in range(2):
                S.add("pe", lambda e, dk=dk, c=c, vs=vs: e.matmul(banks[4 + dk][:, :], kw[:, dk * 128:(dk + 1) * 128], vs[:, c, :], start=True, stop=True),
                      r=["kw", ("vtm", sl, c)], w=[("ps", 4 + dk)])
                S.add("pe", lambda e, dk=dk: e.matmul(banks[6][:, dk:dk + 1], kw[:, dk * 128:(dk + 1) * 128], ones_bf[:, 0:1], start=True, stop=True),
                      r=["kw", "ones_bf"], w=[("ps", 6)])
            for dk in range(2):
                S.add("dve", lambda e, dk=dk, c=c: e.scalar_tensor_tensor(Cf[:, dk, :], Cf[:, dk, :], cols[:, 2 * NCH + c:2 * NCH + c + 1], banks[4 + dk][:, :], ALU.mult, ALU.add),
                      r=["Cf", "cols", ("ps", 4 + dk), "Cb"], w=["Cf"])
            S.add("dve", lambda e, c=c: e.scalar_tensor_tensor(nf[:], nf[:], cols[:, 2 * NCH + c:2 * NCH + c + 1], banks[6][:, 0:2], ALU.mult, ALU.add),
                  r=["nf", "cols", ("ps", 6)], w=["nf"])
            S.add("act", lambda e: e.activation(out=Cb[:], in_=Cf[:], func=AF.Copy), r=["Cf"], w=["Cb"])
            for dk in range(2):
                S.add(PENG, lambda e, dk=dk: e.tensor_scalar(nrep[:, dk, :], ones_f[:], nf[:, dk:dk + 1], None, ALU.mult),
                      r=["nf", "ones_f"], w=["nrep"])
        if out_ag is None:
            S.add("sp", lambda e, t0=t0, sl=sl: e.dma_start(out=ho_r[:, :, t0:t0 + SEG], in_=hst[sl][:]), r=[("hst", sl)], w=[("dram", "ho", sg)], slot="hst%d" % sl)
        else:
            dst = out_ag["h_b"][sg * 512:(sg + 1) * 512, :].rearrange("(c p) t -> p c t", p=128)
            S.add("sp", lambda e, dst=dst, sl=sl: e.dma_start(out=dst, in_=hst[sl][:]), r=[("hst", sl)], w=[("dram", "ho", sg)], slot="hst%d" % sl)
            ag_pending.append(lambda sg=sg: out_ag["agf"](S, "a", 256, [2 * sg, 2 * sg + 1], [("dram", "ho", sg)]))
    for fn in ag_pending:
        fn()


def build_mlstm_core(SEQ, debug=False):
    nc = bass.Bass("TRN2", target_bir_lowering=False)
    qT = nc.dram_tensor("qT", [256, SEQ], BF16, kind="ExternalInput").ap()
    kT = nc.dram_tensor("kT", [256, SEQ], BF16, kind="ExternalInput").ap()
    k_tm = nc.dram_tensor("k_tm", [SEQ, 256], BF16, kind="ExternalInput").ap()
    v_tm = nc.dram_tensor("v_tm", [SEQ, 512], BF16, kind="ExternalInput").ap()
    gi = nc.dram_tensor("gi", [1, SEQ], F32, kind="ExternalInput").ap()
    gf = nc.dram_tensor("gf", [1, SEQ], F32, kind="ExternalInput").ap()
    bif = nc.dram_tensor("bif", [1, 2], F32, kind="ExternalInput").ap()
    ghead = nc.dram_tensor("ghead", [128, 4], F32, kind="ExternalInput").ap()
    hout = nc.dram_tensor("hout", [512, SEQ], F32, kind="ExternalOutput").ap()
    S = Sched(nc)
    dbg = None
    if debug:
        dbg = {n: nc.dram_tensor("dbg_" + n, [128, 1024], F32, kind="ExternalOutput").ap() for n in ("nG", "wi", "cl")}
        dbg["cols"] = nc.dram_tensor("dbg_cols", [128, 24], F32, kind="ExternalOutput").ap()
        dbg["tri"] = nc.dram_tensor("dbg_tri", [128, 128], F32, kind="ExternalOutput").ap()
    emit_mlstm_core(S, SEQ, qT, kT, k_tm, v_tm, gi, gf, bif, ghead, hout, dbg=dbg)
    S.emit()
    return nc, S


def emit_sb_core(S, NH, SEQ, qT, kT, v_tm, oT, gth=None, out_ag=None):
    sb, ps = S.sb, S.ps
    NB = SEQ // 128
    NQB = SEQ // 512
    zb = [ps("zb%d" % i, [128, 512], F32) for i in range(2)]
    ab = [ps("ab%d" % i, [128, 512], F32) for i in range(2)]
    ob = [ps("ob%d" % i, [128, 512], F32) for i in range(2)]
    ones_f = sb("ones_f", [128, 128], F32)
    ntri = sb("ntri", [128, 128], BF16)
    nones = sb("nones", [128, 128], BF16)
    smask = sb("smask", [128, 128], BF16)
    tmpf = sb("tmpf", [128, 128], F32)
    S.add("dve", lambda e: e.memset(ones_f[:], 1.0), w=["ones_f"])
    S.add("dve", lambda e: e.memset(nones[:], -1.0), w=["nones"])
    S.add("pool", lambda e: e.memset(tmpf[:], -1.0), w=["tmpf"])
    S.add("pool", lambda e: e.affine_select(out=tmpf[:], in_=tmpf[:], pattern=[[-1, 128]], compare_op=ALU.is_ge, fill=0.0,
                                            base=0, channel_multiplier=1), r=["tmpf"], w=["tmpf"])
    S.add("dve", lambda e: e.tensor_copy(ntri[:], tmpf[:]), r=["tmpf"], w=["ntri"])
    tmpg = sb("tmpg", [128, 128], F32)
    S.add("pool", lambda e: e.memset(tmpg[:], 1.0), w=["tmpg"])
    S.add("pool", lambda e: e.affine_select(out=tmpg[:], in_=tmpg[:], pattern=[[1, 128]], compare_op=ALU.is_ge, fill=0.0,
                                            base=-1, channel_multiplier=-1), r=["tmpg"], w=["tmpg"])
    S.add("dve", lambda e: e.tensor_copy(smask[:], tmpg[:]), r=["tmpg"], w=["smask"])
    qhs = [sb("qh%d" % i, [128, SEQ], BF16) for i in range(2)]
    khs = [sb("kh%d" % i, [128, SEQ], BF16) for i in range(2)]
    vhs = [sb("vh%d" % i, [128, NB, 128], BF16) for i in range(2)]
    KBATCH = 12
    NBUF = 2 * KBATCH + 2
    e_sb = [sb("e_sb%d" % i, [128, 512], F32) for i in range(2)] if not USE_SOFTPLUS else None
    sp_bf = [sb("sp_bf%d" % i, [128, 512], BF16) for i in range(NBUF)]
    A_bf = [sb("A_bf%d" % i, [128, 512], BF16) for i in range(NBUF)]
    RS_f = sb("RS_f", [128, 512], F32)
    RS_b = [sb("RS_b%d" % i, [128, 512], BF16) for i in range(NBUF)]
    ost = [sb("ost%d" % i, [128, 512], F32) for i in range(2)]
    TC = SEQ // 4

    its = []
    for h in range(NH):
        for QB in range(NQB):
            kb_hi = 4 * QB + 3
            for kb in range(kb_hi, -1, -1):
                its.append((h, QB, kb))

    def geom(n):
        h, QB, kb = its[n]
        r_ = kb - 4 * QB
        col0 = r_ * 128 if r_ >= 0 else 0
        return h, QB, kb, r_, col0, 512 - col0, kb == 4 * QB + 3, QB * 512

    def load_head(h):
        hp = h % 2
        qh, kh, vh = qhs[hp], khs[hp], vhs[hp]
        if gth is None:
            S.add("sp", lambda e, h=h: e.dma_start(out=qh[:], in_=qT[h]), w=[("qh", hp, i) for i in range(4)], slot="qh%d" % hp)
            S.add("sp", lambda e, h=h: e.dma_start(out=kh[:], in_=kT[h]), w=[("kh", hp, i) for i in range(4)], slot="kh%d" % hp)
            S.add("sp", lambda e, h=h: e.dma_start(out=vh[:], in_=v_tm[h].rearrange("(b p) d -> p b d", p=128)),
                  w=[("vh", hp, i) for i in range((NB + 7) // 8)], slot="vh%d" % hp)
        else:
            ixt, ixkey = gth["ixt"], gth["ixkey"]
            for s_ in range(4):
                col = gth["SQ"] + h * 4 + s_
                S.add("pool", lambda e, s_=s_, col=col: e.indirect_dma_start(
                    out=qh[:, s_ * TC:(s_ + 1) * TC], out_offset=None, in_=gth["q_view"],
                    in_offset=bass.IndirectOffsetOnAxis(ap=ixt[:, col:col + 1], axis=0)),
                    r=[ixkey] + list(gth["agk"]["q"]), w=[("qh", hp, s_)], slot="qh%d_%d" % (hp, s_))
                S.add("pool", lambda e, s_=s_, col=col: e.indirect_dma_start(
                    out=kh[:, s_ * TC:(s_ + 1) * TC], out_offset=None, in_=gth["k_view"],
                    in_offset=bass.IndirectOffsetOnAxis(ap=ixt[:, col:col + 1], axis=0)),
                    r=[ixkey] + list(gth["agk"]["k"]), w=[("kh", hp, s_)], slot="kh%d_%d" % (hp, s_))
            for blk in range(NB):
                col = gth["SV"] + h * NB + blk
                S.add("pool", lambda e, blk=blk, col=col: e.indirect_dma_start(
                    out=vh[:, blk, :], out_offset=None, in_=gth["v_view"],
                    in_offset=bass.IndirectOffsetOnAxis(ap=ixt[:, col:col + 1], axis=0)),
                    r=[ixkey] + list(gth["agk"]["v"]), w=[("vh", hp, blk // 8)], slot="vh%d_%d" % (hp, blk // 8), group=True)

    def part_a(n):
        h, QB, kb, r_, col0, ncol, first, q0 = geom(n)
        if n == 0:
            load_head(0)
        if (n == 0 or its[n - 1][0] != h) and h + 1 < NH:
            load_head(h + 1)
        i2, i3 = n % 2, n % NBUF
        hp = h % 2
        kblk = khs[hp][:, kb * 128:(kb + 1) * 128]
        qcols = qhs[hp][:, q0 + col0:q0 + 512]
        khk, qhk = ("kh", hp, (kb * 128) // TC), ("qh", hp, q0 // TC)
        if first:
            S.add("dve", lambda e: e.memset(RS_f[:], 0.0), w=["RS_f"])
        S.add("pe", lambda e: e.matmul(zb[i2][:, :ncol], kblk, qcols, start=True, stop=True), r=[khk, qhk], w=[("zb", i2)])
        if USE_SOFTPLUS:
            S.add("act", lambda e: e.activation(out=sp_bf[i3][:, :ncol], in_=zb[i2][:, :ncol], func=AF.Softplus),
                  r=[("zb", i2)], w=[("sp", i3)])
        else:
            S.add("act", lambda e: e.activation(out=e_sb[i2][:, :ncol], in_=zb[i2][:, :ncol], func=AF.Exp), r=[("zb", i2)], w=[("e_sb", i2)])
            S.add("act", lambda e: e.activation(out=sp_bf[i3][:, :ncol], in_=e_sb[i2][:, :ncol], func=AF.Ln, bias=ones_f[:, 0:1]),
                  r=[("e_sb", i2), "ones_f"], w=[("sp", i3)])
        if r_ >= 0:
            S.add("pool", lambda e: e.tensor_tensor(sp_bf[i3][:, :128], sp_bf[i3][:, :128], smask[:], ALU.mult),
                  r=[("sp", i3), "smask"], w=[("sp", i3)])
        if kb > 0:
            S.add("dve", lambda e: e.tensor_tensor(RS_f[:, col0:512], RS_f[:, col0:512], sp_bf[i3][:, :ncol], ALU.add),
                  r=[("sp", i3), "RS_f"], w=["RS_f"])
            nx = (n + 1) % NBUF
            S.add("dve", lambda e: e.tensor_copy(RS_b[nx][:], RS_f[:]), r=["RS_f"], w=[("RS_b", nx)])

    def part_b(n):
        h, QB, kb, r_, col0, ncol, first, q0 = geom(n)
        i2, i3 = n % 2, n % NBUF
        hp = h % 2
        kblk = khs[hp][:, kb * 128:(kb + 1) * 128]
        qcols = qhs[hp][:, q0 + col0:q0 + 512]
        khk, qhk = ("kh", hp, (kb * 128) // TC), ("qh", hp, q0 // TC)
        S.add("pe", lambda e: e.matmul(ab[i2][:, :ncol], kblk, qcols, start=True, stop=False), r=[khk, qhk], w=[("ab", i2)])
        S.add("pe", lambda e: e.matmul(ab[i2][:, :ncol], ntri[:], sp_bf[i3][:, :ncol], start=False, stop=first),
              r=["ntri", ("sp", i3)], w=[("ab", i2)])
        if not first:
            S.add("pe", lambda e: e.matmul(ab[i2][:, :ncol], nones[:], RS_b[i3][:, col0:512], start=False, stop=True),
                  r=["nones", ("RS_b", i3)], w=[("ab", i2)])
        S.add("act", lambda e: e.activation(out=A_bf[i3][:, :ncol], in_=ab[i2][:, :ncol], func=AF.Exp), r=[("ab", i2)], w=[("A", i3)])
        if r_ >= 0:
            S.add("pool", lambda e: e.tensor_tensor(A_bf[i3][:, :128], A_bf[i3][:, :128], smask[:], ALU.mult),
                  r=[("A", i3), "smask"], w=[("A", i3)])

    def part_c(n):
        h, QB, kb, r_, col0, ncol, first, q0 = geom(n)
        i3 = n % NBUF
        o_i = (h * NQB + QB) % 2
        hp = h % 2
        S.add("pe", lambda e: e.matmul(ob[o_i][:, col0:512], vhs[hp][:, kb, :], A_bf[i3][:, :ncol], start=first, stop=(kb == 0)),
              r=[("vh", hp, kb // 8), ("A", i3)], w=[("ob", o_i)])
        if kb == 0:
            S.add("dve", lambda e: e.tensor_copy(ost[o_i][:], ob[o_i][:]), r=[("ob", o_i)], w=[("ost", o_i)])
            if out_ag is None:
                S.add("sp", lambda e: e.dma_start(out=oT[h][:, q0:q0 + 512], in_=ost[o_i][:]),
                      r=[("ost", o_i)], w=[("dram", "o", h, QB)], slot="ost%d" % o_i)
            else:
                ck = h * 4 + QB // 4
                dst = out_ag["o_b"][ck * 128:(ck + 1) * 128, (QB % 4) * 512:(QB % 4 + 1) * 512]
                S.add("sp", lambda e: e.dma_start(out=dst, in_=ost[o_i][:]),
                      r=[("ost", o_i)], w=[("dram", "o", h, QB)], slot="ost%d" % o_i)
                if QB % 4 == 3:
                    ag_pending.append([n + 12, lambda: out_ag["agf"](S, "a", 128, [ck], [("dram", "o", h, QB - i_) for i_ in range(4)])])

    N = len(its)
    ag_pending = []
    nbat = (N + KBATCH - 1) // KBATCH
    for m in range(nbat + 2):
        for item in [it_ for it_ in ag_pending if it_[0] <= m * KBATCH]:
            item[1]()
            ag_pending.remove(item)
        for i_ in range(KBATCH):
            n = m * KBATCH + i_
            if n < N:
                part_a(n)
            nc_ = (m - 2) * KBATCH + i_
            if 0 <= m - 2 < nbat and nc_ < N:
                part_c(nc_)
        if 0 <= m - 1 < nbat:
            for n in range((m - 1) * KBATCH, min(N, m * KBATCH)):
                part_b(n)
    for item in ag_pending:
        item[1]()


def build_sb_core(NH, SEQ):
    nc = bass.Bass("TRN2", target_bir_lowering=False)
    qT = nc.dram_tensor("qT", [NH, 128, SEQ], BF16, kind="ExternalInput").ap()
    kT = nc.dram_tensor("kT", [NH, 128, SEQ], BF16, kind="ExternalInput").ap()
    v_tm = nc.dram_tensor("v_tm", [NH, SEQ, 128], BF16, kind="ExternalInput").ap()
    oT = nc.dram_tensor("oT", [NH, 128, SEQ], F32, kind="ExternalOutput").ap()
    S = Sched(nc)
    emit_sb_core(S, NH, SEQ, qT, kT, v_tm, oT)
    S.emit()
    return nc, S


def _vec(nc, name, n):
    return nc.dram_tensor(name, [128, n], F32, kind="ExternalInput").ap()


def build_ple(T, TT=512):
    nc = bass.Bass("TRN2", target_bir_lowering=False)
    xin = nc.dram_tensor("xin", [D, T], F32, kind="ExternalInput").ap()
    pT = nc.dram_tensor("pT", [256, T], F32, kind="ExternalInput").ap()
    g = _vec(nc, "g", KC)
    wpe = nc.dram_tensor("wpe", [256, D], F32, kind="ExternalInput").ap()
    wpg = nc.dram_tensor("wpg", [D, D], F32, kind="ExternalInput").ap()
    xout = nc.dram_tensor("xout", [D, T], F32, kind="ExternalOutput").ap()
    S = Sched(nc)
    C = Ctx(S, TT)
    C.load_vec("g", g, KC)
    emit_ple(C, xin, xout, pT, T, "g", wpe, wpg)
    S.emit()
    return nc, S


def build_proj_res(T, gated, TT=512):
    nc = bass.Bass("TRN2", target_bir_lowering=False)
    xin = nc.dram_tensor("xin", [D, T], F32, kind="ExternalInput").ap()
    aT = nc.dram_tensor("aT", [D, T], F32, kind="ExternalInput").ap()
    bT = nc.dram_tensor("bT", [D, T], F32, kind="ExternalInput").ap() if gated else None
    W = nc.dram_tensor("W", [D, D], F32, kind="ExternalInput").ap()
    xout = nc.dram_tensor("xout", [D, T], F32, kind="ExternalOutput").ap()
    S = Sched(nc)
    C = Ctx(S, TT)
    emit_proj_res(C, xin, xout, aT, bT, T, W)
    S.emit()
    return nc, S


def build_mlstm_in(T, TT=512):
    nc = bass.Bass("TRN2", target_bir_lowering=False)
    xin = nc.dram_tensor("xin", [D, T], F32, kind="ExternalInput").ap()
    g = _vec(nc, "g", KC)
    w_in = nc.dram_tensor("w_in", [D, 6152], F32, kind="ExternalInput").ap()
    qT = nc.dram_tensor("qT", [1024, T], BF16, kind="ExternalOutput").ap()
    kT = nc.dram_tensor("kT", [1024, T], BF16, kind="ExternalOutput").ap()
    k_tm = nc.dram_tensor("k_tm", [T, 1024], BF16, kind="ExternalOutput").ap()
    v_tm = nc.dram_tensor("v_tm", [T, 2048], BF16, kind="ExternalOutput").ap()
    sog = nc.dram_tensor("sog", [2048, T], F32, kind="ExternalOutput").ap()
    gates = nc.dram_tensor("gates", [8, T], F32, kind="ExternalOutput").ap()
    S = Sched(nc)
    C = Ctx(S, TT)
    C.load_vec("g", g, KC)
    emit_mlstm_in(C, xin, T, "g", w_in, qT, kT, k_tm, v_tm, sog, gates)
    S.emit()
    return nc, S


def build_kv(T, TT=512):
    nc = bass.Bass("TRN2", target_bir_lowering=False)
    xin = nc.dram_tensor("xin", [D, T], F32, kind="ExternalInput").ap()
    g = _vec(nc, "g", KC)
    gk = _vec(nc, "g_k", 1)
    w_kv = nc.dram_tensor("w_kv", [D, 4096], F32, kind="ExternalInput").ap()
    kT = nc.dram_tensor("kT", [2048, T], BF16, kind="ExternalOutput").ap()
    v_tm = nc.dram_tensor("v_tm", [T, 2048], BF16, kind="ExternalOutput").ap()
    S = Sched(nc)
    C = Ctx(S, TT)
    C.load_vec("g", g, KC)
    C.load_vec("g_k", gk, 1)
    emit_kv(C, xin, T, "g", w_kv, kT, v_tm)
    S.emit()
    return nc, S


def build_q(T, TT=512):
    nc = bass.Bass("TRN2", target_bir_lowering=False)
    xin = nc.dram_tensor("xin", [D, T], F32, kind="ExternalInput").ap()
    g = _vec(nc, "g", KC)
    gq = _vec(nc, "g_q", 1)
    w_q = nc.dram_tensor("w_q", [D, D], F32, kind="ExternalInput").ap()
    qT = nc.dram_tensor("qT", [2048, T], BF16, kind="ExternalOutput").ap()
    S = Sched(nc)
    C = Ctx(S, TT)
    C.load_vec("g", g, KC)
    C.load_vec("g_q", gq, 1)
    for _ in emit_headproj(C, xin, T, "g", w_q, 0, "g_q", 128 ** -0.5, qT):
        pass
    S.emit()
    return nc, S


def build_ffn(T, TT=512):
    nc = bass.Bass("TRN2", target_bir_lowering=False)
    xin = nc.dram_tensor("xin", [D, T], F32, kind="ExternalInput").ap()
    g = nc.dram_tensor("g", [128, KC], F32, kind="ExternalInput").ap()
    wg = nc.dram_tensor("wg", [D, DFF], F32, kind="ExternalInput").ap()
    wu = nc.dram_tensor("wu", [D, DFF], F32, kind="ExternalInput").ap()
    wd = nc.dram_tensor("wd", [DFF, D], F32, kind="ExternalInput").ap()
    xout = nc.dram_tensor("xout", [D, T], F32, kind="ExternalOutput").ap()
    S = Sched(nc)
    C = Ctx(S, TT)
    C.load_vec("g", g, KC)
    emit_ffn(C, xin, xout, T, "g", wg, wu, wd)
    S.emit()
    return nc, S


RG = [[0, 1, 2, 3], [4, 5, 6, 7]]
IX = dict(MQ=0, MK=16, MV=80, PAM=144, PAS=208, SQ=272, SV=288)
NIDX = 544
TCORE = 2048
SEQLEN = 8192
I32 = mybir.dt.int32


def build_fused(plan="full"):
    nc = bass.Bass("TRN2", target_bir_lowering=False)
    T = TCORE
    ext_names = []
    _cache = {}
    shapes = {
        "xT": ([D, T], F32), "pT": ([4, 256, T], F32), "ffn_norm": ([4, 2, 128, KC], F32),
        "ffn_w_gate": ([4, 2, D, DFF], F32), "ffn_w_up": ([4, 2, D, DFF], F32), "ffn_w_down": ([4, 2, DFF, D], F32),
        "mix_norm": ([4, 128, KC], F32), "mlstm_w_in": ([2, D, 6152], F32), "bif": ([2, 1, 2], F32),
        "ghead": ([2, 128, 4], F32), "mlstm_w_out": ([2, D, D], F32), "kv_norm": ([128, KC], F32),
        "sb_w_kv": ([D, 4096], F32), "g_k": ([128, 1], F32), "sb_w_q": ([2, D, D], F32), "g_q": ([2, 128, 1], F32),
        "sb_w_o": ([2, D, D], F32), "ple_norm": ([4, 128, KC], F32), "ple_w_proj": ([4, 256, D], F32),
        "ple_w_gate": ([4, D, D], F32), "idx": ([128, NIDX], I32), "onehot": ([1, 4], F32),
    }

    class _Ext:
        def __getattr__(self, name):
            if name not in _cache:
                shp, dt = shapes[name]
                _cache[name] = nc.dram_tensor(name, shp, dt, kind="ExternalInput").ap()
                ext_names.append(name)
            return _cache[name]
    E = _Ext()
    nc._ext_names = ext_names
    outT = nc.dram_tensor("outT", [D, T], F32, kind="ExternalOutput").ap()

    def dt_(name, shape, dt=F32):
        return nc.dram_tensor(name, shape, dt)

    XA, XB = dt_("XA", [D, T]), dt_("XB", [D, T])
    q_b, k_b = dt_("q_b", [1024, T], BF16), dt_("k_b", [1024, T], BF16)
    ktm_b, vtm_b = dt_("ktm_b", [T, 1024], BF16), dt_("vtm_b", [T, 2048], BF16)
    gates_b = dt_("gates_b", [8, T])
    sog = dt_("sog", [2048, T])
    q_all, k_all = dt_("q_all", [4096, T], BF16), dt_("k_all", [4096, T], BF16)
    ktm_all, vtm_all = dt_("ktm_all", [4 * T, 1024], BF16), dt_("vtm_all", [4 * T, 2048], BF16)
    gates_all = dt_("gates_all", [32, T])
    h_bm, h_allm = dt_("h_bm", [8 * 512, 1024]), dt_("h_allm", [16 * 1024, 1024])
    h_bs, h_alls = dt_("h_bs", [16 * 128, 2048]), dt_("h_alls", [16 * 512, 2048])
    sq_b, sk_b, sv_b = dt_("sq_b", [2048, T], BF16), dt_("sk_b", [2048, T], BF16), dt_("sv_b", [T, 2048], BF16)
    sq_all, sk_all, sv_all = dt_("sq_all", [8192, T], BF16), dt_("sk_all", [8192, T], BF16), dt_("sv_all", [4 * T, 2048], BF16)

    stage_no = [0]

    def stage(fn):
        with nc.cleanup_on_exit():
            S = Sched(nc, "s%d_" % stage_no[0])
            stage_no[0] += 1
            fn(S)
            S.emit()

    def agc(S, name, src, dst, rk, ks, rkeys=()):
        for k in ks:
            S.add("pool", lambda e, k=k: e.collective_compute(
                "AllGather", ALU.bypass, replica_groups=RG,
                ins=[src[k * rk:(k + 1) * rk, :].opt()], outs=[dst[k * 4 * rk:(k + 1) * 4 * rk, :].opt()]),
                r=list(rkeys), w=[("ag", name)], slot="cc_%s" % name, inc=1, group=True)
        return [("ag", name)]

    def ag(S, name, src, dst, rk):
        return agc(S, name, src, dst, rk, range(src.shape[0] // rk))

    def load_idx(S):
        ixt = S.sb("ixt", [128, NIDX], I32)
        S.add("sp", lambda e: e.dma_start(out=ixt[:], in_=E.idx), w=["ixt"], slot="ixt")
        return ixt

    state = {"cur": E.xT, "flip": 0}

    def nxt_buf(final=False):
        if final:
            return outT
        t = (XA, XB)[state["flip"]]
        state["flip"] ^= 1
        return t.ap()

    def st_ffn(i, k):
        cur, nxt = state["cur"], nxt_buf()

        def f(S):
            C = Ctx(S, 512)
            C.load_vec("g", E.ffn_norm[i, k], KC)
            emit_ffn(C, cur, nxt, T, "g", E.ffn_w_gate[i, k], E.ffn_w_up[i, k], E.ffn_w_down[i, k])
        stage(f)
        state["cur"] = nxt

    def st_ple(i, final):
        cur, nxt = state["cur"], nxt_buf(final)

        def f(S):
            C = Ctx(S, 512)
            C.load_vec("g", E.ple_norm[i], KC)
            emit_ple(C, cur, nxt, E.pT[i], T, "g", E.ple_w_proj[i], E.ple_w_gate[i])
        stage(f)
        state["cur"] = nxt

    def st_proj(W, gated, final=False):
        cur, nxt = state["cur"], nxt_buf(final)

        def f(S):
            C = Ctx(S, 512)
            ixt = load_idx(S)
            if gated:
                view, cb = h_allm.ap().rearrange("r (k f) -> (r k) f", k=2), IX["PAM"]
            else:
                view, cb = h_alls.ap().rearrange("r (k f) -> (r k) f", k=4), IX["PAS"]
            emit_proj_res(C, cur, nxt, None, sog.ap() if gated else None, T, W, gth=(view, ixt, "ixt", cb, []))
        stage(f)
        state["cur"] = nxt

    def mlstm_part(i):
        def f(S, i=i):
            C = Ctx(S, 512)
            C.load_vec("g", E.mix_norm[i], KC)
            def agf(S, name, rk, ks, rkeys):
                src, dst = {"ktm": (ktm_b, ktm_all), "vtm": (vtm_b, vtm_all)}[name]
                agc(S, name, src, dst, rk, ks, rkeys)
            emit_mlstm_in(C, state["cur"], T, "g", E.mlstm_w_in[i], q_b.ap(), k_b.ap(), ktm_b.ap(), vtm_b.ap(), sog.ap(), gates_b.ap(), agf=agf)
        stage(f)

        def f(S, i=i):
            ixt = load_idx(S)
            agk = {"q": ag(S, "q", q_b, q_all, 256), "k": ag(S, "k", k_b, k_all, 256),
                   "ktm": [], "vtm": [], "g": ag(S, "g", gates_b, gates_all, 8)}
            gth = dict(IX)
            gth["agk"] = agk
            gth.update(ixt=ixt, ixkey="ixt", onehot=E.onehot,
                       q_view=q_all.ap().rearrange("r (two f) -> (r two) f", two=2),
                       k_view=k_all.ap().rearrange("r (two f) -> (r two) f", two=2),
                       ktm_view=ktm_all.ap().rearrange("t (h f) -> (t h) f", h=4),
                       vtm_view=vtm_all.ap().rearrange("t (h f) -> (t h) f", h=4),
                       gates_all=gates_all.ap())
            out_ag = {"h_b": h_bm.ap(), "agf": lambda S, name, rk, ks, rkeys: agc(S, name, h_bm, h_allm, rk, ks, rkeys)}
            emit_mlstm_core(S, SEQLEN, None, None, None, None, None, None, E.bif[i], E.ghead[i], None, gth=gth, out_ag=out_ag)
        stage(f)
        st_proj(E.mlstm_w_out[i], True, final=(plan == "mlstm0"))

    def kv_part():
        def f(S):
            C = Ctx(S, 512)
            C.load_vec("g", E.kv_norm, KC)
            C.load_vec("g_k", E.g_k, 1)
            emit_kv(C, state["cur"], T, "g", E.sb_w_kv, sk_b.ap(), sv_b.ap(),
                    agf=lambda S, name, rk, ks, rkeys: agc(S, name, sv_b, sv_all, rk, ks, rkeys))
        stage(f)

    def sb_part(j):
        def f(S, j=j):
            C = Ctx(S, 512)
            C.load_vec("g", E.mix_norm[2 + j], KC)
            C.load_vec("g_q", E.g_q[j], 1)
            for _ in emit_headproj(C, state["cur"], T, "g", E.sb_w_q[j], 0, "g_q", 128 ** -0.5, sq_b.ap()):
                pass
        stage(f)

        def f(S, j=j):
            ixt = load_idx(S)
            agk = {"q": ag(S, "q", sq_b, sq_all, 256), "k": [], "v": []}
            if j == 0:
                agk["k"] = ag(S, "k", sk_b, sk_all, 256)
            gth = dict(IX)
            gth["agk"] = agk
            gth.update(ixt=ixt, ixkey="ixt", q_view=sq_all.ap(), k_view=sk_all.ap(),
                       v_view=sv_all.ap().rearrange("t (h f) -> (t h) f", h=16))
            out_ag = {"o_b": h_bs.ap(), "agf": lambda S, name, rk, ks, rkeys: agc(S, name, h_bs, h_alls, rk, ks, rkeys)}
            emit_sb_core(S, 4, SEQLEN, None, None, None, None, gth=gth, out_ag=out_ag)
        stage(f)
        st_proj(E.sb_w_o[j], False, final=(plan == "sb0"))

    if plan == "mlstm0":
        mlstm_part(0)
        return nc
    if plan == "sb0":
        kv_part()
        sb_part(0)
        return nc
    for i in range(4):
        if i == 2:
            kv_part()
        st_ffn(i, 0)
        if i < 2:
            mlstm_part(i)
        else:
            sb_part(i - 2)
        st_ffn(i, 1)
        st_ple(i, final=(i == 3))
    return nc


_PROG = {}


def _c(a):
    return np.ascontiguousarray(a)


def _vl(g, n):
    g = np.asarray(g, dtype=np.float32)
    lead = g.shape[:-1]
    return _c(np.swapaxes(g.reshape(lead + (n, 128)), -1, -2))


def _idx_table(c):
    h = c % 4
    p = np.arange(128, dtype=np.int64)
    t = np.zeros((128, NIDX), dtype=np.int64)

    def g256(r, s_):
        return (r // 256) * 1024 + s_ * 256 + r % 256

    for sg in range(8):
        s_, half = sg // 2, sg % 2
        for dk in range(2):
            t[:, IX["MQ"] + sg * 2 + dk] = g256(h * 256 + dk * 128 + p, s_) * 2 + half
    for g in range(64):
        tg = g * 128 + p
        s_, tt = tg // 2048, tg % 2048
        t[:, IX["MK"] + g] = ((tt // 512) * 2048 + s_ * 512 + tt % 512) * 4 + h
        t[:, IX["MV"] + g] = g256(tt, s_) * 4 + h
    for cch in range(16):
        hh, r = cch // 4, (cch % 4) * 128 + p
        for ti in range(4):
            sg = h * 2 + ti // 2
            ck = sg * 2 + r // 256
            t[:, IX["PAM"] + cch * 4 + ti] = (ck * 1024 + hh * 256 + r % 256) * 2 + ti % 2
            ck = (r // 128) * 4 + h
            t[:, IX["PAS"] + cch * 4 + ti] = (ck * 512 + hh * 128 + r % 128) * 4 + ti
    for hl in range(4):
        for s_ in range(4):
            t[:, IX["SQ"] + hl * 4 + s_] = g256((4 * h + hl) * 128 + p, s_)
        for blk in range(64):
            tg = blk * 128 + p
            s_, tt = tg // 2048, tg % 2048
            t[:, IX["SV"] + hl * 64 + blk] = g256(tt, s_) * 16 + 4 * h + hl
    return t.astype(np.int32)


def kernel(x, p, ffn_norm, ffn_w_gate, ffn_w_up, ffn_w_down, mix_norm,
           mlstm_w_in, mlstm_b_if, mlstm_head_norm, mlstm_w_out,
           kv_norm, sb_w_kv, sb_k_norm, sb_w_q, sb_q_norm, sb_w_o,
           ple_norm, ple_w_proj, ple_w_gate):
    f32 = np.float32
    T = TCORE
    x = np.asarray(x, f32)
    p = np.asarray(p, f32)
    if "nc" not in _PROG:
        _PROG["nc"] = build_fused()
    nc = _PROG["nc"]
    shared = {
        "ffn_norm": _vl(ffn_norm, KC),
        "ffn_w_gate": _c(np.asarray(ffn_w_gate, f32)), "ffn_w_up": _c(np.asarray(ffn_w_up, f32)),
        "ffn_w_down": _c(np.asarray(ffn_w_down, f32)),
        "mix_norm": _vl(mix_norm, KC),
        "mlstm_w_in": _c(np.asarray(mlstm_w_in, f32)), "mlstm_w_out": _c(np.asarray(mlstm_w_out, f32)),
        "kv_norm": _vl(kv_norm, KC), "sb_w_kv": _c(np.asarray(sb_w_kv, f32)), "g_k": _vl(sb_k_norm, 1),
        "sb_w_q": _c(np.asarray(sb_w_q, f32)), "g_q": _vl(sb_q_norm, 1), "sb_w_o": _c(np.asarray(sb_w_o, f32)),
        "ple_norm": _vl(ple_norm, KC), "ple_w_proj": _c(np.asarray(ple_w_proj, f32)),
        "ple_w_gate": _c(np.asarray(ple_w_gate, f32)),
    }
    b_if = np.asarray(mlstm_b_if, f32)
    hn = np.asarray(mlstm_head_norm, f32)
    in_maps = []
    for c in range(NCORES):
        b, s = c // 4, c % 4
        h = s
        m = dict(shared)
        m["xT"] = _c(x[b, s * T:(s + 1) * T].T)
        m["pT"] = _c(p[:, b, s * T:(s + 1) * T, :].transpose(0, 2, 1))
        m["bif"] = _c(np.stack([b_if[:, h], b_if[:, 4 + h]], axis=-1)[:, None, :])
        m["ghead"] = _vl(hn[:, h * 512:(h + 1) * 512], 4)
        m["idx"] = _idx_table(c)
        oh = np.zeros((1, 4), f32)
        oh[0, h] = 1.0
        m["onehot"] = oh
        in_maps.append({k: v for k, v in m.items() if k in nc._ext_names})
    res = run_bass_kernel_spmd(nc, in_maps, core_ids=list(range(NCORES)))
    out = np.empty((2, SEQLEN, D), dtype=f32)
    for c in range(NCORES):
        b, s = c // 4, c % 4
        out[b, s * T:(s + 1) * T] = res.results[c]["outT"].T
    return out
```
